# Optimizing a Trainium2 kernel written in Bass

```python
import jax, jax.numpy as jnp
from jax import lax
import numpy as np

D_MODEL = 1024
BATCH = 8
SEQ = 4096
DEPTH = 2

HEAD_DIM = 64
SB_HEADS = 8
NSA_HEADS = 8
NSA_KV_HEADS = 2
NSA_GROUP = NSA_HEADS // NSA_KV_HEADS
SB_WIDTH = SB_HEADS * HEAD_DIM
NSA_WIDTH = NSA_HEADS * HEAD_DIM
KV_WIDTH = NSA_KV_HEADS * HEAD_DIM
N_BRANCH = 3
QKV_WIDTH = 3 * SB_WIDTH + NSA_WIDTH + 2 * N_BRANCH * KV_WIDTH + N_BRANCH * NSA_HEADS
CMP_LEN = 32
CMP_STRIDE = 16
CMP_HIDDEN = 256
SEL_LEN = 64
SEL_TOPK = 16
N_LOCAL = 2
WINDOW = 512
Q_BLOCK = 128
ROPE_THETA = 10000.0
D_FF = 2816
CONV_W = 3
EPS = 1e-6
NEG = -1e30
FORCED_SCORE = 1e6

kernel_name = 'hybrid_sbattn_nsa_convffn_adaln'


def rms_norm(x, g):
    xf = x.astype(jnp.float32)
    y = xf * lax.rsqrt(jnp.mean(xf * xf, axis=-1, keepdims=True) + EPS)
    return (y * g.astype(jnp.float32)).astype(x.dtype)


def rope(x, pos):
    half = HEAD_DIM // 2
    inv = ROPE_THETA ** (-jnp.arange(half, dtype=jnp.float32) / half)
    ang = pos.astype(jnp.float32)[:, None] * inv[None, :]
    cos = jnp.cos(ang)[:, None, :]
    sin = jnp.sin(ang)[:, None, :]
    xf = x.astype(jnp.float32)
    x1, x2 = xf[..., :half], xf[..., half:]
    return jnp.concatenate([x1 * cos - x2 * sin, x2 * cos + x1 * sin], axis=-1).astype(x.dtype)


def masked_softmax(s, mask):
    s = jnp.where(mask, s, NEG)
    m = jnp.max(s, axis=-1, keepdims=True)
    p = jnp.exp(s - m) * mask
    return p / jnp.maximum(jnp.sum(p, axis=-1, keepdims=True), 1e-6)


def stick_breaking_attention(q, k, v):
    B, T, H, D = q.shape
    nb = T // Q_BLOCK
    scale = D ** -0.5
    qb = q.reshape(B, nb, Q_BLOCK, H, D).transpose(1, 0, 3, 2, 4)
    key_pos = jnp.arange(T)

    def block(args):
        i, qi = args
        z = jnp.einsum('bhqd,bshd->bhqs', qi, k).astype(jnp.float32) * scale
        q_pos = i * Q_BLOCK + jnp.arange(Q_BLOCK)
        mask = key_pos[None, :] < q_pos[:, None]
        log_1m = jnp.where(mask, jax.nn.log_sigmoid(-z), 0.0)
        tail = lax.cumsum(log_1m, axis=3, reverse=True) - log_1m
        w = jnp.where(mask, jnp.exp(jax.nn.log_sigmoid(z) + tail), 0.0)
        return jnp.einsum('bhqs,bshd->bqhd', w, v.astype(jnp.float32))

    out = lax.map(block, (jnp.arange(nb), qb))
    return out.transpose(1, 0, 2, 3, 4).reshape(B, T, H, D)


def compress_blocks(x, pos_emb, w1, w2):
    B, T, G, D = x.shape
    r = CMP_LEN // CMP_STRIDE
    n_chunks = T // CMP_STRIDE
    n = n_chunks - r + 1
    chunks = x.reshape(B, n_chunks, CMP_STRIDE, G, D)
    blocks = jnp.concatenate([chunks[:, j:j + n] for j in range(r)], axis=2)
    blocks = blocks + pos_emb[None, None, :, None, :]
    flat = blocks.transpose(0, 1, 3, 2, 4).reshape(B, n, G, CMP_LEN * D)
    return jax.nn.gelu(flat @ w1) @ w2


def nsa_attention(q, k_c, v_c, k_s, v_s, k_w, v_w, gates,
                  cmp_pos_k, cmp_w1_k, cmp_w2_k, cmp_pos_v, cmp_w1_v, cmp_w2_v):
    B, T, H, D = q.shape
    G, HG = NSA_KV_HEADS, NSA_GROUP
    scale = D ** -0.5
    kc = compress_blocks(k_c, cmp_pos_k, cmp_w1_k, cmp_w2_k)
    vc = compress_blocks(v_c, cmp_pos_v, cmp_w1_v, cmp_w2_v).astype(jnp.float32)
    n_cmp = kc.shape[1]
    cmp_start = jnp.arange(n_cmp) * CMP_STRIDE
    cmp_end = cmp_start + CMP_LEN - 1
    kc = rope(kc, cmp_end)
    n_sel = T // SEL_LEN
    sel_start = jnp.arange(n_sel) * SEL_LEN
    ov = jnp.clip(jnp.minimum(cmp_start[:, None] + CMP_LEN, sel_start[None, :] + SEL_LEN)
                  - jnp.maximum(cmp_start[:, None], sel_start[None, :]), 0, None)
    ov = ov.astype(jnp.float32) / CMP_LEN
    ks_blocks = k_s.reshape(B, n_sel, SEL_LEN, G, D).transpose(0, 3, 1, 2, 4)
    vs_blocks = v_s.reshape(B, n_sel, SEL_LEN, G, D).transpose(0, 3, 1, 2, 4)
    top = min(SEL_TOPK, n_sel)
    kw_pad = jnp.pad(k_w, ((0, 0), (WINDOW, 0), (0, 0), (0, 0)))
    vw_pad = jnp.pad(v_w, ((0, 0), (WINDOW, 0), (0, 0), (0, 0)))
    nb = T // Q_BLOCK
    qb = q.reshape(B, nb, Q_BLOCK, G, HG, D).transpose(1, 0, 2, 3, 4, 5)
    gb = gates.reshape(B, nb, Q_BLOCK, G, HG, N_BRANCH).transpose(1, 0, 2, 3, 4, 5)
    b_ix = jnp.arange(B)[:, None, None, None]
    g_ix = jnp.arange(G)[None, :, None, None]
    sel_ids = jnp.arange(n_sel)

    def block(args):
        i, qi, gi = args
        q_pos = i * Q_BLOCK + jnp.arange(Q_BLOCK)
        s_c = jnp.einsum('bqghd,bngd->bghqn', qi, kc).astype(jnp.float32) * scale
        p_c = masked_softmax(s_c, cmp_end[None, :] <= q_pos[:, None])
        o_c = jnp.einsum('bghqn,bngd->bqghd', p_c, vc)
        imp = jnp.einsum('bghqn,nj->bgqj', p_c, ov)
        dist = (q_pos // SEL_LEN)[:, None] - sel_ids[None, :]
        forced = (sel_ids[None, :] == 0) | ((dist >= 0) & (dist < N_LOCAL))
        score = jnp.where(dist < 0, -1.0, jnp.where(forced, FORCED_SCORE, imp))
        top_score, idx = lax.top_k(score, top)
        kg = ks_blocks[b_ix, g_ix, idx].reshape(B, G, Q_BLOCK, top * SEL_LEN, D)
        vg = vs_blocks[b_ix, g_ix, idx].reshape(B, G, Q_BLOCK, top * SEL_LEN, D)
        tok = (idx[..., None] * SEL_LEN + jnp.arange(SEL_LEN)).reshape(B, G, Q_BLOCK, top * SEL_LEN)
        smask = (tok <= q_pos[None, None, :, None]) & jnp.repeat(top_score >= 0, SEL_LEN, axis=-1)
        s_s = jnp.einsum('bqghd,bgqsd->bghqs', qi, kg).astype(jnp.float32) * scale
        p_s = masked_softmax(s_s, smask[:, :, None])
        o_s = jnp.einsum('bghqs,bgqsd->bqghd', p_s, vg.astype(jnp.float32))
        kw = lax.dynamic_slice_in_dim(kw_pad, i * Q_BLOCK, WINDOW + Q_BLOCK, axis=1)
        vw = lax.dynamic_slice_in_dim(vw_pad, i * Q_BLOCK, WINDOW + Q_BLOCK, axis=1)
        kpos = i * Q_BLOCK - WINDOW + jnp.arange(WINDOW + Q_BLOCK)
        rel = q_pos[:, None] - kpos[None, :]
        wmask = (rel >= 0) & (rel < WINDOW) & (kpos[None, :] >= 0)
        s_w = jnp.einsum('bqghd,bsgd->bghqs', qi, kw).astype(jnp.float32) * scale
        p_w = masked_softmax(s_w, wmask)
        o_w = jnp.einsum('bghqs,bsgd->bqghd', p_w, vw.astype(jnp.float32))
        gf = gi.astype(jnp.float32)
        return gf[..., 0:1] * o_c + gf[..., 1:2] * o_s + gf[..., 2:3] * o_w

    out = lax.map(block, (jnp.arange(nb), qb, gb))
    return out.transpose(1, 0, 2, 3, 4, 5).reshape(B, T, H * D)


def causal_depthwise_conv(u, w, b):
    C = u.shape[-1]
    y = lax.conv_general_dilated(u, w[:, None, :].astype(u.dtype), window_strides=(1,),
                                 padding=[(CONV_W - 1, 0)],
                                 dimension_numbers=('NWC', 'WIO', 'NWC'),
                                 feature_group_count=C)
    return y + b


def token_mixer(h, w_in, cmp_pos_k, cmp_w1_k, cmp_w2_k, cmp_pos_v, cmp_w1_v, cmp_w2_v,
                sb_out_g, nsa_out_g, w_out):
    B, T, _ = h.shape
    proj = h @ w_in
    sizes = [SB_WIDTH] * 3 + [NSA_WIDTH] + [KV_WIDTH] * (2 * N_BRANCH)
    offsets = [int(v) for v in np.cumsum(sizes)]
    parts = jnp.split(proj, offsets, axis=-1)
    sb_q, sb_k, sb_v, nq, kc, vc, ks, vs, kw, vw, gl = parts
    hd = lambda t, n: t.reshape(B, T, n, HEAD_DIM)
    pos = jnp.arange(T)
    o_sb = stick_breaking_attention(hd(sb_q, SB_HEADS), hd(sb_k, SB_HEADS), hd(sb_v, SB_HEADS))
    o_sb = rms_norm(o_sb.reshape(B, T, SB_WIDTH), sb_out_g)
    gates = jax.nn.sigmoid(gl.astype(jnp.float32)).reshape(B, T, NSA_HEADS, N_BRANCH)
    o_nsa = nsa_attention(rope(hd(nq, NSA_HEADS), pos),
                          hd(kc, NSA_KV_HEADS), hd(vc, NSA_KV_HEADS),
                          rope(hd(ks, NSA_KV_HEADS), pos), hd(vs, NSA_KV_HEADS),
                          rope(hd(kw, NSA_KV_HEADS), pos), hd(vw, NSA_KV_HEADS),
                          gates, cmp_pos_k, cmp_w1_k, cmp_w2_k, cmp_pos_v, cmp_w1_v, cmp_w2_v)
    o_nsa = rms_norm(o_nsa, nsa_out_g)
    mixed = jnp.concatenate([o_sb.astype(h.dtype), o_nsa.astype(h.dtype)], axis=-1)
    return mixed @ w_out


def channel_mixer(h, ffn_w_in, ffn_conv_w, ffn_conv_b, ffn_w_down):
    u = causal_depthwise_conv(h @ ffn_w_in, ffn_conv_w, ffn_conv_b)
    a, b = jnp.split(u, 2, axis=-1)
    return (jax.nn.silu(a) * b) @ ffn_w_down


def setup_inputs(seed: int = 0) -> dict:
    key = jax.random.key(seed)
    ks = jax.random.split(key, 24)
    f32 = jnp.float32
    nrm = lambda k, shape, s: jax.random.normal(k, shape, f32) * s
    L = DEPTH
    return {
        'x': nrm(ks[0], (BATCH, SEQ, D_MODEL), 1.0),
        'c': nrm(ks[1], (BATCH, D_MODEL), 1.0),
        'ln1_g': 1.0 + nrm(ks[2], (L, D_MODEL), 0.02),
        'ln2_g': 1.0 + nrm(ks[3], (L, D_MODEL), 0.02),
        'w_ada': nrm(ks[4], (L, D_MODEL, 6 * D_MODEL), 0.5 * D_MODEL ** -0.5),
        'b_ada': nrm(ks[5], (L, 6 * D_MODEL), 0.02),
        'w_in': nrm(ks[6], (L, D_MODEL, QKV_WIDTH), D_MODEL ** -0.5),
        'cmp_pos_k': nrm(ks[7], (L, CMP_LEN, HEAD_DIM), 0.1),
        'cmp_w1_k': nrm(ks[8], (L, CMP_LEN * HEAD_DIM, CMP_HIDDEN), (CMP_LEN * HEAD_DIM) ** -0.5),
        'cmp_w2_k': nrm(ks[9], (L, CMP_HIDDEN, HEAD_DIM), CMP_HIDDEN ** -0.5),
        'cmp_pos_v': nrm(ks[10], (L, CMP_LEN, HEAD_DIM), 0.1),
        'cmp_w1_v': nrm(ks[11], (L, CMP_LEN * HEAD_DIM, CMP_HIDDEN), (CMP_LEN * HEAD_DIM) ** -0.5),
        'cmp_w2_v': nrm(ks[12], (L, CMP_HIDDEN, HEAD_DIM), CMP_HIDDEN ** -0.5),
        'sb_out_g': 1.0 + nrm(ks[13], (L, SB_WIDTH), 0.02),
        'nsa_out_g': 1.0 + nrm(ks[14], (L, NSA_WIDTH), 0.02),
        'w_out': nrm(ks[15], (L, D_MODEL, D_MODEL), D_MODEL ** -0.5),
        'ffn_w_in': nrm(ks[16], (L, D_MODEL, 2 * D_FF), D_MODEL ** -0.5),
        'ffn_conv_w': nrm(ks[17], (L, CONV_W, 2 * D_FF), CONV_W ** -0.5),
        'ffn_conv_b': nrm(ks[18], (L, 2 * D_FF), 0.02),
        'ffn_w_down': nrm(ks[19], (L, D_FF, D_MODEL), D_FF ** -0.5),
        'final_g': 1.0 + nrm(ks[20], (D_MODEL,), 0.02),
    }


def reference(x, c, ln1_g, ln2_g, w_ada, b_ada, w_in, cmp_pos_k, cmp_w1_k, cmp_w2_k,
              cmp_pos_v, cmp_w1_v, cmp_w2_v, sb_out_g, nsa_out_g, w_out,
              ffn_w_in, ffn_conv_w, ffn_conv_b, ffn_w_down, final_g):
    for l in range(DEPTH):
        mod = jax.nn.silu(c) @ w_ada[l] + b_ada[l]
        sh1, sc1, g1, sh2, sc2, g2 = [m[:, None, :] for m in jnp.split(mod, 6, axis=-1)]
        h = rms_norm(x, ln1_g[l]) * (1.0 + sc1) + sh1
        a = token_mixer(h, w_in[l], cmp_pos_k[l], cmp_w1_k[l], cmp_w2_k[l],
                        cmp_pos_v[l], cmp_w1_v[l], cmp_w2_v[l], sb_out_g[l], nsa_out_g[l], w_out[l])
        x = x + (g1 * a).astype(x.dtype)
        h = rms_norm(x, ln2_g[l]) * (1.0 + sc2) + sh2
        f = channel_mixer(h, ffn_w_in[l], ffn_conv_w[l], ffn_conv_b[l], ffn_w_down[l])
        x = x + (g2 * f).astype(x.dtype)
    return rms_norm(x, final_g)
```

```python
import numpy as np
import ml_dtypes
import concourse.bass as bass
import concourse.mybir as mybir
from concourse.bass_utils import run_bass_kernel_spmd

F32 = mybir.dt.float32
BF16 = mybir.dt.bfloat16
AF = mybir.ActivationFunctionType
ALU = mybir.AluOpType
AX = mybir.AxisListType

T = 4096
D = 1024
L = 2
NT = T // 128
DFF = 2816
EPS = 1e-6
N_FM = 22 * 128
N_TM = 792
N_EXT = N_FM + N_TM


class Res:
    __slots__ = ("w", "r", "excl")

    def __init__(self, excl=False):
        self.w = []
        self.r = []
        self.excl = excl


class Eng:
    def __init__(self, name, e, sem, is_pe=False):
        self.name = name
        self.e = e
        self.sem = sem
        self.count = 0
        self.waited = {}
        self.is_pe = is_pe

    def _wait(self, tok):
        sem, val = tok
        k = id(sem)
        if self.waited.get(k, 0) >= val:
            return
        self.e.wait_ge(sem, val)
        self.waited[k] = val

    def _deps(self, reads, writes):
        for r in reads:
            for t in r.w:
                if not (self.is_pe and t[0] is self.sem):
                    self._wait(t)
            if r.excl:
                for t in r.r:
                    if t[0] is not self.sem:
                        self._wait(t)
        for r in writes:
            for t in r.w:
                if not (self.is_pe and t[0] is self.sem):
                    self._wait(t)
            for t in r.r:
                if t[0] is self.sem:
                    continue
                self._wait(t)

    def op(self, fn, reads=(), writes=()):
        self._deps(reads, writes)
        ins = fn(self.e)
        self.count += 1
        ins.then_inc(self.sem, 1)
        tok = (self.sem, self.count)
        for r in reads:
            r.r = [t for t in r.r if t[0] is not self.sem] + [tok]
        for r in writes:
            r.w = [tok]
            r.r = []
        return tok


class Prog:
    def __init__(self, nc, n_dma_sems=40):
        self.nc = nc
        self._ctx = []
        mk = lambda name: self._enter(nc.semaphore(name))
        self.pe = Eng("pe", nc.tensor, mk("s_pe"), is_pe=True)
        self.act = Eng("act", nc.scalar, mk("s_act"))
        self.dve = Eng("dve", nc.vector, mk("s_dve"))
        self.pool = Eng("pool", nc.gpsimd, mk("s_pool"))
        self.sp = Eng("sp", nc.sync, mk("s_sp"))
        self.dma_sems = [mk(f"s_dma{i}") for i in range(n_dma_sems)]
        self.dma_vals = [0] * n_dma_sems
        self.dma_rr = 0

    def _enter(self, cm):
        v = cm.__enter__()
        self._ctx.append(cm)
        return v

    def sbuf(self, name, shape, dtype):
        return self._enter(self.nc.sbuf_tensor(name, list(shape), dtype))

    def psum(self, name, shape, dtype=F32):
        return self._enter(self.nc.psum_tensor(name, list(shape), dtype))

    def dma_group(self, items, reads=(), writes=()):
        dep = []
        for r in reads:
            dep += r.w
        for r in writes:
            dep += r.w
            dep += r.r
        toks = []
        for it in items:
            out, in_, q, kw = (list(it) + [None, None])[:4]
            q = q or self.sp
            kw = kw or {}
            i = self.dma_rr
            self.dma_rr = (self.dma_rr + 1) % len(self.dma_sems)
            sem = self.dma_sems[i]
            if self.dma_vals[i] > 0:
                q._wait((sem, self.dma_vals[i]))
            for t in dep:
                q._wait(t)
            ins = q.e.dma_start(out=out, in_=in_, **kw)
            self.dma_vals[i] += 16
            ins.then_inc(sem, 16)
            toks.append((sem, self.dma_vals[i]))
        for r in reads:
            r.r = r.r + toks
        for r in writes:
            r.w = list(toks)
            r.r = []
        return toks

    def dma(self, out, in_, reads=(), writes=(), q=None, **kw):
        return self.dma_group([(out, in_, q, kw)], reads, writes)

    def barrier(self):
        engs = (self.pe, self.act, self.dve, self.pool, self.sp)
        for e in engs:
            for i, sem in enumerate(self.dma_sems):
                if self.dma_vals[i] > 0:
                    e._wait((sem, self.dma_vals[i]))
            for o in engs:
                if o is not e and o.count > 0:
                    e._wait((o.sem, o.count))

    def finish(self):
        self.barrier()
        for cm in reversed(self._ctx):
            cm.__exit__(None, None, None)
        self._ctx = []


class Scope:
    _uid = [0]

    def __init__(self, P):
        self.P = P
        self.cms = []
        Scope._uid[0] += 1
        self.sfx = f"_s{Scope._uid[0]}"

    def sbuf(self, name, shape, dtype):
        cm = self.P.nc.sbuf_tensor(name + self.sfx, list(shape), dtype)
        v = cm.__enter__()
        self.cms.append(cm)
        return v

    def psum(self, name, shape, dtype=F32):
        isz = 4 if dtype == F32 else 2
        cm = self.P.nc.psum_tensor(name + self.sfx, [128, 2048 // isz], dtype)
        v = cm.__enter__()
        self.cms.append(cm)
        n = int(np.prod(shape[1:]))
        view = v[0:shape[0], 0:n]
        if len(shape) == 3:
            view = view.rearrange("p (a b) -> p a b", b=shape[2])
        return view

    def close(self):
        for cm in reversed(self.cms):
            cm.__exit__(None, None, None)
        self.cms = []


class Ring:
    def __init__(self, tiles, excl=False):
        self.tiles = tiles
        self.res = [Res(excl) for _ in tiles]
        self.i = 0

    def next(self):
        t, r = self.tiles[self.i], self.res[self.i]
        self.i = (self.i + 1) % len(self.tiles)
        return t, r


def _swap64(cols):
    cols = np.asarray(cols).reshape(-1, 64)
    return np.concatenate([cols[:, 32:], cols[:, :32]], axis=1).reshape(-1)


def _ext_cols():
    r = lambda a, b: np.arange(a, b)
    sbq, sbk, sbv = r(0, 512), r(512, 1024), r(1024, 1536)
    nq, kc, vc = r(1536, 2048), r(2048, 2176), r(2176, 2304)
    ks, vs, kw, vw, gl = r(2304, 2432), r(2432, 2560), r(2560, 2688), r(2688, 2816), r(2816, 2840)
    fm = [sbq, sbk]
    for c in range(4):
        fm += [nq[c * 128:(c + 1) * 128], _swap64(nq[c * 128:(c + 1) * 128])]
    fm += [ks, _swap64(ks), kw, _swap64(kw), kc, vc]
    tm = [sbv, vs, vw, gl]
    cols = np.concatenate(fm + tm)
    assert cols.shape[0] == N_EXT
    return cols


def _consts():
    half = 32
    inv = (10000.0 ** (-np.arange(half, dtype=np.float32) / half)).astype(np.float32)
    pos = np.arange(T, dtype=np.float32)
    d = np.arange(128) % 64
    ang = (pos[None, :] * inv[d % 32][:, None]).astype(np.float32)
    cos = np.cos(ang).astype(np.float32)
    sin = np.sin(ang).astype(np.float32)
    sgn = np.where(d < 32, -1.0, 1.0).astype(np.float32)[:, None]
    c = {
        "rope_cos": cos, "rope_sin": (sin * sgn).astype(np.float32),
        "ident_bf": np.eye(128).astype(ml_dtypes.bfloat16),
        "ident_f32": np.eye(128).astype(np.float32),
    }
    j = np.arange(128)
    bf = ml_dtypes.bfloat16
    c["tri_ge"] = (j[:, None] >= j[None, :]).astype(bf)
    c["tri_lt"] = (j[:, None] < j[None, :]).astype(bf)
    c["tri_le"] = (j[:, None] <= j[None, :]).astype(bf)
    c["tri_gt"] = (j[:, None] > j[None, :]).astype(bf)
    n = np.arange(256, dtype=np.float32)
    angc = ((16.0 * n + 31.0)[None, :] * inv[d % 32][:, None]).astype(np.float32)
    c["cmp_cos"] = np.cos(angc).astype(np.float32)
    c["cmp_sin"] = (np.sin(angc).astype(np.float32) * sgn).astype(np.float32)
    nn = np.arange(128)[:, None, None]
    qq = np.arange(17)[None, :, None]
    tt = np.arange(128)[None, None, :]
    BIG = 240000.0
    c["cmaskb"] = np.where(16 * nn + 31 <= 128 * qq + tt, 0.0, -BIG).astype(bf)
    c["cb_le"] = np.where(j[:, None] <= j[None, :], 0.0, -BIG).astype(bf)
    c["cb_gt"] = np.where(j[:, None] > j[None, :], 0.0, -BIG).astype(bf)
    ncmp = np.arange(256)[:, None]
    jj = np.arange(64)[None, :]
    ov = np.clip(np.minimum(16 * ncmp + 32, 64 * jj + 64) - np.maximum(16 * ncmp, 64 * jj), 0, None) / 32.0
    ov[255, :] = 0.0
    c["ovT"] = np.ascontiguousarray(ov.reshape(2, 128, 64).transpose(1, 0, 2)).astype(bf)
    sidx = np.arange(T)[None, :]
    c["eexp"] = (sidx // 64 == np.arange(64)[:, None]).astype(bf)
    ttq = np.arange(128)[:, None]
    rel = np.arange(128)[None, :] - 62
    dist = (ttq >= 64).astype(np.int64) - rel
    c["rkeep"] = (dist >= 2).astype(np.float32)
    c["radd"] = np.where(dist < 0, -1.0, np.where(dist <= 1, 1e6, 0.0)).astype(np.float32)
    return c


def build_program(dbg=None, stop_after=None, nsa_tiles=None, dbg_tile=None):
    nc = bass.Bass("TRN2", target_bir_lowering=False)
    dbg = dbg or []

    def din(name, shape, dt=F32):
        return nc.dram_tensor(name, list(shape), dt, kind="ExternalInput").ap()

    def dscr(name, shape, dt):
        kind = "ExternalOutput" if name in dbg else "Internal"
        return nc.dram_tensor(name, list(shape), dt, kind=kind).ap()

    x_in = din("x", [T, D])
    cT_in = din("cT", [128, 8])
    ln1T = din("ln1T", [L, 128, 8])
    ln2T = din("ln2T", [L, 128, 8])
    w_ada = din("w_ada", [L, D, 6 * D])
    b_adaT = din("b_adaT", [L, 128, 48])
    w_ext = din("w_ext", [L, D, N_EXT])
    rope_cos = din("rope_cos", [128, T])
    rope_sin = din("rope_sin", [128, T])
    ident_bf_d = din("ident_bf", [128, 128], BF16)
    ident_f_d = din("ident_f32", [128, 128])
    tri_d = {n: din(n, [128, 128], BF16) for n in ("tri_ge", "tri_lt", "tri_le", "tri_gt")}
    w1k_d = din("cmp_w1_k", [L, 2048, 256])
    w1v_d = din("cmp_w1_v", [L, 2048, 256])
    w2k_d = din("cmp_w2_k", [L, 256, 64])
    w2ksw_d = din("cmp_w2_k_sw", [L, 256, 64])
    w2v_d = din("cmp_w2_v", [L, 256, 64])
    posTk_d = din("posTk", [L, 64, 32])
    posTv_d = din("posTv", [L, 64, 32])
    sbg_d = din("sb_out_g", [L, 512])
    nsag_d = din("nsa_out_g", [L, 512])
    w_out_d = din("w_out", [L, D, D])
    wffn_d = din("ffn_w_in", [L, D, 2 * DFF])
    wdn_d = din("ffn_w_down", [L, DFF, D])
    cw_d = din("conv_w", [L, 128, 44, 3])
    cb_d = din("conv_b", [L, 128, 44])
    fg_d = din("final_g", [D])
    cmp_cos_d = din("cmp_cos", [128, 256])
    cmp_sin_d = din("cmp_sin", [128, 256])
    cmaskb_d = din("cmaskb", [128, 17, 128], BF16)
    cb_le_d = din("cb_le", [128, 128], BF16)
    cb_gt_d = din("cb_gt", [128, 128], BF16)
    ovT_d = din("ovT", [128, 2, 64], BF16)
    eexp_d = din("eexp", [64, T], BF16)
    rkeep_d = din("rkeep", [128, 128])
    radd_d = din("radd", [128, 128])
    out_d = nc.dram_tensor("out", [T, D], F32, kind="ExternalOutput").ap()

    o_sb_d = dscr("o_sb", [T, 512], F32)
    o_nsa_d = dscr("o_nsa", [T, 512], F32)
    xbuf_d = dscr("xbuf", [T, D], F32)
    x1_d = dscr("x1buf", [T, D], F32)
    h2T_d = dscr("h2T", [D, T], BF16)
    fmT = dscr("fmT", [2048, T], BF16)
    tmv = dscr("tmv", [T, 768], BF16)
    gates_d = dscr("gates", [T, 24], F32)
    modrow = dscr("modrow", [L, 6 * D], F32)

    P = Prog(nc)
    def dump(name, ap, res):
        if name not in dbg:
            return
        shp = [int(v) for v in ap.shape]
        dt_ = ap.dtype
        d = nc.dram_tensor(name, shp, dt_, kind="ExternalOutput").ap()
        P.dma(d, ap, reads=[res])

    ident_bf = P.sbuf("ident_bf_sb", [128, 128], BF16)
    r_ident = Res()
    P.dma(ident_bf[:], ident_bf_d[:, :], writes=[r_ident])
    ident_f = P.sbuf("ident_f_sb", [128, 128], F32)
    r_identf = Res()
    P.dma(ident_f[:], ident_f_d[:, :], writes=[r_identf])
    tri = {}
    r_tri = Res()
    for n in tri_d:
        tri[n] = P.sbuf(n + "_sb", [128, 128], BF16)
    P.dma_group([(tri[n][:], tri_d[n][:, :], None, None) for n in tri_d], writes=[r_tri])
    eps_t = P.sbuf("eps_t", [128, 1], F32)
    r_eps = Res()
    P.dve.op(lambda e: e.memset(eps_t[:], EPS), writes=[r_eps])
    cT = P.sbuf("cT_sb", [128, 8], F32)
    r_cT = Res()
    P.dma(cT[:], cT_in[:, :], writes=[r_cT])
    siluc = P.sbuf("siluc", [128, 8], F32)
    r_siluc = Res()
    P.act.op(lambda e: e.activation(out=siluc[:], in_=cT[:], func=AF.Silu), reads=[r_cT], writes=[r_siluc])
    modT = [P.sbuf(f"modT{l}", [128, 48], F32) for l in range(L)]
    r_modT = [Res() for _ in range(L)]
    A1 = [P.sbuf(f"A1_{l}", [128, 8], F32) for l in range(L)]
    A2 = [P.sbuf(f"A2_{l}", [128, 8], F32) for l in range(L)]
    r_A = [Res() for _ in range(L)]
    r_modrow = [Res() for _ in range(L)]

    def phase0(l):
        S = Scope(P)
        wbuf = Ring([S.sbuf(f"wada{i}", [128, 6 * D], F32) for i in range(2)])
        pm = S.psum("pmod", [128, 48], F32)
        r_pm = Res(True)
        lnt = S.sbuf("lnt", [128, 16], F32)
        r_lnt = Res()
        bT = S.sbuf("bT", [128, 48], F32)
        r_bT = Res()
        P.dma(lnt[:, 0:8], ln1T[l], writes=[r_lnt])
        P.dma(lnt[:, 8:16], ln2T[l], writes=[r_lnt])
        P.dma(bT[:], b_adaT[l], writes=[r_bT])
        for k in range(8):
            wt, rw = wbuf.next()
            P.dma_group([(wt[:, hf * 3072:(hf + 1) * 3072],
                          w_ada[l, k * 128:(k + 1) * 128, hf * 3072:(hf + 1) * 3072], None, None)
                         for hf in range(2)], writes=[rw])
            for m in range(48):
                P.pe.op(lambda e: e.matmul(pm[:, m:m + 1], lhsT=wt[:, m * 128:(m + 1) * 128], rhs=siluc[:, k:k + 1],
                                           start=(k == 0 and m == 0), stop=(k == 7 and m == 47),
                                           skip_group_check=True),
                        reads=[rw, r_siluc], writes=[r_pm])
        P.dve.op(lambda e: e.tensor_tensor(out=modT[l][:], in0=pm[:], in1=bT[:], op=ALU.add),
                 reads=[r_pm, r_bT], writes=[r_modT[l]])
        P.dve.op(lambda e: e.scalar_tensor_tensor(out=A1[l][:], in0=modT[l][:, 8:16], scalar=1.0, in1=lnt[:, 0:8],
                                                  op0=ALU.add, op1=ALU.mult),
                 reads=[r_modT[l], r_lnt], writes=[r_A[l]])
        P.dve.op(lambda e: e.scalar_tensor_tensor(out=A2[l][:], in0=modT[l][:, 32:40], scalar=1.0, in1=lnt[:, 8:16],
                                                  op0=ALU.add, op1=ALU.mult),
                 reads=[r_modT[l], r_lnt], writes=[r_A[l]])
        pmt = S.psum("pmt", [48, 128], F32)
        r_pmt = Res(True)
        P.pe.op(lambda e: e.transpose(out=pmt[:], in_=modT[l][:], identity=ident_f[:]),
                reads=[r_modT[l], r_identf], writes=[r_pmt])
        mrow = S.sbuf("mrow", [48, 128], F32)
        r_mrow = Res()
        P.dve.op(lambda e: e.tensor_copy(out=mrow[:], in_=pmt[:]), reads=[r_pmt], writes=[r_mrow])
        P.dma(modrow[l].rearrange("(m p) -> m p", p=128), mrow[:], reads=[r_mrow])
        S.close()

    def phase1(l, x_src):
        S = Scope(P)
        wsb = S.sbuf("w1sb", [128, 8, N_EXT], BF16)
        blocks = [(N_FM, N_EXT), (0, 1024), (1024, 2048), (2048, N_FM)]
        r_wb = [Res() for _ in blocks]
        for (c0, c1), rw in zip(blocks, r_wb):
            P.dma_group([(wsb[:, k, c0:c1], w_ext[l, k * 128:(k + 1) * 128, c0:c1], P.pool, None) for k in range(8)],
                        writes=[rw])

        def r_wcols(c0):
            for (b0, b1), rw in zip(blocks, r_wb):
                if b0 <= c0 < b1:
                    return rw
            raise AssertionError

        cosb = S.sbuf("cosb", [128, T], F32)
        sinb = S.sbuf("sinb", [128, T], F32)
        r_rope = Res()
        P.dma(cosb[:], rope_cos[:, :], writes=[r_rope])
        r_rope2 = Res()
        P.dma(sinb[:], rope_sin[:, :], writes=[r_rope2])
        xt = Ring([S.sbuf(f"xt{i}", [128, D], F32) for i in range(3)])
        junk = S.sbuf("junk", [128, D], F32)
        r_junk = Res()
        stat = Ring([S.sbuf(f"stat{i}", [128, 4], F32) for i in range(4)])
        xn = Ring([S.sbuf(f"xn{i}", [128, D], BF16) for i in range(3)])
        hT = Ring([S.sbuf(f"hT{i}", [128, 8, 512], BF16) for i in range(2)])
        stg = Ring([S.sbuf(f"stg{i}", [128, 512], BF16) for i in range(6)])
        stgtm = Ring([S.sbuf(f"stgtm{i}", [128, 768], BF16) for i in range(2)])
        gst = Ring([S.sbuf(f"gst{i}", [128, 24], F32) for i in range(2)])
        rt1 = Ring([S.sbuf(f"rt1_{i}", [128, 512], F32) for i in range(2)])
        rt2 = Ring([S.sbuf(f"rt2_{i}", [128, 512], F32) for i in range(2)])
        pT_r = Ring([S.psum(f"pT{i}", [128, 8, 128], BF16) for i in range(2)], excl=True)
        ptm_r = Ring([S.psum(f"ptm{i}", [128, 512], F32) for i in range(2)], excl=True)
        pfm = Ring([S.psum(f"pfm{i}", [128, 512], F32) for i in range(4)], excl=True)
        B1 = modT[l][:, 0:8]
        xl = {}
        hcur = {}

        def ldx(ti):
            xtile, r_x = xt.next()
            P.dma(xtile[:], x_src[ti * 128:(ti + 1) * 128, :], writes=[r_x])
            xl[ti] = (xtile, r_x)

        def g1(ti, d):
            xtile, r_x = xl.pop(ti)
            if ti + 1 < NT:
                ldx(ti + 1)
            st, r_st = stat.next()
            P.act.op(lambda e: e.activation(out=junk[:], in_=xtile[:], func=AF.Square, accum_out=st[:, 0:1]),
                     reads=[r_x], writes=[r_junk, r_st])
            rstd_ops(st, r_st, 0, 1, D)
            d.update(xtile=xtile, r_x=r_x, st=st, r_st=r_st)

        def g2(ti, d):
            xnt, r_xn = xn.next()
            P.dve.op(lambda e: e.tensor_scalar(out=xnt[:], in0=d["xtile"][:], scalar1=d["st"][:, 2:3], scalar2=None,
                                               op0=ALU.mult), reads=[d["r_x"], d["r_st"]], writes=[r_xn])
            d.update(xnt=xnt, r_xn=r_xn)

        def g3(ti, d):
            xnt, r_xn = d["xnt"], d["r_xn"]
            pT, r_pT = pT_r.next()
            for k in range(8):
                P.pe.op(lambda e: e.transpose(out=pT[:, k, :], in_=xnt[:, k * 128:(k + 1) * 128], identity=ident_bf[:]),
                        reads=[r_xn, r_ident], writes=[r_pT])
            d.update(pT=pT, r_pT=r_pT)

        def g4(ti, d):
            grp, tt = divmod(ti, 4)
            if tt == 0:
                hcur[grp] = hT.next()
            h, r_h = hcur[grp]
            pT, r_pT = d["pT"], d["r_pT"]
            for k in range(8):
                dst = h[:, k, tt * 128:(tt + 1) * 128]
                if ti % 2 == 0:
                    P.act.op(lambda e: e.activation(out=dst, in_=pT[:, k, :], func=AF.Identity,
                                                    scale=A1[l][:, k:k + 1], bias=B1[:, k:k + 1]),
                             reads=[r_pT, r_A[l], r_modT[l]], writes=[r_h])
                else:
                    P.dve.op(lambda e: e.tensor_scalar(out=dst, in0=pT[:, k, :], scalar1=A1[l][:, k:k + 1],
                                                       scalar2=B1[:, k:k + 1], op0=ALU.mult, op1=ALU.add),
                             reads=[r_pT, r_A[l], r_modT[l]], writes=[r_h])

        def g5(ti, d):
            grp, tt = divmod(ti, 4)
            h, r_h = hcur[grp]
            p0, r_p0 = ptm_r.next()
            p1, r_p1 = ptm_r.next()
            for k in range(8):
                lhsT = h[:, k, tt * 128:(tt + 1) * 128]
                P.pe.op(lambda e: e.matmul(p0[:], lhsT=lhsT, rhs=wsb[:, k, N_FM:N_FM + 512],
                                           start=(k == 0), stop=(k == 7)), reads=[r_h, r_wb[0]], writes=[r_p0])
            for k in range(8):
                lhsT = h[:, k, tt * 128:(tt + 1) * 128]
                P.pe.op(lambda e: e.matmul(p1[:, 0:280], lhsT=lhsT, rhs=wsb[:, k, N_FM + 512:N_EXT],
                                           start=(k == 0), stop=(k == 7)), reads=[r_h, r_wb[0]], writes=[r_p1])
            d.update(p0=p0, r_p0=r_p0, p1=p1, r_p1=r_p1)

        def g6(ti, d):
            p0, r_p0, p1, r_p1 = d["p0"], d["r_p0"], d["p1"], d["r_p1"]
            sg, r_sg = stgtm.next()
            P.act.op(lambda e: e.activation(out=sg[:, 0:512], in_=p0[:], func=AF.Copy), reads=[r_p0], writes=[r_sg])
            P.dve.op(lambda e: e.tensor_copy(out=sg[:, 512:768], in_=p1[:, 0:256]), reads=[r_p1], writes=[r_sg])
            gs, r_gs = gst.next()
            P.act.op(lambda e: e.activation(out=gs[:], in_=p1[:, 256:280], func=AF.Sigmoid), reads=[r_p1], writes=[r_gs])
            P.dma(tmv[ti * 128:(ti + 1) * 128, :], sg[:], reads=[r_sg])
            P.dma(gates_d[ti * 128:(ti + 1) * 128, :], gs[:], reads=[r_gs])

        evc = [0]

        def fm_group(grp):
            h, r_h = hcur[grp]
            tsl = slice(grp * 512, (grp + 1) * 512)

            def fm_mm(ch):
                pf, r_pf = pfm.next()
                rw = r_wcols(ch * 128)
                for k in range(8):
                    P.pe.op(lambda e: e.matmul(pf[:], lhsT=wsb[:, k, ch * 128:(ch + 1) * 128], rhs=h[:, k, :],
                                               start=(k == 0), stop=(k == 7)), reads=[r_h, rw], writes=[r_pf])
                return pf, r_pf

            for ch in range(8):
                pf, r_pf = fm_mm(ch)
                s_, r_s = stg.next()
                if evc[0] % 2 == 0:
                    P.act.op(lambda e: e.activation(out=s_[:], in_=pf[:], func=AF.Copy), reads=[r_pf], writes=[r_s])
                else:
                    P.dve.op(lambda e: e.tensor_copy(out=s_[:], in_=pf[:]), reads=[r_pf], writes=[r_s])
                evc[0] += 1
                P.dma(fmT[ch * 128:(ch + 1) * 128, tsl], s_[:], reads=[r_s])
            for pr in range(6):
                pa, r_pa = fm_mm(8 + 2 * pr)
                pb, r_pb = fm_mm(9 + 2 * pr)
                t1, r_t1 = rt1.next()
                t2, r_t2 = rt2.next()
                P.dve.op(lambda e: e.tensor_tensor(out=t1[:], in0=pa[:], in1=cosb[:, tsl], op=ALU.mult),
                         reads=[r_pa, r_rope], writes=[r_t1])
                P.dve.op(lambda e: e.tensor_tensor(out=t2[:], in0=pb[:], in1=sinb[:, tsl], op=ALU.mult),
                         reads=[r_pb, r_rope2], writes=[r_t2])
                s_, r_s = stg.next()
                P.pool.op(lambda e: e.tensor_tensor(out=s_[:], in0=t1[:], in1=t2[:], op=ALU.add),
                          reads=[r_t1, r_t2], writes=[r_s])
                P.dma(fmT[1024 + pr * 128:1024 + (pr + 1) * 128, tsl], s_[:], reads=[r_s])
            for j in range(2):
                pf, r_pf = fm_mm(20 + j)
                s_, r_s = stg.next()
                P.act.op(lambda e: e.activation(out=s_[:], in_=pf[:], func=AF.Copy), reads=[r_pf], writes=[r_s])
                P.dma(fmT[1792 + j * 128:1792 + (j + 1) * 128, tsl], s_[:], reads=[r_s])

        ldx(0)
        stages = [g1, g2, g3, g4, g5, g6]
        sts = {}
        for it in range(NT + len(stages) - 1):
            fm_after = None
            for si, fn in reversed(list(enumerate(stages))):
                ti = it - si
                if 0 <= ti < NT:
                    if si == 0:
                        sts[ti] = {}
                    fn(ti, sts[ti])
                    if si == 3 and ti % 4 == 3:
                        fm_after = ti // 4
                    if si == len(stages) - 1:
                        sts.pop(ti)
            if fm_after is not None:
                fm_group(fm_after)
        S.close()

    def phase3(l):
        S = Scope(P)
        qT = Ring([S.sbuf(f"sbq{i}", [64, T], BF16) for i in range(2)])
        kT = Ring([S.sbuf(f"sbk{i}", [64, T], BF16) for i in range(2)])
        vt = Ring([S.sbuf(f"sbv{i}", [128, NT, 64], BF16) for i in range(2)])
        e_r = Ring([S.sbuf(f"e{i}", [128, 512], F32) for i in range(3)])
        sp_r = Ring([S.sbuf(f"sp{i}", [128, 512], BF16) for i in range(3)])
        w_r = Ring([S.sbuf(f"w{i}", [128, 512], BF16) for i in range(3)])
        o_r = Ring([S.sbuf(f"osb{i}", [128, 4, 64], F32) for i in range(2)])
        pS = Ring([S.psum(f"pS{i}", [128, 512], F32) for i in range(2)], excl=True)
        p2 = Ring([S.psum(f"p2{i}", [128, 512], F32) for i in range(2)], excl=True)
        pD = Ring([S.psum(f"pD{i}", [128, 512], F32) for i in range(2)], excl=True)
        pacc = Ring([S.psum(f"pacc{i}", [128, 512], F32) for i in range(2)], excl=True)
        oT_r = Ring([S.sbuf(f"oT{i}", [64, 512], F32) for i in range(2)])
        heads = {}

        def ldh(h):
            q, rq = qT.next()
            k, rk = kT.next()
            v, rv = vt.next()
            P.dma(q[:], fmT[h * 64:(h + 1) * 64, :], writes=[rq])
            P.dma(k[:], fmT[512 + h * 64:512 + (h + 1) * 64, :], writes=[rk])
            P.dma(v[:], tmv[:, h * 64:(h + 1) * 64].rearrange("(kb p) d -> p kb d", p=128), writes=[rv])
            heads[h] = (q, rq, k, rk, v, rv)

        units = []
        for h in range(8):
            for c in range(8):
                for kb in range(4 * c + 3, -1, -1):
                    units.append(dict(h=h, c=c, kb=kb, first=(kb == 4 * c + 3), last=(kb == 0)))

        def stage1(u):
            h, c, kb = u["h"], u["c"], u["kb"]
            if u["first"] and c == 0:
                if h == 0:
                    ldh(0)
                if h + 1 < 8:
                    ldh(h + 1)
            q, rq, k, rk, v, rv = heads[h]
            i = kb - 4 * c
            q0 = 128 * i if i > 0 else 0
            u["q0"] = q0
            ps, r_ps = pS.next()
            P.pe.op(lambda e: e.matmul(ps[:, q0:512], lhsT=k[:, kb * 128:(kb + 1) * 128],
                                       rhs=q[:, c * 512 + q0:(c + 1) * 512], start=True, stop=True),
                    reads=[rq, rk], writes=[r_ps])
            et, r_e = e_r.next()
            P.act.op(lambda e: e.activation(out=et[:, q0:512], in_=ps[:, q0:512], func=AF.Exp, scale=0.125),
                     reads=[r_ps], writes=[r_e])
            if i >= 0:
                P.dve.op(lambda e: e.tensor_tensor(out=et[:, q0:q0 + 128], in0=et[:, q0:q0 + 128],
                                                   in1=tri["tri_lt"][:], op=ALU.mult),
                         reads=[r_e, r_tri], writes=[r_e])
            spt, r_sp = sp_r.next()
            P.act.op(lambda e: e.activation(out=spt[:, q0:512], in_=et[:, q0:512], func=AF.Ln, bias=1.0, scale=1.0),
                     reads=[r_e], writes=[r_sp])
            u.update(et=et, r_e=r_e, spt=spt, r_sp=r_sp)

        chain = {}

        def stage2(u):
            q0 = u["q0"]
            if u["first"]:
                chain["p2"], chain["r_p2"] = p2.next()
                chain["prev"] = None
            pp, r_pp = chain["p2"], chain["r_p2"]
            prev = chain["prev"]
            if prev is not None:
                pq0 = prev["q0"]
                P.pe.op(lambda e: e.matmul(pp[:, pq0:512], lhsT=tri["tri_lt"][:], rhs=prev["spt"][:, pq0:512],
                                           start=False, stop=False, skip_group_check=True),
                        reads=[prev["r_sp"], r_tri], writes=[r_pp])
            P.pe.op(lambda e: e.matmul(pp[:, q0:512], lhsT=tri["tri_ge"][:], rhs=u["spt"][:, q0:512],
                                       start=(prev is None), stop=True, skip_group_check=True),
                    reads=[u["r_sp"], r_tri], writes=[r_pp])
            chain["prev"] = u
            pd, r_pd = pD.next()
            P.act.op(lambda e: e.activation(out=pd[:, q0:512], in_=pp[:, q0:512], func=AF.Exp, scale=-1.0),
                     reads=[r_pp], writes=[r_pd])
            wt, r_w = w_r.next()
            P.dve.op(lambda e: e.tensor_tensor(out=wt[:, q0:512], in0=u["et"][:, q0:512], in1=pd[:, q0:512],
                                               op=ALU.mult),
                     reads=[u["r_e"], r_pd], writes=[r_w])
            u.update(wt=wt, r_w=r_w)

        accs = {}

        def stage3(u):
            h, c, kb, q0 = u["h"], u["c"], u["kb"], u["q0"]
            q, rq, k, rk, v, rv = heads[h]
            if u["first"]:
                accs["a"], accs["r"] = pacc.next()
            acc, r_acc = accs["a"], accs["r"]
            P.pe.op(lambda e: e.matmul(acc[0:64, q0:512], lhsT=v[:, kb, :], rhs=u["wt"][:, q0:512],
                                       start=u["first"], stop=u["last"], skip_group_check=True),
                    reads=[u["r_w"], rv], writes=[r_acc])
            if u["last"]:
                oT, r_oT = oT_r.next()
                P.dve.op(lambda e: e.tensor_copy(out=oT[:], in_=acc[0:64, :]), reads=[r_acc], writes=[r_oT])
                ptr, r_ptr = pS.next()
                for ts in range(4):
                    P.pe.op(lambda e: e.transpose(out=ptr[:, ts * 64:(ts + 1) * 64], in_=oT[:, ts * 128:(ts + 1) * 128],
                                                  identity=ident_f[0:64, 0:64]),
                            reads=[r_oT, r_identf], writes=[r_ptr])
                ot, r_o = o_r.next()
                P.dve.op(lambda e: e.tensor_copy(out=ot[:].rearrange("p a b -> p (a b)"), in_=ptr[:, 0:256]),
                         reads=[r_ptr], writes=[r_o])
                P.dma(o_sb_d[c * 512:(c + 1) * 512, h * 64:(h + 1) * 64].rearrange("(s p) d -> p s d", p=128),
                      ot[:], reads=[r_o])

        n = len(units)
        for it in range(n + 2):
            if it < n:
                stage1(units[it])
            if 0 <= it - 1 < n:
                stage2(units[it - 1])
            if 0 <= it - 2 < n:
                stage3(units[it - 2])
        S.close()


    def phase4(l):
        S = Scope(P)
        qs = [S.sbuf(f"qs{g}", [128, 4, T], BF16) for g in range(2)]
        kse = [S.sbuf(f"kse{g}", [128, T], BF16) for g in range(2)]
        kw2 = S.sbuf("kw2", [64, 2, T], BF16)
        vs1 = S.sbuf("vs1", [128, 2, NT, 65], BF16)
        vw1 = S.sbuf("vw1", [128, 2, NT, 65], BF16)
        kcr = S.sbuf("kcr", [64, 2, 256], BF16)
        vov = S.sbuf("vov", [128, 2, 2, 65], BF16)
        ovT = S.sbuf("ovT_sb", [128, 2, 64], BF16)
        cmaskb = S.sbuf("cmaskb_sb", [128, 17, 128], BF16)
        cb_le = S.sbuf("cb_le_sb", [128, 128], BF16)
        cb_gt = S.sbuf("cb_gt_sb", [128, 128], BF16)
        rkeep = S.sbuf("rkeep_sb", [128, 128], F32)
        radd = S.sbuf("radd_sb", [128, 128], F32)
        r_q = [Res(), Res()]
        r_kse = [Res(), Res()]
        r_kw, r_vs, r_vw, r_kcr, r_vov, r_c4 = (Res() for _ in range(6))
        for g in range(2):
            P.dma(qs[g][0:64, :, :], fmT[1024 + 256 * g:1024 + 256 * (g + 1), :].rearrange("(hh d) t -> d hh t", d=64),
                  writes=[r_q[g]])
            P.dma_group([(kse[g][0:64, :], fmT[1536 + 64 * g:1536 + 64 * (g + 1), :], None, None),
                         (kse[g][64:128, :], eexp_d[:, :], None, None)], writes=[r_kse[g]])
        P.dma_group([(kw2[:, g, :], fmT[1664 + 64 * g:1664 + 64 * (g + 1), :], None, None) for g in range(2)], writes=[r_kw])
        P.pool.op(lambda e: e.memset(vs1[:], 1.0), writes=[r_vs])
        P.pool.op(lambda e: e.memset(vw1[:], 1.0), writes=[r_vw])
        P.pool.op(lambda e: e.memset(vov[:], 1.0), writes=[r_vov])
        P.dma_group([(vs1[:, g, :, 0:64],
                      tmv[:, 512 + 64 * g:512 + 64 * (g + 1)].rearrange("(kb p) d -> p kb d", p=128), None, None)
                     for g in range(2)], writes=[r_vs])
        P.dma_group([(vw1[:, g, :, 0:64],
                      tmv[:, 640 + 64 * g:640 + 64 * (g + 1)].rearrange("(kb p) d -> p kb d", p=128), None, None)
                     for g in range(2)], writes=[r_vw])
        P.dma_group([(ovT[:], ovT_d[:, :, :], None, None), (cmaskb[:], cmaskb_d[:, :, :], None, None),
                     (cb_le[:], cb_le_d[:, :], None, None), (cb_gt[:], cb_gt_d[:, :], None, None),
                     (rkeep[:], rkeep_d[:, :], None, None), (radd[:], radd_d[:, :], None, None)], writes=[r_c4])

        S2 = Scope(P)
        kcT = S2.sbuf("kcT", [128, T], BF16)
        vcT = S2.sbuf("vcT", [128, T], BF16)
        w1 = {"k": S2.sbuf("w1k", [128, 32, 256], BF16), "v": S2.sbuf("w1v", [128, 32, 256], BF16)}
        w2k = S2.sbuf("w2k", [128, 2, 64], BF16)
        w2ks = S2.sbuf("w2ks", [128, 2, 64], BF16)
        w2v = S2.sbuf("w2v", [128, 2, 64], BF16)
        posT = {"k": S2.sbuf("posTk_sb", [64, 32], BF16), "v": S2.sbuf("posTv_sb", [64, 32], BF16)}
        ccos = S2.sbuf("ccos", [128, 256], F32)
        csin = S2.sbuf("csin", [128, 256], F32)
        bias_sb = S2.sbuf("cbias", [128, 4], F32)
        r_x2, r_w1, r_w2, r_pos, r_cc, r_bias = (Res() for _ in range(6))
        P.dma_group([(kcT[:], fmT[1792:1920, :], None, None), (vcT[:], fmT[1920:2048, :], None, None)], writes=[r_x2])
        srcs = {"k": w1k_d, "v": w1v_d}
        P.dma_group([(w1[kd][64 * hf:64 * hf + 64, :, :], srcs[kd][l].rearrange("(l d) h -> d l h", d=64), P.pool, None)
                     for kd in ("k", "v") for hf in range(2)], writes=[r_w1])
        P.dma_group([(w2k[:], w2k_d[l].rearrange("(hc p) d -> p hc d", p=128), P.pool, None),
                     (w2ks[:], w2ksw_d[l].rearrange("(hc p) d -> p hc d", p=128), P.pool, None),
                     (w2v[:], w2v_d[l].rearrange("(hc p) d -> p hc d", p=128), P.pool, None)], writes=[r_w2])
        P.dma_group([(posT["k"][:], posTk_d[l], P.pool, None), (posT["v"][:], posTv_d[l], P.pool, None)], writes=[r_pos])
        P.dma_group([(ccos[:], cmp_cos_d[:, :], None, None), (csin[:], cmp_sin_d[:, :], None, None)], writes=[r_cc])
        pb = S2.psum("pb", [128, 4], F32)
        r_pb = Res(True)
        first = True
        for ki, kd in enumerate(("k", "v")):
            for hc in range(2):
                for ll in range(32):
                    P.pe.op(lambda e: e.matmul(pb[:, 2 * ki + hc:2 * ki + hc + 1],
                                               lhsT=w1[kd][0:64, ll, hc * 128:(hc + 1) * 128],
                                               rhs=posT[kd][:, ll:ll + 1], start=first, stop=False,
                                               skip_group_check=True),
                            reads=[r_w1, r_pos], writes=[r_pb])
                    first = False
        P.dve.op(lambda e: e.tensor_copy(out=bias_sb[:], in_=pb[:]), reads=[r_pb], writes=[r_bias])
        ph = Ring([S2.psum(f"ph{i}", [128, 256], F32) for i in range(2)], excl=True)
        pk = Ring([S2.psum(f"pk{i}", [128, 256], F32) for i in range(2)], excl=True)
        u_r = Ring([S2.sbuf(f"cu{i}", [128, 256], F32) for i in range(2)])
        t_r = Ring([S2.sbuf(f"ct{i}", [128, 256], F32) for i in range(2)])
        g_r = [S2.sbuf(f"cg{i}", [128, 256], BF16) for i in range(4)]
        r_g = [Res() for _ in range(4)]
        for i in range(4):
            P.pool.op(lambda e: e.memset(g_r[i][:], 0.0), writes=[r_g[i]])
        gi = 0
        xsrc = {"k": kcT, "v": vcT}
        for ki, kd in enumerate(("k", "v")):
            for g in range(2):
                gts = []
                for hc in range(2):
                    pht, r_ph = ph.next()
                    for ll in range(32):
                        P.pe.op(lambda e: e.matmul(pht[:, 0:255], lhsT=w1[kd][64 * g:64 * g + 64, ll, hc * 128:(hc + 1) * 128],
                                                   rhs=xsrc[kd][64 * g:64 * g + 64, ll:ll + 16 * 254 + 1:16],
                                                   start=(ll == 0), stop=(ll == 31)),
                                reads=[r_w1, r_x2], writes=[r_ph])
                    ut, r_u = u_r.next()
                    P.act.op(lambda e: e.activation(out=ut[:, 0:255], in_=pht[:, 0:255], func=AF.Identity,
                                                    bias=bias_sb[:, 2 * ki + hc:2 * ki + hc + 1], scale=1.0),
                             reads=[r_ph, r_bias], writes=[r_u])
                    tt_, r_t = t_r.next()
                    P.dve.op(lambda e: e.tensor_tensor(out=tt_[:, 0:255], in0=ut[:, 0:255], in1=ut[:, 0:255], op=ALU.mult),
                             reads=[r_u], writes=[r_t])
                    P.dve.op(lambda e: e.tensor_scalar(out=tt_[:, 0:255], in0=tt_[:, 0:255], scalar1=0.044715, scalar2=1.0,
                                                       op0=ALU.mult, op1=ALU.add), reads=[r_t], writes=[r_t])
                    P.dve.op(lambda e: e.tensor_tensor(out=tt_[:, 0:255], in0=tt_[:, 0:255], in1=ut[:, 0:255], op=ALU.mult),
                             reads=[r_t, r_u], writes=[r_t])
                    P.act.op(lambda e: e.activation(out=tt_[:, 0:255], in_=tt_[:, 0:255], func=AF.Sigmoid,
                                                    scale=1.5957691216057308), reads=[r_t], writes=[r_t])
                    gt_, r_gt = g_r[gi % 4], r_g[gi % 4]
                    gi += 1
                    P.dve.op(lambda e: e.tensor_tensor(out=gt_[:, 0:255], in0=ut[:, 0:255], in1=tt_[:, 0:255], op=ALU.mult),
                             reads=[r_t, r_u], writes=[r_gt])
                    gts.append((gt_, r_gt))
                if kd == "k":
                    pa, r_pa = pk.next()
                    pb2, r_pb2 = pk.next()
                    for hc in range(2):
                        P.pe.op(lambda e: e.matmul(pa[0:64, :], lhsT=w2k[:, hc, :], rhs=gts[hc][0][:], start=(hc == 0), stop=(hc == 1)),
                                reads=[r_w2, gts[hc][1]], writes=[r_pa])
                    for hc in range(2):
                        P.pe.op(lambda e: e.matmul(pb2[0:64, :], lhsT=w2ks[:, hc, :], rhs=gts[hc][0][:], start=(hc == 0), stop=(hc == 1)),
                                reads=[r_w2, gts[hc][1]], writes=[r_pb2])
                    t1, r_t1 = u_r.next()
                    t2, r_t2 = t_r.next()
                    P.dve.op(lambda e: e.tensor_tensor(out=t1[0:64, :], in0=pa[0:64, :], in1=ccos[0:64, :], op=ALU.mult),
                             reads=[r_pa, r_cc], writes=[r_t1])
                    P.dve.op(lambda e: e.tensor_tensor(out=t2[0:64, :], in0=pb2[0:64, :], in1=csin[0:64, :], op=ALU.mult),
                             reads=[r_pb2, r_cc], writes=[r_t2])
                    P.dve.op(lambda e: e.tensor_tensor(out=kcr[:, g, :], in0=t1[0:64, :], in1=t2[0:64, :], op=ALU.add),
                             reads=[r_t1, r_t2], writes=[r_kcr])
                else:
                    for nn in range(2):
                        pv, r_pv = pk.next()
                        for hc in range(2):
                            P.pe.op(lambda e: e.matmul(pv[:, 0:64], lhsT=gts[hc][0][:, nn * 128:(nn + 1) * 128],
                                                       rhs=w2v[:, hc, :], start=(hc == 0), stop=(hc == 1)),
                                    reads=[r_w2, gts[hc][1]], writes=[r_pv])
                        P.dve.op(lambda e: e.tensor_copy(out=vov[:, g, nn, 0:64], in_=pv[:, 0:64]),
                                 reads=[r_pv], writes=[r_vov])
        S2.close()
        P.barrier()

        pSr = Ring([S.psum(f"pSn{i}", [128, 4, 128], F32) for i in range(3)], excl=True)
        pOc = S.psum("pOc", [128, 4, 65], F32)
        pImp = S.psum("pImp", [128, 4, 64], F32)
        pOs = S.psum("pOs", [128, 4, 65], F32)
        pOw = S.psum("pOw", [128, 4, 65], F32)
        pMTb = S.psum("pMT", [128, 1024], BF16)
        r_pOc, r_pImp, r_pOs, r_pOw, r_pT4 = (Res(True) for _ in range(5))
        ex_r = Ring([S.sbuf(f"ex{i}", [128, 4, 128], BF16) for i in range(4)])
        sm_r = Ring([S.sbuf(f"sm{i}", [128, 32], F32) for i in range(2)])
        oc_r = Ring([S.sbuf(f"oc{i}", [128, 4, 64], F32) for i in range(2)])
        imp_r = Ring([S.sbuf(f"imp{i}", [128, 64], F32) for i in range(2)])
        sc_r = Ring([S.sbuf(f"sc{i}", [128, 64], F32) for i in range(2)])
        sc2_r = Ring([S.sbuf(f"sc2{i}", [128, 64], F32) for i in range(2)])
        m8_r = Ring([S.sbuf(f"m8{i}", [128, 16], F32) for i in range(2)])
        sel_r = Ring([S.sbuf(f"sel{i}", [128, 128], BF16) for i in range(2)])
        for i in range(2):
            P.pool.op(lambda e: e.memset(sel_r.tiles[i][:], 0.0), writes=[sel_r.res[i]])
        gt_r = Ring([S.sbuf(f"gt{i}", [128, 12], F32) for i in range(2)])
        cf_r = Ring([S.sbuf(f"cf{i}", [128, 16], F32) for i in range(2)])
        ta_r = Ring([S.sbuf(f"ta{i}", [128, 4, 64], F32) for i in range(2)])
        tb_r = Ring([S.sbuf(f"tb{i}", [128, 4, 64], F32) for i in range(2)])
        on_r = Ring([S.sbuf(f"on{i}", [128, 4, 64], F32) for i in range(2)])
        BIG = 240000.0

        def bc(ap, shape):
            return ap.to_broadcast(shape)

        def prologue(g, qt):
            st = {}
            qsl = qs[g][0:64, :, qt * 128:(qt + 1) * 128]
            gt_, r_gt = gt_r.next()
            P.dma(gt_[:], gates_d[qt * 128:(qt + 1) * 128, 12 * g:12 * (g + 1)], writes=[r_gt])
            st["gt"], st["r_gt"] = gt_, r_gt
            nns = [0] if qt < 16 else [0, 1]
            exs = []
            for nn in nns:
                ps, r_ps = pSr.next()
                qp = qt - 16 * nn
                msk = qp <= 16
                P.pe.op(lambda e: e.matmul(ps[:], lhsT=kcr[:, g, nn * 128:(nn + 1) * 128], rhs=qsl, start=True, stop=not msk),
                        reads=[r_kcr, r_q[g]], writes=[r_ps])
                if msk:
                    P.pe.op(lambda e: e.matmul(ps[:], lhsT=ident_bf[:], rhs=bc(cmaskb[:, qp:qp + 1, :], [128, 4, 128]),
                                               start=False, stop=True), reads=[r_ident, r_c4], writes=[r_ps])
                ex, r_ex = ex_r.next()
                P.act.op(lambda e: e.activation(out=ex[:], in_=ps[:], func=AF.Exp, scale=0.125), reads=[r_ps], writes=[r_ex])
                exs.append((nn, ex, r_ex))
            firstc = True
            for (nn, ex, r_ex) in exs:
                for hh in range(4):
                    P.pe.op(lambda e: e.matmul(pOc[:, hh, :], lhsT=ex[:, hh, :], rhs=vov[:, g, nn, :],
                                               start=firstc, stop=False, skip_group_check=True),
                            reads=[r_ex, r_vov], writes=[r_pOc])
                    firstc = False
            firstc = True
            for (nn, ex, r_ex) in exs:
                for hh in range(4):
                    P.pe.op(lambda e: e.matmul(pImp[:, hh, :], lhsT=ex[:, hh, :], rhs=ovT[:, nn, :],
                                               start=firstc, stop=False, skip_group_check=True),
                            reads=[r_ex, r_c4], writes=[r_pImp])
                    firstc = False
            sm, r_sm = sm_r.next()
            P.dve.op(lambda e: e.tensor_scalar(out=sm[:, 0:4], in0=pOc[:, :, 64], scalar1=1e-6, scalar2=None, op0=ALU.max),
                     reads=[r_pOc], writes=[r_sm])
            P.dve.op(lambda e: e.reciprocal(out=sm[:, 4:8], in_=sm[:, 0:4]), reads=[r_sm], writes=[r_sm])
            oc, r_oc = oc_r.next()
            P.dve.op(lambda e: e.tensor_tensor(out=oc[:], in0=pOc[:, :, 0:64], in1=bc(sm[:, 4:8].unsqueeze(2), [128, 4, 64]),
                                               op=ALU.mult), reads=[r_pOc, r_sm], writes=[r_oc])
            st["oc"], st["r_oc"] = oc, r_oc
            imp, r_imp = imp_r.next()
            P.dve.op(lambda e: e.tensor_scalar(out=imp[:], in0=pImp[:, 0, :], scalar1=sm[:, 4:5], scalar2=None, op0=ALU.mult),
                     reads=[r_pImp, r_sm], writes=[r_imp])
            for hh in range(1, 4):
                P.dve.op(lambda e: e.scalar_tensor_tensor(out=imp[:], in0=pImp[:, hh, :], scalar=sm[:, 4 + hh:5 + hh],
                                                          in1=imp[:], op0=ALU.mult, op1=ALU.add),
                         reads=[r_pImp, r_sm, r_imp], writes=[r_imp])
            sc, r_sc = sc_r.next()
            o0 = 62 - 2 * qt
            P.dve.op(lambda e: e.tensor_tensor(out=sc[:], in0=imp[:], in1=rkeep[:, o0:o0 + 64], op=ALU.mult),
                     reads=[r_imp, r_c4], writes=[r_sc])
            P.dve.op(lambda e: e.tensor_tensor(out=sc[:], in0=sc[:], in1=radd[:, o0:o0 + 64], op=ALU.add),
                     reads=[r_sc, r_c4], writes=[r_sc])
            P.dve.op(lambda e: e.memset(sc[:, 0:1], 1e6), reads=[r_sc], writes=[r_sc])
            m8, r_m8 = m8_r.next()
            sc2, r_sc2 = sc2_r.next()
            P.dve.op(lambda e: e.max(out=m8[:, 0:8], in_=sc[:]), reads=[r_sc], writes=[r_m8])
            P.dve.op(lambda e: e.match_replace(out=sc2[:], in_to_replace=m8[:, 0:8], in_values=sc[:], imm_value=-1e30),
                     reads=[r_sc, r_m8], writes=[r_sc2])
            P.dve.op(lambda e: e.max(out=m8[:, 8:16], in_=sc2[:]), reads=[r_sc2, r_m8], writes=[r_m8])
            sel, r_sel = sel_r.next()
            P.dve.op(lambda e: e.tensor_scalar(out=sel[:, 64:128], in0=sc[:], scalar1=m8[:, 15:16], scalar2=-BIG,
                                               op0=ALU.is_lt, op1=ALU.mult),
                     reads=[r_sc, r_m8], writes=[r_sel])
            st["sel"], st["r_sel"] = sel, r_sel
            st["r_sb"] = Res()
            return st

        def prologue_b(g, qt, st):
            sel, r_sel = st["sel"], st["r_sel"]
            P.pe.op(lambda e: e.transpose(out=pMTb[:, 0:128], in_=sel[:], identity=ident_bf[:]),
                    reads=[r_sel, r_ident], writes=[r_pT4])
            r_sb = st["r_sb"]
            P.dve.op(lambda e: e.tensor_copy(out=qs[g][64:128, :, qt * 128:(qt + 1) * 128],
                                             in_=bc(pMTb[64:128, 0:128].unsqueeze(1), [64, 4, 128])),
                     reads=[r_pT4], writes=[r_sb])

        def attend(g, qt, st):
            units = [("s", kb) for kb in range(qt + 1)] + [("w", kb) for kb in range(max(0, qt - 4), qt + 1)]
            pend = []

            def s1(kind, kb):
                ps, r_ps = pSr.next()
                if kind == "s":
                    diag = (kb == qt)
                    P.pe.op(lambda e: e.matmul(ps[:], lhsT=kse[g][:, kb * 128:(kb + 1) * 128],
                                               rhs=qs[g][:, :, qt * 128:(qt + 1) * 128], start=True, stop=not diag),
                            reads=[r_kse[g], r_q[g], st["r_sb"]], writes=[r_ps])
                    if diag:
                        P.pe.op(lambda e: e.matmul(ps[:], lhsT=ident_bf[:], rhs=bc(cb_le[:].unsqueeze(1), [128, 4, 128]),
                                                   start=False, stop=True), reads=[r_ident, r_c4], writes=[r_ps])
                else:
                    m = cb_le if kb == qt else (cb_gt if kb == qt - 4 else None)
                    P.pe.op(lambda e: e.matmul(ps[:], lhsT=kw2[:, g, kb * 128:(kb + 1) * 128],
                                               rhs=qs[g][0:64, :, qt * 128:(qt + 1) * 128], start=True, stop=(m is None)),
                            reads=[r_kw, r_q[g]], writes=[r_ps])
                    if m is not None:
                        P.pe.op(lambda e: e.matmul(ps[:], lhsT=ident_bf[:], rhs=bc(m[:].unsqueeze(1), [128, 4, 128]),
                                                   start=False, stop=True), reads=[r_ident, r_c4], writes=[r_ps])
                ex, r_ex = ex_r.next()
                P.act.op(lambda e: e.activation(out=ex[:], in_=ps[:], func=AF.Exp, scale=0.125), reads=[r_ps], writes=[r_ex])
                return (kind, kb, ex, r_ex)

            def s2(kind, kb, ex, r_ex):
                if kind == "s":
                    po, r_po, v1, r_v1, first = pOs, r_pOs, vs1, r_vs, (kb == 0)
                else:
                    po, r_po, v1, r_v1, first = pOw, r_pOw, vw1, r_vw, (kb == max(0, qt - 4))
                for hh in range(4):
                    P.pe.op(lambda e: e.matmul(po[:, hh, :], lhsT=ex[:, hh, :], rhs=v1[:, g, kb, :],
                                               start=(first and hh == 0), stop=False, skip_group_check=True),
                            reads=[r_ex, r_v1], writes=[r_po])

            for i in range(len(units) + 2):
                if i < len(units):
                    pend.append(s1(*units[i]))
                if 0 <= i - 2 < len(units):
                    s2(*pend[i - 2])

        def combine(g, qt, st):
            gt_, r_gt = st["gt"], st["r_gt"]
            gv = gt_[:, 0:12].rearrange("p (h b) -> p h b", b=3)
            cf, r_cf = cf_r.next()
            P.dve.op(lambda e: e.reciprocal(out=cf[:, 0:4], in_=pOs[:, :, 64]), reads=[r_pOs], writes=[r_cf])
            P.dve.op(lambda e: e.reciprocal(out=cf[:, 4:8], in_=pOw[:, :, 64]), reads=[r_pOw, r_cf], writes=[r_cf])
            P.dve.op(lambda e: e.tensor_tensor(out=cf[:, 8:12], in0=cf[:, 0:4], in1=gv[:, :, 1], op=ALU.mult),
                     reads=[r_cf, r_gt], writes=[r_cf])
            P.dve.op(lambda e: e.tensor_tensor(out=cf[:, 12:16], in0=cf[:, 4:8], in1=gv[:, :, 2], op=ALU.mult),
                     reads=[r_cf, r_gt], writes=[r_cf])
            ta, r_ta = ta_r.next()
            tb, r_tb = tb_r.next()
            on, r_on = on_r.next()
            P.dve.op(lambda e: e.tensor_tensor(out=ta[:], in0=pOs[:, :, 0:64], in1=bc(cf[:, 8:12].unsqueeze(2), [128, 4, 64]),
                                               op=ALU.mult), reads=[r_pOs, r_cf], writes=[r_ta])
            P.dve.op(lambda e: e.tensor_tensor(out=tb[:], in0=pOw[:, :, 0:64], in1=bc(cf[:, 12:16].unsqueeze(2), [128, 4, 64]),
                                               op=ALU.mult), reads=[r_pOw, r_cf], writes=[r_tb])
            P.pool.op(lambda e: e.tensor_tensor(out=on[:], in0=st["oc"][:], in1=bc(gv[:, :, 0:1], [128, 4, 64]), op=ALU.mult),
                      reads=[st["r_oc"], r_gt], writes=[r_on])
            P.pool.op(lambda e: e.tensor_tensor(out=ta[:], in0=ta[:], in1=tb[:], op=ALU.add), reads=[r_ta, r_tb], writes=[r_ta])
            P.pool.op(lambda e: e.tensor_tensor(out=on[:], in0=on[:], in1=ta[:], op=ALU.add), reads=[r_on, r_ta], writes=[r_on])
            P.dma(o_nsa_d[qt * 128:(qt + 1) * 128, 256 * g:256 * (g + 1)], on[:].rearrange("p h d -> p (h d)"), reads=[r_on])

        tiles = [(g, qt) for g in range(2) for qt in range(NT)]
        if nsa_tiles is not None:
            tiles = nsa_tiles
        stn = prologue(*tiles[0])
        prologue_b(*tiles[0], stn)
        for i, (g, qt) in enumerate(tiles):
            stc = stn
            if i + 1 < len(tiles):
                stn = prologue(*tiles[i + 1])
            attend(g, qt, stc)
            if i + 1 < len(tiles):
                prologue_b(*tiles[i + 1], stn)
            combine(g, qt, stc)
        S.close()

    def rstd_ops(st, r_st, c0, n, dim):
        P.act.op(lambda e: e.activation(out=st[:, c0 + n:c0 + 2 * n], in_=st[:, c0:c0 + n], func=AF.Ln,
                                        scale=1.0 / dim, bias=eps_t[:, 0:1]), reads=[r_st, r_eps], writes=[r_st])
        P.act.op(lambda e: e.activation(out=st[:, c0 + 2 * n:c0 + 3 * n], in_=st[:, c0 + n:c0 + 2 * n], func=AF.Exp,
                                        scale=-0.5), reads=[r_st], writes=[r_st])

    def phase5a(l, x_src):
        S = Scope(P)
        wo = S.sbuf("wo_sb", [128, 8, D], BF16)
        r_wo = Res()
        P.dma_group([(wo[:, k, :], w_out_d[l, k * 128:(k + 1) * 128, :], P.pool, None) for k in range(8)], writes=[r_wo])
        gbc = S.sbuf("gbc", [128, D], F32)
        g1bc = S.sbuf("g1bc", [128, D], F32)
        r_bc = Res()
        P.dma_group([(gbc[:, 0:512], sbg_d[l].partition_broadcast(128), None, None),
                     (gbc[:, 512:1024], nsag_d[l].partition_broadcast(128), None, None),
                     (g1bc[:], modrow[l, 2 * D:3 * D].partition_broadcast(128), None, None)], writes=[r_bc])
        oin = Ring([S.sbuf(f"oin{i}", [128, D], F32) for i in range(3)])
        xin = Ring([S.sbuf(f"x5_{i}", [128, D], F32) for i in range(8)])
        junk = S.sbuf("junk5", [128, D], F32)
        r_junk = Res()
        stat = Ring([S.sbuf(f"st5_{i}", [128, 12], F32) for i in range(12)])
        mixed = Ring([S.sbuf(f"mixed{i}", [128, D], BF16) for i in range(3)])
        mixT = Ring([S.sbuf(f"mixT{i}", [128, 8, 128], BF16) for i in range(3)])
        tmp = Ring([S.sbuf(f"tmp5_{i}", [128, D], F32) for i in range(5)])
        xn2 = Ring([S.sbuf(f"xn2_{i}", [128, D], BF16) for i in range(3)])
        h2 = Ring([S.sbuf(f"h2_{i}", [128, 8, 512], BF16) for i in range(2)])
        pT_r = Ring([S.psum(f"pT5{i}", [128, 8, 128], BF16) for i in range(4)], excl=True)
        pa_r = Ring([S.psum(f"pa5{i}", [128, 512], F32) for i in range(4)], excl=True)
        B2 = modT[l][:, 24:32]
        ld = {}

        def load(ti):
            o, r_o = oin.next()
            xt, r_x = xin.next()
            rows = slice(ti * 128, (ti + 1) * 128)
            P.dma_group([(o[:, 0:512], o_sb_d[rows, :], None, None), (o[:, 512:1024], o_nsa_d[rows, :], None, None)],
                        writes=[r_o])
            P.dma(xt[:], x_src[rows, :], writes=[r_x])
            ld[ti] = (o, r_o, xt, r_x)

        def g1(ti, d):
            o, r_o, xt, r_x = ld.pop(ti)
            if ti + 1 < NT:
                load(ti + 1)
            st, r_st = stat.next()
            for hf in range(2):
                P.act.op(lambda e: e.activation(out=junk[:, 0:512], in_=o[:, hf * 512:(hf + 1) * 512], func=AF.Square,
                                                accum_out=st[:, hf:hf + 1]), reads=[r_o], writes=[r_junk, r_st])
            rstd_ops(st, r_st, 0, 2, 512)
            d.update(o=o, r_o=r_o, xt=xt, r_x=r_x, st=st, r_st=r_st)

        def g2(ti, d):
            o, r_o, st, r_st = d["o"], d["r_o"], d["st"], d["r_st"]
            mx, r_mx = mixed.next()
            for hf in range(2):
                P.dve.op(lambda e: e.scalar_tensor_tensor(out=mx[:, hf * 512:(hf + 1) * 512], in0=o[:, hf * 512:(hf + 1) * 512],
                                                          scalar=st[:, 4 + hf:5 + hf], in1=gbc[:, hf * 512:(hf + 1) * 512],
                                                          op0=ALU.mult, op1=ALU.mult),
                         reads=[r_o, r_st, r_bc], writes=[r_mx])
            d.update(mx=mx, r_mx=r_mx)

        def g3(ti, d):
            mx, r_mx = d["mx"], d["r_mx"]
            pT, r_pT = pT_r.next()
            for k in range(8):
                P.pe.op(lambda e: e.transpose(out=pT[:, k, :], in_=mx[:, k * 128:(k + 1) * 128], identity=ident_bf[:]),
                        reads=[r_mx, r_ident], writes=[r_pT])
            d.update(pT=pT, r_pT=r_pT)

        def g4(ti, d):
            mT, r_mT = mixT.next()
            P.act.op(lambda e: e.activation(out=mT[:], in_=d["pT"][:], func=AF.Copy), reads=[d["r_pT"]], writes=[r_mT])
            d.update(mT=mT, r_mT=r_mT)

        def g5(ti, d):
            mT, r_mT = d["mT"], d["r_mT"]
            pas = []
            for hf in range(2):
                pa, r_pa = pa_r.next()
                for k in range(8):
                    P.pe.op(lambda e: e.matmul(pa[:], lhsT=mT[:, k, :], rhs=wo[:, k, hf * 512:(hf + 1) * 512],
                                               start=(k == 0), stop=(k == 7)), reads=[r_mT, r_wo], writes=[r_pa])
                pas.append((pa, r_pa))
            d.update(pas=pas)

        def g6(ti, d):
            tm_, r_tm = tmp.next()
            for hf in range(2):
                pa, r_pa = d["pas"][hf]
                P.dve.op(lambda e: e.tensor_tensor(out=tm_[:, hf * 512:(hf + 1) * 512], in0=pa[:],
                                                   in1=g1bc[:, hf * 512:(hf + 1) * 512], op=ALU.mult),
                         reads=[r_pa, r_bc], writes=[r_tm])
            d.update(tm=tm_, r_tm=r_tm)

        def g7(ti, d):
            rows = slice(ti * 128, (ti + 1) * 128)
            tm_, r_tm, xt, r_x = d["tm"], d["r_tm"], d["xt"], d["r_x"]
            P.pool.op(lambda e: e.tensor_tensor(out=tm_[:], in0=tm_[:], in1=xt[:], op=ALU.add),
                      reads=[r_tm, r_x], writes=[r_tm])
            P.dma(x1_d[rows, :], tm_[:], reads=[r_tm])

        def g8(ti, d):
            tm_, r_tm, st, r_st = d["tm"], d["r_tm"], d["st"], d["r_st"]
            P.act.op(lambda e: e.activation(out=junk[:], in_=tm_[:], func=AF.Square, accum_out=st[:, 6:7]),
                     reads=[r_tm], writes=[r_junk, r_st])
            rstd_ops(st, r_st, 6, 1, D)

        def g9(ti, d):
            tm_, r_tm, st, r_st = d["tm"], d["r_tm"], d["st"], d["r_st"]
            xn, r_xn = xn2.next()
            P.dve.op(lambda e: e.tensor_scalar(out=xn[:], in0=tm_[:], scalar1=st[:, 8:9], scalar2=None, op0=ALU.mult),
                     reads=[r_tm, r_st], writes=[r_xn])
            d.update(xn=xn, r_xn=r_xn)

        def g10(ti, d):
            xn, r_xn = d["xn"], d["r_xn"]
            pT, r_pT = pT_r.next()
            for k in range(8):
                P.pe.op(lambda e: e.transpose(out=pT[:, k, :], in_=xn[:, k * 128:(k + 1) * 128], identity=ident_bf[:]),
                        reads=[r_xn, r_ident], writes=[r_pT])
            d.update(pT=pT, r_pT=r_pT)

        def g11(ti, d):
            pT, r_pT = d["pT"], d["r_pT"]
            if ti % 4 == 0:
                h2cur[0] = h2.next()
            hh4, r_hh = h2cur[0]
            hh = hh4[:, :, (ti % 4) * 128:(ti % 4 + 1) * 128]
            for k in range(8):
                if ti % 2 == 0:
                    P.act.op(lambda e: e.activation(out=hh[:, k, :], in_=pT[:, k, :], func=AF.Identity,
                                                    scale=A2[l][:, k:k + 1], bias=B2[:, k:k + 1]),
                             reads=[r_pT, r_A[l], r_modT[l]], writes=[r_hh])
                else:
                    P.dve.op(lambda e: e.tensor_scalar(out=hh[:, k, :], in0=pT[:, k, :], scalar1=A2[l][:, k:k + 1],
                                                       scalar2=B2[:, k:k + 1], op0=ALU.mult, op1=ALU.add),
                             reads=[r_pT, r_A[l], r_modT[l]], writes=[r_hh])
            if ti % 4 == 3:
                g0 = (ti // 4) * 512
                P.dma(h2T_d[:, g0:g0 + 512].rearrange("(k p) t -> p k t", p=128), hh4[:], reads=[r_hh])

        h2cur = [None]
        load(0)
        stages = [g1, g2, g3, g4, g5, g6, g7, g8, g9, g10, g11]
        sts = {}
        for it in range(NT + len(stages) - 1):
            for si, fn in enumerate(stages):
                ti = it - si
                if 0 <= ti < NT:
                    if si == 0:
                        sts[ti] = {}
                    fn(ti, sts[ti])
                    if si == len(stages) - 1:
                        sts.pop(ti)
        S.close()

    GT = 256
    NG = T // GT

    def phase5b(l, x_dst, final):
        S = Scope(P)
        wf = S.sbuf("wf_sb", [128, 8, 2 * DFF], BF16)
        wd = S.sbuf("wd_sb", [128, 22, D], BF16)
        r_wd = Res()
        r_wfb = [Res() for _ in range(4)]
        for blk in (0, 2, 1, 3):
            c0 = blk * 1408
            P.dma_group([(wf[:, k, c0:c0 + 1408], wffn_d[l, k * 128:(k + 1) * 128, c0:c0 + 1408], P.pool, None)
                         for k in range(8)], writes=[r_wfb[blk]])
            if blk == 2:
                P.dma_group([(wd[:, fc, :], wdn_d[l, fc * 128:(fc + 1) * 128, :], P.pool, None) for fc in range(22)],
                            writes=[r_wd])
        cw = S.sbuf("cw_sb", [128, 44, 3], F32)
        cb = S.sbuf("cb_sb", [128, 44], F32)
        g2bc = S.sbuf("g2bc", [128, D], F32)
        fgbc = S.sbuf("fgbc", [128, D], F32)
        r_cp = Res()
        P.dma_group([(cw[:], cw_d[l], None, None), (cb[:], cb_d[l], None, None),
                     (g2bc[:], modrow[l, 5 * D:6 * D].partition_broadcast(128), None, None),
                     (fgbc[:], fg_d.partition_broadcast(128), None, None)], writes=[r_cp])
        hal = S.sbuf("halo", [128, 44, 2], F32)
        r_hal = Res()
        P.pool.op(lambda e: e.memset(hal[:], 0.0), writes=[r_hal])
        r_hals = [Res() for _ in range(44)]
        for rr in r_hals:
            rr.w = list(r_hal.w)
        r_uhs = [Res() for _ in range(4)]
        h2 = Ring([S.sbuf(f"h2g{i}", [128, 8, GT], BF16) for i in range(2)])
        gT = S.sbuf("gT", [128, 22, GT], BF16)
        r_gT = Res()
        us = Ring([S.sbuf(f"us{i}", [128, GT + 2], F32) for i in range(4)])
        ys = Ring([S.sbuf(f"ys{i}", [128, GT], F32) for i in range(6)])
        sa_r = Ring([S.sbuf(f"sa{i}", [128, GT], F32) for i in range(2)])
        x1t = Ring([S.sbuf(f"x1t{i}", [128, D], F32) for i in range(2)])
        tmp = Ring([S.sbuf(f"tmpb{i}", [128, D], F32) for i in range(2)])
        junk = S.sbuf("junkb", [128, D], F32)
        r_junk = Res()
        stat = Ring([S.sbuf(f"stb{i}", [128, 4], F32) for i in range(2)])
        pu_r = Ring([S.psum(f"pu{i}", [128, GT], F32) for i in range(4)], excl=True)
        pd_r = Ring([S.psum(f"pd{i}", [128, 512], F32) for i in range(4)], excl=True)
        ld = {}

        def load(gi):
            h, r_h = h2.next()
            P.dma(h[:], h2T_d[:, gi * GT:(gi + 1) * GT].rearrange("(k p) t -> p k t", p=128), writes=[r_h])
            ld[gi] = (h, r_h)

        load(0)
        for gi in range(NG):
            h, r_h = ld.pop(gi)
            if gi + 1 < NG:
                load(gi + 1)
            pend_g = None

            def gate(fc_, ys_):
                sa, r_sa = sa_r.next()
                P.act.op(lambda e: e.activation(out=sa[:], in_=ys_[0][0][:], func=AF.Silu), reads=[ys_[0][1]], writes=[r_sa])
                P.dve.op(lambda e: e.tensor_tensor(out=gT[:, fc_, :], in0=sa[:], in1=ys_[1][0][:], op=ALU.mult),
                         reads=[r_sa, ys_[1][1]], writes=[r_gT])

            for fc in range(22):
                ysab = []
                uts = []
                for part in range(2):
                    ci = part * 22 + fc
                    ui = us.i
                    u, r_u = us.next()
                    r_uh = r_uhs[ui]
                    P.pool.op(lambda e: e.tensor_copy(out=u[:, 0:2], in_=hal[:, ci, :]), reads=[r_hals[ci]], writes=[r_uh])
                    uts.append((u, r_u, r_uh))
                for part in range(2):
                    ci = part * 22 + fc
                    pu, r_pu = pu_r.next()
                    for k in range(8):
                        P.pe.op(lambda e: e.matmul(pu[:], lhsT=wf[:, k, ci * 128:(ci + 1) * 128], rhs=h[:, k, :],
                                                   start=(k == 0), stop=(k == 7)),
                                reads=[r_wfb[(ci * 128) // 1408], r_h], writes=[r_pu])
                    u, r_u, r_uh = uts[part]
                    y, r_y = ys.next()
                    P.act.op(lambda e: e.activation(out=u[:, 2:GT + 2], in_=pu[:], func=AF.Copy), reads=[r_pu], writes=[r_u])
                    P.act.op(lambda e: e.activation(out=y[:], in_=pu[:], func=AF.Identity, scale=cw[:, ci, 2:3],
                                                    bias=cb[:, ci:ci + 1]), reads=[r_pu, r_cp], writes=[r_y])
                    P.act.op(lambda e: e.activation(out=hal[:, ci, :], in_=pu[:, GT - 2:GT], func=AF.Copy),
                             reads=[r_pu], writes=[r_hals[ci]])
                    P.dve.op(lambda e: e.scalar_tensor_tensor(out=y[:], in0=u[:, 1:GT + 1], scalar=cw[:, ci, 1:2], in1=y[:],
                                                              op0=ALU.mult, op1=ALU.add),
                             reads=[r_u, r_uh, r_cp, r_y], writes=[r_y])
                    P.dve.op(lambda e: e.scalar_tensor_tensor(out=y[:], in0=u[:, 0:GT], scalar=cw[:, ci, 0:1], in1=y[:],
                                                              op0=ALU.mult, op1=ALU.add),
                             reads=[r_u, r_uh, r_cp, r_y], writes=[r_y])
                    ysab.append((y, r_y))
                if pend_g is not None:
                    gate(*pend_g)
                pend_g = (fc, ysab)
            gate(*pend_g)
            for ts in range(GT // 128):
                ti = gi * (GT // 128) + ts
                rows = slice(ti * 128, (ti + 1) * 128)
                x1, r_x1 = x1t.next()
                P.dma(x1[:], x1_d[rows, :], writes=[r_x1])
                tm_, r_tm = tmp.next()
                for hf in range(2):
                    pd, r_pd = pd_r.next()
                    for fc in range(22):
                        P.pe.op(lambda e: e.matmul(pd[:], lhsT=gT[:, fc, ts * 128:(ts + 1) * 128],
                                                   rhs=wd[:, fc, hf * 512:(hf + 1) * 512], start=(fc == 0), stop=(fc == 21)),
                                reads=[r_gT, r_wd], writes=[r_pd])
                    P.dve.op(lambda e: e.tensor_tensor(out=tm_[:, hf * 512:(hf + 1) * 512], in0=pd[:],
                                                       in1=g2bc[:, hf * 512:(hf + 1) * 512], op=ALU.mult),
                             reads=[r_pd, r_cp], writes=[r_tm])
                P.pool.op(lambda e: e.tensor_tensor(out=tm_[:], in0=tm_[:], in1=x1[:], op=ALU.add),
                          reads=[r_tm, r_x1], writes=[r_tm])
                if not final:
                    P.dma(x_dst[rows, :], tm_[:], reads=[r_tm])
                else:
                    st, r_st = stat.next()
                    P.act.op(lambda e: e.activation(out=junk[:], in_=tm_[:], func=AF.Square, accum_out=st[:, 0:1]),
                             reads=[r_tm], writes=[r_junk, r_st])
                    rstd_ops(st, r_st, 0, 1, D)
                    P.dve.op(lambda e: e.scalar_tensor_tensor(out=x1[:], in0=tm_[:], scalar=st[:, 2:3], in1=fgbc[:],
                                                              op0=ALU.mult, op1=ALU.mult),
                             reads=[r_tm, r_st, r_cp, r_x1], writes=[r_x1])
                    P.dma(x_dst[rows, :], x1[:], reads=[r_x1])
        S.close()

    if stop_after is None:
        for l in range(L):
            x_src = x_in if l == 0 else xbuf_d
            phase0(l)
            P.barrier()
            phase1(l, x_src)
            P.barrier()
            phase3(l)
            P.barrier()
            phase4(l)
            P.barrier()
            phase5a(l, x_src)
            P.barrier()
            phase5b(l, out_d if l == L - 1 else xbuf_d, final=(l == L - 1))
            P.barrier()
    else:
        phase0(0)
        P.barrier()
        phase1(0, x_in)
        P.barrier()
        if stop_after in ("p3", "l0"):
            phase3(0)
            P.barrier()
        if stop_after in ("p4", "l0"):
            phase4(0)
            P.barrier()
        if stop_after == "l0":
            phase5a(0, x_in)
            P.barrier()
            phase5b(0, xbuf_d, final=False)
            P.barrier()
        S = Scope(P)
        z = S.sbuf("zz", [128, D], F32)
        rz = Res()
        P.dve.op(lambda e: e.memset(z[:], 0.0), writes=[rz])
        for ti in range(NT):
            P.dma(out_d[ti * 128:(ti + 1) * 128, :], z[:], reads=[rz])
        S.close()
    P.finish()
    return nc


def host_inputs(inputs):
    cols = _ext_cols()
    f32 = lambda a: np.ascontiguousarray(np.asarray(a, dtype=np.float32))
    w_ext = f32(np.asarray(inputs["w_in"])[:, :, cols])
    shared = {
        "ln1T": f32(np.asarray(inputs["ln1_g"]).reshape(L, 8, 128).transpose(0, 2, 1)),
        "ln2T": f32(np.asarray(inputs["ln2_g"]).reshape(L, 8, 128).transpose(0, 2, 1)),
        "w_ada": f32(inputs["w_ada"]),
        "b_adaT": f32(np.asarray(inputs["b_ada"]).reshape(L, 48, 128).transpose(0, 2, 1)),
        "w_ext": w_ext,
        "cmp_w1_k": f32(inputs["cmp_w1_k"]), "cmp_w1_v": f32(inputs["cmp_w1_v"]),
        "cmp_w2_k": f32(inputs["cmp_w2_k"]), "cmp_w2_v": f32(inputs["cmp_w2_v"]),
        "cmp_w2_k_sw": f32(np.asarray(inputs["cmp_w2_k"])[:, :, _swap64(np.arange(64))]),
        "sb_out_g": f32(inputs["sb_out_g"]), "nsa_out_g": f32(inputs["nsa_out_g"]),
        "w_out": f32(inputs["w_out"]), "ffn_w_in": f32(inputs["ffn_w_in"]), "ffn_w_down": f32(inputs["ffn_w_down"]),
        "conv_w": f32(np.asarray(inputs["ffn_conv_w"]).reshape(L, 3, 44, 128).transpose(0, 3, 2, 1)),
        "conv_b": f32(np.asarray(inputs["ffn_conv_b"]).reshape(L, 44, 128).transpose(0, 2, 1)),
        "final_g": f32(inputs["final_g"]),
        "posTk": f32(np.asarray(inputs["cmp_pos_k"]).transpose(0, 2, 1)),
        "posTv": f32(np.asarray(inputs["cmp_pos_v"]).transpose(0, 2, 1)),
    }
    shared.update(_consts())
    x = np.asarray(inputs["x"])
    c = np.asarray(inputs["c"])
    in_maps = []
    for b in range(8):
        m = dict(shared)
        m["x"] = f32(x[b])
        m["cT"] = f32(c[b].reshape(8, 128).T)
        in_maps.append(m)
    return in_maps


def kernel(**inputs):
    in_maps = host_inputs(inputs)
    nc = build_program()
    res = run_bass_kernel_spmd(nc, in_maps, core_ids=list(range(8)))
    return np.stack([r["out"] for r in res.results], axis=0).astype(np.float32)
```

```python
import numpy as np
import ml_dtypes
import concourse.bass as bass
import concourse.mybir as mybir
from concourse.bass_utils import run_bass_kernel_spmd

F32 = mybir.dt.float32
BF16 = mybir.dt.bfloat16
AF = mybir.ActivationFunctionType
ALU = mybir.AluOpType
AX = mybir.AxisListType

T = 4096
D = 1024
L = 2
NT = T // 128
DFF = 2816
EPS = 1e-6
N_FM = 22 * 128
N_TM = 792
N_EXT = N_FM + N_TM


class Res:
    __slots__ = ("w", "r", "excl")

    def __init__(self, excl=False):
        self.w = []
        self.r = []
        self.excl = excl


class Eng:
    def __init__(self, name, e, sem, is_pe=False):
        self.name = name
        self.e = e
        self.sem = sem
        self.count = 0
        self.waited = {}
        self.is_pe = is_pe

    def _wait(self, tok):
        sem, val = tok
        k = id(sem)
        if self.waited.get(k, 0) >= val:
            return
        self.e.wait_ge(sem, val)
        self.waited[k] = val

    def _deps(self, reads, writes):
        for r in reads:
            for t in r.w:
                if not (self.is_pe and t[0] is self.sem):
                    self._wait(t)
            if r.excl:
                for t in r.r:
                    if t[0] is not self.sem:
                        self._wait(t)
        for r in writes:
            for t in r.w:
                if not (self.is_pe and t[0] is self.sem):
                    self._wait(t)
            for t in r.r:
                if t[0] is self.sem:
                    continue
                self._wait(t)

    def op(self, fn, reads=(), writes=()):
        self._deps(reads, writes)
        ins = fn(self.e)
        self.count += 1
        ins.then_inc(self.sem, 1)
        tok = (self.sem, self.count)
        for r in reads:
            r.r = [t for t in r.r if t[0] is not self.sem] + [tok]
        for r in writes:
            r.w = [tok]
            r.r = []
        return tok


class Prog:
    def __init__(self, nc, n_dma_sems=40):
        self.nc = nc
        self._ctx = []
        mk = lambda name: self._enter(nc.semaphore(name))
        self.pe = Eng("pe", nc.tensor, mk("s_pe"), is_pe=True)
        self.act = Eng("act", nc.scalar, mk("s_act"))
        self.dve = Eng("dve", nc.vector, mk("s_dve"))
        self.pool = Eng("pool", nc.gpsimd, mk("s_pool"))
        self.sp = Eng("sp", nc.sync, mk("s_sp"))
        self.dma_sems = [mk(f"s_dma{i}") for i in range(n_dma_sems)]
        self.dma_vals = [0] * n_dma_sems
        self.dma_rr = 0

    def _enter(self, cm):
        v = cm.__enter__()
        self._ctx.append(cm)
        return v

    def sbuf(self, name, shape, dtype):
        return self._enter(self.nc.sbuf_tensor(name, list(shape), dtype))

    def psum(self, name, shape, dtype=F32):
        return self._enter(self.nc.psum_tensor(name, list(shape), dtype))

    def dma_group(self, items, reads=(), writes=()):
        dep = []
        for r in reads:
            dep += r.w
        for r in writes:
            dep += r.w
            dep += r.r
        toks = []
        for it in items:
            out, in_, q, kw = (list(it) + [None, None])[:4]
            q = q or self.sp
            kw = kw or {}
            i = self.dma_rr
            self.dma_rr = (self.dma_rr + 1) % len(self.dma_sems)
            sem = self.dma_sems[i]
            if self.dma_vals[i] > 0:
                q._wait((sem, self.dma_vals[i]))
            for t in dep:
                q._wait(t)
            ins = q.e.dma_start(out=out, in_=in_, **kw)
            self.dma_vals[i] += 16
            ins.then_inc(sem, 16)
            toks.append((sem, self.dma_vals[i]))
        for r in reads:
            r.r = r.r + toks
        for r in writes:
            r.w = list(toks)
            r.r = []
        return toks

    def dma(self, out, in_, reads=(), writes=(), q=None, **kw):
        return self.dma_group([(out, in_, q, kw)], reads, writes)

    def barrier(self):
        engs = (self.pe, self.act, self.dve, self.pool, self.sp)
        for e in engs:
            for i, sem in enumerate(self.dma_sems):
                if self.dma_vals[i] > 0:
                    e._wait((sem, self.dma_vals[i]))
            for o in engs:
                if o is not e and o.count > 0:
                    e._wait((o.sem, o.count))

    def finish(self):
        self.barrier()
        for cm in reversed(self._ctx):
            cm.__exit__(None, None, None)
        self._ctx = []


class Scope:
    _uid = [0]

    def __init__(self, P):
        self.P = P
        self.cms = []
        Scope._uid[0] += 1
        self.sfx = f"_s{Scope._uid[0]}"

    def sbuf(self, name, shape, dtype):
        cm = self.P.nc.sbuf_tensor(name + self.sfx, list(shape), dtype)
        v = cm.__enter__()
        self.cms.append(cm)
        return v

    def psum(self, name, shape, dtype=F32):
        isz = 4 if dtype == F32 else 2
        cm = self.P.nc.psum_tensor(name + self.sfx, [128, 2048 // isz], dtype)
        v = cm.__enter__()
        self.cms.append(cm)
        n = int(np.prod(shape[1:]))
        view = v[0:shape[0], 0:n]
        if len(shape) == 3:
            view = view.rearrange("p (a b) -> p a b", b=shape[2])
        return view

    def close(self):
        for cm in reversed(self.cms):
            cm.__exit__(None, None, None)
        self.cms = []


class Ring:
    def __init__(self, tiles, excl=False):
        self.tiles = tiles
        self.res = [Res(excl) for _ in tiles]
        self.i = 0

    def next(self):
        t, r = self.tiles[self.i], self.res[self.i]
        self.i = (self.i + 1) % len(self.tiles)
        return t, r


def _swap64(cols):
    cols = np.asarray(cols).reshape(-1, 64)
    return np.concatenate([cols[:, 32:], cols[:, :32]], axis=1).reshape(-1)


def _ext_cols():
    r = lambda a, b: np.arange(a, b)
    sbq, sbk, sbv = r(0, 512), r(512, 1024), r(1024, 1536)
    nq, kc, vc = r(1536, 2048), r(2048, 2176), r(2176, 2304)
    ks, vs, kw, vw, gl = r(2304, 2432), r(2432, 2560), r(2560, 2688), r(2688, 2816), r(2816, 2840)
    fm = [sbq, sbk]
    for c in range(4):
        fm += [nq[c * 128:(c + 1) * 128], _swap64(nq[c * 128:(c + 1) * 128])]
    fm += [ks, _swap64(ks), kw, _swap64(kw), kc, vc]
    tm = [sbv, vs, vw, gl]
    cols = np.concatenate(fm + tm)
    assert cols.shape[0] == N_EXT
    return cols


def _consts():
    half = 32
    inv = (10000.0 ** (-np.arange(half, dtype=np.float32) / half)).astype(np.float32)
    pos = np.arange(T, dtype=np.float32)
    d = np.arange(128) % 64
    ang = (pos[None, :] * inv[d % 32][:, None]).astype(np.float32)
    cos = np.cos(ang).astype(np.float32)
    sin = np.sin(ang).astype(np.float32)
    sgn = np.where(d < 32, -1.0, 1.0).astype(np.float32)[:, None]
    c = {
        "rope_cos": cos, "rope_sin": (sin * sgn).astype(np.float32),
        "ident_bf": np.eye(128).astype(ml_dtypes.bfloat16),
        "ident_f32": np.eye(128).astype(np.float32),
    }
    j = np.arange(128)
    bf = ml_dtypes.bfloat16
    c["tri_ge"] = (j[:, None] >= j[None, :]).astype(bf)
    c["tri_lt"] = (j[:, None] < j[None, :]).astype(bf)
    c["tri_le"] = (j[:, None] <= j[None, :]).astype(bf)
    c["tri_gt"] = (j[:, None] > j[None, :]).astype(bf)
    n = np.arange(256, dtype=np.float32)
    angc = ((16.0 * n + 31.0)[None, :] * inv[d % 32][:, None]).astype(np.float32)
    c["cmp_cos"] = np.cos(angc).astype(np.float32)
    c["cmp_sin"] = (np.sin(angc).astype(np.float32) * sgn).astype(np.float32)
    nn = np.arange(128)[:, None, None]
    qq = np.arange(17)[None, :, None]
    tt = np.arange(128)[None, None, :]
    BIG = 240000.0
    c["cmaskb"] = np.where(16 * nn + 31 <= 128 * qq + tt, 0.0, -BIG).astype(bf)
    c["cb_le"] = np.where(j[:, None] <= j[None, :], 0.0, -BIG).astype(bf)
    c["cb_gt"] = np.where(j[:, None] > j[None, :], 0.0, -BIG).astype(bf)
    ncmp = np.arange(256)[:, None]
    jj = np.arange(64)[None, :]
    ov = np.clip(np.minimum(16 * ncmp + 32, 64 * jj + 64) - np.maximum(16 * ncmp, 64 * jj), 0, None) / 32.0
    ov[255, :] = 0.0
    c["ovT"] = np.ascontiguousarray(ov.reshape(2, 128, 64).transpose(1, 0, 2)).astype(bf)
    sidx = np.arange(T)[None, :]
    c["eexp"] = (sidx // 64 == np.arange(64)[:, None]).astype(bf)
    ttq = np.arange(128)[:, None]
    rel = np.arange(128)[None, :] - 62
    dist = (ttq >= 64).astype(np.int64) - rel
    c["rkeep"] = (dist >= 2).astype(np.float32)
    c["radd"] = np.where(dist < 0, -1.0, np.where(dist <= 1, 1e6, 0.0)).astype(np.float32)
    return c


def build_program(dbg=None, stop_after=None, nsa_tiles=None, dbg_tile=None):
    nc = bass.Bass("TRN2", target_bir_lowering=False)
    dbg = dbg or []

    def din(name, shape, dt=F32):
        return nc.dram_tensor(name, list(shape), dt, kind="ExternalInput").ap()

    def dscr(name, shape, dt):
        kind = "ExternalOutput" if name in dbg else "Internal"
        return nc.dram_tensor(name, list(shape), dt, kind=kind).ap()

    x_in = din("x", [T, D])
    cT_in = din("cT", [128, 8])
    ln1T = din("ln1T", [L, 128, 8])
    ln2T = din("ln2T", [L, 128, 8])
    w_ada = din("w_ada", [L, D, 6 * D])
    b_adaT = din("b_adaT", [L, 128, 48])
    w_ext = din("w_ext", [L, D, N_EXT])
    rope_cos = din("rope_cos", [128, T])
    rope_sin = din("rope_sin", [128, T])
    ident_bf_d = din("ident_bf", [128, 128], BF16)
    ident_f_d = din("ident_f32", [128, 128])
    tri_d = {n: din(n, [128, 128], BF16) for n in ("tri_ge", "tri_lt", "tri_le", "tri_gt")}
    w1k_d = din("cmp_w1_k", [L, 2048, 256])
    w1v_d = din("cmp_w1_v", [L, 2048, 256])
    w2k_d = din("cmp_w2_k", [L, 256, 64])
    w2ksw_d = din("cmp_w2_k_sw", [L, 256, 64])
    w2v_d = din("cmp_w2_v", [L, 256, 64])
    posTk_d = din("posTk", [L, 64, 32])
    posTv_d = din("posTv", [L, 64, 32])
    sbg_d = din("sb_out_g", [L, 512])
    nsag_d = din("nsa_out_g", [L, 512])
    w_out_d = din("w_out", [L, D, D])
    wffn_d = din("ffn_w_in", [L, D, 2 * DFF])
    wdn_d = din("ffn_w_down", [L, DFF, D])
    cw_d = din("conv_w", [L, 128, 44, 3])
    cb_d = din("conv_b", [L, 128, 44])
    fg_d = din("final_g", [D])
    cmp_cos_d = din("cmp_cos", [128, 256])
    cmp_sin_d = din("cmp_sin", [128, 256])
    cmaskb_d = din("cmaskb", [128, 17, 128], BF16)
    cb_le_d = din("cb_le", [128, 128], BF16)
    cb_gt_d = din("cb_gt", [128, 128], BF16)
    ovT_d = din("ovT", [128, 2, 64], BF16)
    eexp_d = din("eexp", [64, T], BF16)
    rkeep_d = din("rkeep", [128, 128])
    radd_d = din("radd", [128, 128])
    out_d = nc.dram_tensor("out", [T, D], F32, kind="ExternalOutput").ap()

    o_sb_d = dscr("o_sb", [T, 512], F32)
    o_nsa_d = dscr("o_nsa", [T, 512], F32)
    xbuf_d = dscr("xbuf", [T, D], F32)
    x1_d = dscr("x1buf", [T, D], F32)
    h2T_d = dscr("h2T", [D, T], BF16)
    fmT = dscr("fmT", [2048, T], BF16)
    tmv = dscr("tmv", [T, 768], BF16)
    gates_d = dscr("gates", [T, 24], F32)
    modrow = dscr("modrow", [L, 6 * D], F32)

    P = Prog(nc)
    def dump(name, ap, res):
        if name not in dbg:
            return
        shp = [int(v) for v in ap.shape]
        dt_ = ap.dtype
        d = nc.dram_tensor(name, shp, dt_, kind="ExternalOutput").ap()
        P.dma(d, ap, reads=[res])

    ident_bf = P.sbuf("ident_bf_sb", [128, 128], BF16)
    r_ident = Res()
    P.dma(ident_bf[:], ident_bf_d[:, :], writes=[r_ident])
    ident_f = P.sbuf("ident_f_sb", [128, 128], F32)
    r_identf = Res()
    P.dma(ident_f[:], ident_f_d[:, :], writes=[r_identf])
    tri = {}
    r_tri = Res()
    for n in tri_d:
        tri[n] = P.sbuf(n + "_sb", [128, 128], BF16)
    P.dma_group([(tri[n][:], tri_d[n][:, :], None, None) for n in tri_d], writes=[r_tri])
    eps_t = P.sbuf("eps_t", [128, 1], F32)
    r_eps = Res()
    P.dve.op(lambda e: e.memset(eps_t[:], EPS), writes=[r_eps])
    cT = P.sbuf("cT_sb", [128, 8], F32)
    r_cT = Res()
    P.dma(cT[:], cT_in[:, :], writes=[r_cT])
    siluc = P.sbuf("siluc", [128, 8], F32)
    r_siluc = Res()
    P.act.op(lambda e: e.activation(out=siluc[:], in_=cT[:], func=AF.Silu), reads=[r_cT], writes=[r_siluc])
    modT = [P.sbuf(f"modT{l}", [128, 48], F32) for l in range(L)]
    r_modT = [Res() for _ in range(L)]
    A1 = [P.sbuf(f"A1_{l}", [128, 8], F32) for l in range(L)]
    A2 = [P.sbuf(f"A2_{l}", [128, 8], F32) for l in range(L)]
    r_A = [Res() for _ in range(L)]
    r_modrow = [Res() for _ in range(L)]

    def phase0(l):
        S = Scope(P)
        wbuf = Ring([S.sbuf(f"wada{i}", [128, 6 * D], F32) for i in range(2)])
        pm = S.psum("pmod", [128, 48], F32)
        r_pm = Res(True)
        lnt = S.sbuf("lnt", [128, 16], F32)
        r_lnt = Res()
        bT = S.sbuf("bT", [128, 48], F32)
        r_bT = Res()
        P.dma(lnt[:, 0:8], ln1T[l], writes=[r_lnt])
        P.dma(lnt[:, 8:16], ln2T[l], writes=[r_lnt])
        P.dma(bT[:], b_adaT[l], writes=[r_bT])
        for k in range(8):
            wt, rw = wbuf.next()
            P.dma_group([(wt[:, hf * 3072:(hf + 1) * 3072],
                          w_ada[l, k * 128:(k + 1) * 128, hf * 3072:(hf + 1) * 3072], None, None)
                         for hf in range(2)], writes=[rw])
            for m in range(48):
                P.pe.op(lambda e: e.matmul(pm[:, m:m + 1], lhsT=wt[:, m * 128:(m + 1) * 128], rhs=siluc[:, k:k + 1],
                                           start=(k == 0 and m == 0), stop=(k == 7 and m == 47),
                                           skip_group_check=True),
                        reads=[rw, r_siluc], writes=[r_pm])
        P.dve.op(lambda e: e.tensor_tensor(out=modT[l][:], in0=pm[:], in1=bT[:], op=ALU.add),
                 reads=[r_pm, r_bT], writes=[r_modT[l]])
        P.dve.op(lambda e: e.scalar_tensor_tensor(out=A1[l][:], in0=modT[l][:, 8:16], scalar=1.0, in1=lnt[:, 0:8],
                                                  op0=ALU.add, op1=ALU.mult),
                 reads=[r_modT[l], r_lnt], writes=[r_A[l]])
        P.dve.op(lambda e: e.scalar_tensor_tensor(out=A2[l][:], in0=modT[l][:, 32:40], scalar=1.0, in1=lnt[:, 8:16],
                                                  op0=ALU.add, op1=ALU.mult),
                 reads=[r_modT[l], r_lnt], writes=[r_A[l]])
        pmt = S.psum("pmt", [48, 128], F32)
        r_pmt = Res(True)
        P.pe.op(lambda e: e.transpose(out=pmt[:], in_=modT[l][:], identity=ident_f[:]),
                reads=[r_modT[l], r_identf], writes=[r_pmt])
        mrow = S.sbuf("mrow", [48, 128], F32)
        r_mrow = Res()
        P.dve.op(lambda e: e.tensor_copy(out=mrow[:], in_=pmt[:]), reads=[r_pmt], writes=[r_mrow])
        P.dma(modrow[l].rearrange("(m p) -> m p", p=128), mrow[:], reads=[r_mrow])
        S.close()

    def phase1(l, x_src):
        S = Scope(P)
        wsb = S.sbuf("w1sb", [128, 8, N_EXT], BF16)
        blocks = [(N_FM, N_EXT), (0, 1024), (1024, 2048), (2048, N_FM)]
        r_wb = [Res() for _ in blocks]
        for (c0, c1), rw in zip(blocks, r_wb):
            P.dma_group([(wsb[:, k, c0:c1], w_ext[l, k * 128:(k + 1) * 128, c0:c1], P.pool, None) for k in range(8)],
                        writes=[rw])

        def r_wcols(c0):
            for (b0, b1), rw in zip(blocks, r_wb):
                if b0 <= c0 < b1:
                    return rw
            raise AssertionError

        cosb = S.sbuf("cosb", [128, T], F32)
        sinb = S.sbuf("sinb", [128, T], F32)
        r_rope = Res()
        P.dma(cosb[:], rope_cos[:, :], writes=[r_rope])
        r_rope2 = Res()
        P.dma(sinb[:], rope_sin[:, :], writes=[r_rope2])
        xt = Ring([S.sbuf(f"xt{i}", [128, D], F32) for i in range(3)])
        junk = S.sbuf("junk", [128, D], F32)
        r_junk = Res()
        stat = Ring([S.sbuf(f"stat{i}", [128, 4], F32) for i in range(4)])
        xn = Ring([S.sbuf(f"xn{i}", [128, D], BF16) for i in range(3)])
        hT = Ring([S.sbuf(f"hT{i}", [128, 8, 512], BF16) for i in range(2)])
        stg = Ring([S.sbuf(f"stg{i}", [128, 512], BF16) for i in range(6)])
        stgtm = Ring([S.sbuf(f"stgtm{i}", [128, 768], BF16) for i in range(2)])
        gst = Ring([S.sbuf(f"gst{i}", [128, 24], F32) for i in range(2)])
        rt1 = Ring([S.sbuf(f"rt1_{i}", [128, 512], F32) for i in range(2)])
        rt2 = Ring([S.sbuf(f"rt2_{i}", [128, 512], F32) for i in range(2)])
        pT_r = Ring([S.psum(f"pT{i}", [128, 8, 128], BF16) for i in range(2)], excl=True)
        ptm_r = Ring([S.psum(f"ptm{i}", [128, 512], F32) for i in range(2)], excl=True)
        pfm = Ring([S.psum(f"pfm{i}", [128, 512], F32) for i in range(4)], excl=True)
        B1 = modT[l][:, 0:8]
        xl = {}
        hcur = {}

        def ldx(ti):
            xtile, r_x = xt.next()
            P.dma(xtile[:], x_src[ti * 128:(ti + 1) * 128, :], writes=[r_x])
            xl[ti] = (xtile, r_x)

        def g1(ti, d):
            xtile, r_x = xl.pop(ti)
            if ti + 1 < NT:
                ldx(ti + 1)
            st, r_st = stat.next()
            P.act.op(lambda e: e.activation(out=junk[:], in_=xtile[:], func=AF.Square, accum_out=st[:, 0:1]),
                     reads=[r_x], writes=[r_junk, r_st])
            rstd_ops(st, r_st, 0, 1, D)
            d.update(xtile=xtile, r_x=r_x, st=st, r_st=r_st)

        def g2(ti, d):
            xnt, r_xn = xn.next()
            P.dve.op(lambda e: e.tensor_scalar(out=xnt[:], in0=d["xtile"][:], scalar1=d["st"][:, 2:3], scalar2=None,
                                               op0=ALU.mult), reads=[d["r_x"], d["r_st"]], writes=[r_xn])
            d.update(xnt=xnt, r_xn=r_xn)

        def g3(ti, d):
            xnt, r_xn = d["xnt"], d["r_xn"]
            pT, r_pT = pT_r.next()
            for k in range(8):
                P.pe.op(lambda e: e.transpose(out=pT[:, k, :], in_=xnt[:, k * 128:(k + 1) * 128], identity=ident_bf[:]),
                        reads=[r_xn, r_ident], writes=[r_pT])
            d.update(pT=pT, r_pT=r_pT)

        def g4(ti, d):
            grp, tt = divmod(ti, 4)
            if tt == 0:
                hcur[grp] = hT.next()
            h, r_h = hcur[grp]
            pT, r_pT = d["pT"], d["r_pT"]
            for k in range(8):
                dst = h[:, k, tt * 128:(tt + 1) * 128]
                if ti % 2 == 0:
                    P.act.op(lambda e: e.activation(out=dst, in_=pT[:, k, :], func=AF.Identity,
                                                    scale=A1[l][:, k:k + 1], bias=B1[:, k:k + 1]),
                             reads=[r_pT, r_A[l], r_modT[l]], writes=[r_h])
                else:
                    P.dve.op(lambda e: e.tensor_scalar(out=dst, in0=pT[:, k, :], scalar1=A1[l][:, k:k + 1],
                                                       scalar2=B1[:, k:k + 1], op0=ALU.mult, op1=ALU.add),
                             reads=[r_pT, r_A[l], r_modT[l]], writes=[r_h])

        def g5(ti, d):
            grp, tt = divmod(ti, 4)
            h, r_h = hcur[grp]
            p0, r_p0 = ptm_r.next()
            p1, r_p1 = ptm_r.next()
            for k in range(8):
                lhsT = h[:, k, tt * 128:(tt + 1) * 128]
                P.pe.op(lambda e: e.matmul(p0[:], lhsT=lhsT, rhs=wsb[:, k, N_FM:N_FM + 512],
                                           start=(k == 0), stop=(k == 7)), reads=[r_h, r_wb[0]], writes=[r_p0])
            for k in range(8):
                lhsT = h[:, k, tt * 128:(tt + 1) * 128]
                P.pe.op(lambda e: e.matmul(p1[:, 0:280], lhsT=lhsT, rhs=wsb[:, k, N_FM + 512:N_EXT],
                                           start=(k == 0), stop=(k == 7)), reads=[r_h, r_wb[0]], writes=[r_p1])
            d.update(p0=p0, r_p0=r_p0, p1=p1, r_p1=r_p1)

        def g6(ti, d):
            p0, r_p0, p1, r_p1 = d["p0"], d["r_p0"], d["p1"], d["r_p1"]
            sg, r_sg = stgtm.next()
            P.act.op(lambda e: e.activation(out=sg[:, 0:512], in_=p0[:], func=AF.Copy), reads=[r_p0], writes=[r_sg])
            P.dve.op(lambda e: e.tensor_copy(out=sg[:, 512:768], in_=p1[:, 0:256]), reads=[r_p1], writes=[r_sg])
            gs, r_gs = gst.next()
            P.act.op(lambda e: e.activation(out=gs[:], in_=p1[:, 256:280], func=AF.Sigmoid), reads=[r_p1], writes=[r_gs])
            P.dma(tmv[ti * 128:(ti + 1) * 128, :], sg[:], reads=[r_sg])
            P.dma(gates_d[ti * 128:(ti + 1) * 128, :], gs[:], reads=[r_gs])

        evc = [0]

        def fm_group(grp):
            h, r_h = hcur[grp]
            tsl = slice(grp * 512, (grp + 1) * 512)

            def fm_mm(ch):
                pf, r_pf = pfm.next()
                rw = r_wcols(ch * 128)
                for k in range(8):
                    P.pe.op(lambda e: e.matmul(pf[:], lhsT=wsb[:, k, ch * 128:(ch + 1) * 128], rhs=h[:, k, :],
                                               start=(k == 0), stop=(k == 7)), reads=[r_h, rw], writes=[r_pf])
                return pf, r_pf

            for ch in range(8):
                pf, r_pf = fm_mm(ch)
                s_, r_s = stg.next()
                if evc[0] % 2 == 0:
                    P.act.op(lambda e: e.activation(out=s_[:], in_=pf[:], func=AF.Copy), reads=[r_pf], writes=[r_s])
                else:
                    P.dve.op(lambda e: e.tensor_copy(out=s_[:], in_=pf[:]), reads=[r_pf], writes=[r_s])
                evc[0] += 1
                P.dma(fmT[ch * 128:(ch + 1) * 128, tsl], s_[:], reads=[r_s])
            for pr in range(6):
                pa, r_pa = fm_mm(8 + 2 * pr)
                pb, r_pb = fm_mm(9 + 2 * pr)
                t1, r_t1 = rt1.next()
                t2, r_t2 = rt2.next()
                P.dve.op(lambda e: e.tensor_tensor(out=t1[:], in0=pa[:], in1=cosb[:, tsl], op=ALU.mult),
                         reads=[r_pa, r_rope], writes=[r_t1])
                P.dve.op(lambda e: e.tensor_tensor(out=t2[:], in0=pb[:], in1=sinb[:, tsl], op=ALU.mult),
                         reads=[r_pb, r_rope2], writes=[r_t2])
                s_, r_s = stg.next()
                P.pool.op(lambda e: e.tensor_tensor(out=s_[:], in0=t1[:], in1=t2[:], op=ALU.add),
                          reads=[r_t1, r_t2], writes=[r_s])
                P.dma(fmT[1024 + pr * 128:1024 + (pr + 1) * 128, tsl], s_[:], reads=[r_s])
            for j in range(2):
                pf, r_pf = fm_mm(20 + j)
                s_, r_s = stg.next()
                P.act.op(lambda e: e.activation(out=s_[:], in_=pf[:], func=AF.Copy), reads=[r_pf], writes=[r_s])
                P.dma(fmT[1792 + j * 128:1792 + (j + 1) * 128, tsl], s_[:], reads=[r_s])

        ldx(0)
        stages = [g1, g2, g3, g4, g5, g6]
        sts = {}
        for it in range(NT + len(stages) - 1):
            fm_after = None
            for si, fn in reversed(list(enumerate(stages))):
                ti = it - si
                if 0 <= ti < NT:
                    if si == 0:
                        sts[ti] = {}
                    fn(ti, sts[ti])
                    if si == 3 and ti % 4 == 3:
                        fm_after = ti // 4
                    if si == len(stages) - 1:
                        sts.pop(ti)
            if fm_after is not None:
                fm_group(fm_after)
        S.close()

    def phase3(l):
        S = Scope(P)
        qT = Ring([S.sbuf(f"sbq{i}", [64, T], BF16) for i in range(2)])
        kT = Ring([S.sbuf(f"sbk{i}", [64, T], BF16) for i in range(2)])
        vt = Ring([S.sbuf(f"sbv{i}", [128, NT, 64], BF16) for i in range(2)])
        e_r = Ring([S.sbuf(f"e{i}", [128, 512], F32) for i in range(3)])
        sp_r = Ring([S.sbuf(f"sp{i}", [128, 512], BF16) for i in range(3)])
        w_r = Ring([S.sbuf(f"w{i}", [128, 512], BF16) for i in range(3)])
        o_r = Ring([S.sbuf(f"osb{i}", [128, 4, 64], F32) for i in range(2)])
        pS = Ring([S.psum(f"pS{i}", [128, 512], F32) for i in range(2)], excl=True)
        p2 = Ring([S.psum(f"p2{i}", [128, 512], F32) for i in range(2)], excl=True)
        pD = Ring([S.psum(f"pD{i}", [128, 512], F32) for i in range(2)], excl=True)
        pacc = Ring([S.psum(f"pacc{i}", [128, 512], F32) for i in range(2)], excl=True)
        oT_r = Ring([S.sbuf(f"oT{i}", [64, 512], F32) for i in range(2)])
        heads = {}

        def ldh(h):
            q, rq = qT.next()
            k, rk = kT.next()
            v, rv = vt.next()
            P.dma(q[:], fmT[h * 64:(h + 1) * 64, :], writes=[rq])
            P.dma(k[:], fmT[512 + h * 64:512 + (h + 1) * 64, :], writes=[rk])
            P.dma(v[:], tmv[:, h * 64:(h + 1) * 64].rearrange("(kb p) d -> p kb d", p=128), writes=[rv])
            heads[h] = (q, rq, k, rk, v, rv)

        units = []
        for h in range(8):
            for c in range(8):
                for kb in range(4 * c + 3, -1, -1):
                    units.append(dict(h=h, c=c, kb=kb, first=(kb == 4 * c + 3), last=(kb == 0)))

        def stage1(u):
            h, c, kb = u["h"], u["c"], u["kb"]
            if u["first"] and c == 0:
                if h == 0:
                    ldh(0)
                if h + 1 < 8:
                    ldh(h + 1)
            q, rq, k, rk, v, rv = heads[h]
            i = kb - 4 * c
            q0 = 128 * i if i > 0 else 0
            u["q0"] = q0
            ps, r_ps = pS.next()
            P.pe.op(lambda e: e.matmul(ps[:, q0:512], lhsT=k[:, kb * 128:(kb + 1) * 128],
                                       rhs=q[:, c * 512 + q0:(c + 1) * 512], start=True, stop=True),
                    reads=[rq, rk], writes=[r_ps])
            et, r_e = e_r.next()
            P.act.op(lambda e: e.activation(out=et[:, q0:512], in_=ps[:, q0:512], func=AF.Exp, scale=0.125),
                     reads=[r_ps], writes=[r_e])
            if i >= 0:
                P.dve.op(lambda e: e.tensor_tensor(out=et[:, q0:q0 + 128], in0=et[:, q0:q0 + 128],
                                                   in1=tri["tri_lt"][:], op=ALU.mult),
                         reads=[r_e, r_tri], writes=[r_e])
            spt, r_sp = sp_r.next()
            P.act.op(lambda e: e.activation(out=spt[:, q0:512], in_=et[:, q0:512], func=AF.Ln, bias=1.0, scale=1.0),
                     reads=[r_e], writes=[r_sp])
            u.update(et=et, r_e=r_e, spt=spt, r_sp=r_sp)

        chain = {}

        def stage2(u):
            q0 = u["q0"]
            if u["first"]:
                chain["p2"], chain["r_p2"] = p2.next()
                chain["prev"] = None
            pp, r_pp = chain["p2"], chain["r_p2"]
            prev = chain["prev"]
            if prev is not None:
                pq0 = prev["q0"]
                P.pe.op(lambda e: e.matmul(pp[:, pq0:512], lhsT=tri["tri_lt"][:], rhs=prev["spt"][:, pq0:512],
                                           start=False, stop=False, skip_group_check=True),
                        reads=[prev["r_sp"], r_tri], writes=[r_pp])
            P.pe.op(lambda e: e.matmul(pp[:, q0:512], lhsT=tri["tri_ge"][:], rhs=u["spt"][:, q0:512],
                                       start=(prev is None), stop=True, skip_group_check=True),
                    reads=[u["r_sp"], r_tri], writes=[r_pp])
            chain["prev"] = u
            pd, r_pd = pD.next()
            P.act.op(lambda e: e.activation(out=pd[:, q0:512], in_=pp[:, q0:512], func=AF.Exp, scale=-1.0),
                     reads=[r_pp], writes=[r_pd])
            wt, r_w = w_r.next()
            P.dve.op(lambda e: e.tensor_tensor(out=wt[:, q0:512], in0=u["et"][:, q0:512], in1=pd[:, q0:512],
                                               op=ALU.mult),
                     reads=[u["r_e"], r_pd], writes=[r_w])
            u.update(wt=wt, r_w=r_w)

        accs = {}

        pend_t = []

        def flush_tr(force=False):
            while pend_t and (force or pend_t[0][0] <= 0):
                _, oT, r_oT, acc, r_acc, h, c = pend_t.pop(0)
                for ts in range(4):
                    P.pe.op(lambda e: e.transpose(out=acc[:, ts * 64:(ts + 1) * 64], in_=oT[:, ts * 128:(ts + 1) * 128],
                                                  identity=ident_f[0:64, 0:64]),
                            reads=[r_oT, r_identf], writes=[r_acc])
                ot, r_o = o_r.next()
                P.dve.op(lambda e: e.tensor_copy(out=ot[:].rearrange("p a b -> p (a b)"), in_=acc[:, 0:256]),
                         reads=[r_acc], writes=[r_o])
                P.dma(o_sb_d[c * 512:(c + 1) * 512, h * 64:(h + 1) * 64].rearrange("(s p) d -> p s d", p=128),
                      ot[:], reads=[r_o])
            for p_ in pend_t:
                p_[0] -= 1

        def stage3(u):
            h, c, kb, q0 = u["h"], u["c"], u["kb"], u["q0"]
            q, rq, k, rk, v, rv = heads[h]
            flush_tr()
            if u["first"]:
                accs["a"], accs["r"] = pacc.next()
            acc, r_acc = accs["a"], accs["r"]
            P.pe.op(lambda e: e.matmul(acc[0:64, q0:512], lhsT=v[:, kb, :], rhs=u["wt"][:, q0:512],
                                       start=u["first"], stop=u["last"], skip_group_check=True),
                    reads=[u["r_w"], rv], writes=[r_acc])
            if u["last"]:
                oT, r_oT = oT_r.next()
                P.dve.op(lambda e: e.tensor_copy(out=oT[:], in_=acc[0:64, :]), reads=[r_acc], writes=[r_oT])
                pend_t.append([2, oT, r_oT, acc, r_acc, h, c])

        n = len(units)
        for it in range(n + 2):
            if it < n:
                stage1(units[it])
            if 0 <= it - 1 < n:
                stage2(units[it - 1])
            if 0 <= it - 2 < n:
                stage3(units[it - 2])
        flush_tr(force=True)
        S.close()


    def phase4(l):
        S = Scope(P)
        qs = [S.sbuf(f"qs{g}", [128, 4, T], BF16) for g in range(2)]
        kse = [S.sbuf(f"kse{g}", [128, T], BF16) for g in range(2)]
        kw2 = S.sbuf("kw2", [64, 2, T], BF16)
        vs1 = S.sbuf("vs1", [128, 2, NT, 65], BF16)
        vw1 = S.sbuf("vw1", [128, 2, NT, 65], BF16)
        kcr = S.sbuf("kcr", [64, 2, 256], BF16)
        vov = S.sbuf("vov", [128, 2, 2, 65], BF16)
        ovT = S.sbuf("ovT_sb", [128, 2, 64], BF16)
        cmaskb = S.sbuf("cmaskb_sb", [128, 17, 128], BF16)
        cb_le = S.sbuf("cb_le_sb", [128, 128], BF16)
        cb_gt = S.sbuf("cb_gt_sb", [128, 128], BF16)
        rkeep = S.sbuf("rkeep_sb", [128, 128], F32)
        radd = S.sbuf("radd_sb", [128, 128], F32)
        r_q = [Res(), Res()]
        r_kse = [Res(), Res()]
        r_kw, r_vs, r_vw, r_kcr, r_vov, r_c4 = (Res() for _ in range(6))
        for g in range(2):
            P.dma(qs[g][0:64, :, :], fmT[1024 + 256 * g:1024 + 256 * (g + 1), :].rearrange("(hh d) t -> d hh t", d=64),
                  writes=[r_q[g]])
            P.dma_group([(kse[g][0:64, :], fmT[1536 + 64 * g:1536 + 64 * (g + 1), :], None, None),
                         (kse[g][64:128, :], eexp_d[:, :], None, None)], writes=[r_kse[g]])
        P.dma_group([(kw2[:, g, :], fmT[1664 + 64 * g:1664 + 64 * (g + 1), :], None, None) for g in range(2)], writes=[r_kw])
        P.pool.op(lambda e: e.memset(vs1[:], 1.0), writes=[r_vs])
        P.pool.op(lambda e: e.memset(vw1[:], 1.0), writes=[r_vw])
        P.pool.op(lambda e: e.memset(vov[:], 1.0), writes=[r_vov])
        P.dma_group([(vs1[:, g, :, 0:64],
                      tmv[:, 512 + 64 * g:512 + 64 * (g + 1)].rearrange("(kb p) d -> p kb d", p=128), None, None)
                     for g in range(2)], writes=[r_vs])
        P.dma_group([(vw1[:, g, :, 0:64],
                      tmv[:, 640 + 64 * g:640 + 64 * (g + 1)].rearrange("(kb p) d -> p kb d", p=128), None, None)
                     for g in range(2)], writes=[r_vw])
        P.dma_group([(ovT[:], ovT_d[:, :, :], None, None), (cmaskb[:], cmaskb_d[:, :, :], None, None),
                     (cb_le[:], cb_le_d[:, :], None, None), (cb_gt[:], cb_gt_d[:, :], None, None),
                     (rkeep[:], rkeep_d[:, :], None, None), (radd[:], radd_d[:, :], None, None)], writes=[r_c4])

        S2 = Scope(P)
        kcT = S2.sbuf("kcT", [128, T], BF16)
        vcT = S2.sbuf("vcT", [128, T], BF16)
        w1 = {"k": S2.sbuf("w1k", [128, 32, 256], BF16), "v": S2.sbuf("w1v", [128, 32, 256], BF16)}
        w2k = S2.sbuf("w2k", [128, 2, 64], BF16)
        w2ks = S2.sbuf("w2ks", [128, 2, 64], BF16)
        w2v = S2.sbuf("w2v", [128, 2, 64], BF16)
        posT = {"k": S2.sbuf("posTk_sb", [64, 32], BF16), "v": S2.sbuf("posTv_sb", [64, 32], BF16)}
        ccos = S2.sbuf("ccos", [128, 256], F32)
        csin = S2.sbuf("csin", [128, 256], F32)
        bias_sb = S2.sbuf("cbias", [128, 4], F32)
        r_x2, r_w1, r_w2, r_pos, r_cc, r_bias = (Res() for _ in range(6))
        P.dma_group([(kcT[:], fmT[1792:1920, :], None, None), (vcT[:], fmT[1920:2048, :], None, None)], writes=[r_x2])
        srcs = {"k": w1k_d, "v": w1v_d}
        P.dma_group([(w1[kd][64 * hf:64 * hf + 64, :, :], srcs[kd][l].rearrange("(l d) h -> d l h", d=64), P.pool, None)
                     for kd in ("k", "v") for hf in range(2)], writes=[r_w1])
        P.dma_group([(w2k[:], w2k_d[l].rearrange("(hc p) d -> p hc d", p=128), P.pool, None),
                     (w2ks[:], w2ksw_d[l].rearrange("(hc p) d -> p hc d", p=128), P.pool, None),
                     (w2v[:], w2v_d[l].rearrange("(hc p) d -> p hc d", p=128), P.pool, None)], writes=[r_w2])
        P.dma_group([(posT["k"][:], posTk_d[l], P.pool, None), (posT["v"][:], posTv_d[l], P.pool, None)], writes=[r_pos])
        P.dma_group([(ccos[:], cmp_cos_d[:, :], None, None), (csin[:], cmp_sin_d[:, :], None, None)], writes=[r_cc])
        pb = S2.psum("pb", [128, 4], F32)
        r_pb = Res(True)
        first = True
        for ki, kd in enumerate(("k", "v")):
            for hc in range(2):
                for ll in range(32):
                    P.pe.op(lambda e: e.matmul(pb[:, 2 * ki + hc:2 * ki + hc + 1],
                                               lhsT=w1[kd][0:64, ll, hc * 128:(hc + 1) * 128],
                                               rhs=posT[kd][:, ll:ll + 1], start=first, stop=False,
                                               skip_group_check=True),
                            reads=[r_w1, r_pos], writes=[r_pb])
                    first = False
        P.dve.op(lambda e: e.tensor_copy(out=bias_sb[:], in_=pb[:]), reads=[r_pb], writes=[r_bias])
        ph = Ring([S2.psum(f"ph{i}", [128, 256], F32) for i in range(2)], excl=True)
        pk = Ring([S2.psum(f"pk{i}", [128, 256], F32) for i in range(2)], excl=True)
        u_r = Ring([S2.sbuf(f"cu{i}", [128, 256], F32) for i in range(2)])
        t_r = Ring([S2.sbuf(f"ct{i}", [128, 256], F32) for i in range(2)])
        g_r = [S2.sbuf(f"cg{i}", [128, 256], BF16) for i in range(4)]
        r_g = [Res() for _ in range(4)]
        for i in range(4):
            P.pool.op(lambda e: e.memset(g_r[i][:], 0.0), writes=[r_g[i]])
        gi = 0
        xsrc = {"k": kcT, "v": vcT}
        for ki, kd in enumerate(("k", "v")):
            for g in range(2):
                gts = []
                for hc in range(2):
                    pht, r_ph = ph.next()
                    for ll in range(32):
                        P.pe.op(lambda e: e.matmul(pht[:, 0:255], lhsT=w1[kd][64 * g:64 * g + 64, ll, hc * 128:(hc + 1) * 128],
                                                   rhs=xsrc[kd][64 * g:64 * g + 64, ll:ll + 16 * 254 + 1:16],
                                                   start=(ll == 0), stop=(ll == 31)),
                                reads=[r_w1, r_x2], writes=[r_ph])
                    ut, r_u = u_r.next()
                    P.act.op(lambda e: e.activation(out=ut[:, 0:255], in_=pht[:, 0:255], func=AF.Identity,
                                                    bias=bias_sb[:, 2 * ki + hc:2 * ki + hc + 1], scale=1.0),
                             reads=[r_ph, r_bias], writes=[r_u])
                    tt_, r_t = t_r.next()
                    P.dve.op(lambda e: e.tensor_tensor(out=tt_[:, 0:255], in0=ut[:, 0:255], in1=ut[:, 0:255], op=ALU.mult),
                             reads=[r_u], writes=[r_t])
                    P.dve.op(lambda e: e.tensor_scalar(out=tt_[:, 0:255], in0=tt_[:, 0:255], scalar1=0.044715, scalar2=1.0,
                                                       op0=ALU.mult, op1=ALU.add), reads=[r_t], writes=[r_t])
                    P.dve.op(lambda e: e.tensor_tensor(out=tt_[:, 0:255], in0=tt_[:, 0:255], in1=ut[:, 0:255], op=ALU.mult),
                             reads=[r_t, r_u], writes=[r_t])
                    P.act.op(lambda e: e.activation(out=tt_[:, 0:255], in_=tt_[:, 0:255], func=AF.Sigmoid,
                                                    scale=1.5957691216057308), reads=[r_t], writes=[r_t])
                    gt_, r_gt = g_r[gi % 4], r_g[gi % 4]
                    gi += 1
                    P.dve.op(lambda e: e.tensor_tensor(out=gt_[:, 0:255], in0=ut[:, 0:255], in1=tt_[:, 0:255], op=ALU.mult),
                             reads=[r_t, r_u], writes=[r_gt])
                    gts.append((gt_, r_gt))
                if kd == "k":
                    pa, r_pa = pk.next()
                    pb2, r_pb2 = pk.next()
                    for hc in range(2):
                        P.pe.op(lambda e: e.matmul(pa[0:64, :], lhsT=w2k[:, hc, :], rhs=gts[hc][0][:], start=(hc == 0), stop=(hc == 1)),
                                reads=[r_w2, gts[hc][1]], writes=[r_pa])
                    for hc in range(2):
                        P.pe.op(lambda e: e.matmul(pb2[0:64, :], lhsT=w2ks[:, hc, :], rhs=gts[hc][0][:], start=(hc == 0), stop=(hc == 1)),
                                reads=[r_w2, gts[hc][1]], writes=[r_pb2])
                    t1, r_t1 = u_r.next()
                    t2, r_t2 = t_r.next()
                    P.dve.op(lambda e: e.tensor_tensor(out=t1[0:64, :], in0=pa[0:64, :], in1=ccos[0:64, :], op=ALU.mult),
                             reads=[r_pa, r_cc], writes=[r_t1])
                    P.dve.op(lambda e: e.tensor_tensor(out=t2[0:64, :], in0=pb2[0:64, :], in1=csin[0:64, :], op=ALU.mult),
                             reads=[r_pb2, r_cc], writes=[r_t2])
                    P.dve.op(lambda e: e.tensor_tensor(out=kcr[:, g, :], in0=t1[0:64, :], in1=t2[0:64, :], op=ALU.add),
                             reads=[r_t1, r_t2], writes=[r_kcr])
                else:
                    for nn in range(2):
                        pv, r_pv = pk.next()
                        for hc in range(2):
                            P.pe.op(lambda e: e.matmul(pv[:, 0:64], lhsT=gts[hc][0][:, nn * 128:(nn + 1) * 128],
                                                       rhs=w2v[:, hc, :], start=(hc == 0), stop=(hc == 1)),
                                    reads=[r_w2, gts[hc][1]], writes=[r_pv])
                        P.dve.op(lambda e: e.tensor_copy(out=vov[:, g, nn, 0:64], in_=pv[:, 0:64]),
                                 reads=[r_pv], writes=[r_vov])
        S2.close()
        P.barrier()

        pSr = Ring([S.psum(f"pSn{i}", [128, 4, 128], F32) for i in range(3)], excl=True)
        pOc = S.psum("pOc", [128, 4, 65], F32)
        pImp = S.psum("pImp", [128, 4, 64], F32)
        pOs = S.psum("pOs", [128, 4, 65], F32)
        pOw = S.psum("pOw", [128, 4, 65], F32)
        pMTb = S.psum("pMT", [128, 1024], BF16)
        r_pOc, r_pImp, r_pOs, r_pOw, r_pT4 = (Res(True) for _ in range(5))
        ex_r = Ring([S.sbuf(f"ex{i}", [128, 4, 128], BF16) for i in range(4)])
        sm_r = Ring([S.sbuf(f"sm{i}", [128, 32], F32) for i in range(2)])
        oc_r = Ring([S.sbuf(f"oc{i}", [128, 4, 64], F32) for i in range(2)])
        imp_r = Ring([S.sbuf(f"imp{i}", [128, 64], F32) for i in range(2)])
        sc_r = Ring([S.sbuf(f"sc{i}", [128, 64], F32) for i in range(2)])
        sc2_r = Ring([S.sbuf(f"sc2{i}", [128, 64], F32) for i in range(2)])
        m8_r = Ring([S.sbuf(f"m8{i}", [128, 16], F32) for i in range(2)])
        sel_r = Ring([S.sbuf(f"sel{i}", [128, 128], BF16) for i in range(2)])
        for i in range(2):
            P.pool.op(lambda e: e.memset(sel_r.tiles[i][:], 0.0), writes=[sel_r.res[i]])
        gt_r = Ring([S.sbuf(f"gt{i}", [128, 12], F32) for i in range(2)])
        cf_r = Ring([S.sbuf(f"cf{i}", [128, 16], F32) for i in range(2)])
        ta_r = Ring([S.sbuf(f"ta{i}", [128, 4, 64], F32) for i in range(2)])
        tb_r = Ring([S.sbuf(f"tb{i}", [128, 4, 64], F32) for i in range(2)])
        on_r = Ring([S.sbuf(f"on{i}", [128, 4, 64], F32) for i in range(2)])
        BIG = 240000.0

        def bc(ap, shape):
            return ap.to_broadcast(shape)

        def prologue(g, qt):
            st = {}
            qsl = qs[g][0:64, :, qt * 128:(qt + 1) * 128]
            gt_, r_gt = gt_r.next()
            P.dma(gt_[:], gates_d[qt * 128:(qt + 1) * 128, 12 * g:12 * (g + 1)], writes=[r_gt])
            st["gt"], st["r_gt"] = gt_, r_gt
            nns = [0] if qt < 16 else [0, 1]
            exs = []
            for nn in nns:
                ps, r_ps = pSr.next()
                qp = qt - 16 * nn
                msk = qp <= 16
                P.pe.op(lambda e: e.matmul(ps[:], lhsT=kcr[:, g, nn * 128:(nn + 1) * 128], rhs=qsl, start=True, stop=not msk),
                        reads=[r_kcr, r_q[g]], writes=[r_ps])
                if msk:
                    P.pe.op(lambda e: e.matmul(ps[:], lhsT=ident_bf[:], rhs=bc(cmaskb[:, qp:qp + 1, :], [128, 4, 128]),
                                               start=False, stop=True), reads=[r_ident, r_c4], writes=[r_ps])
                ex, r_ex = ex_r.next()
                P.act.op(lambda e: e.activation(out=ex[:], in_=ps[:], func=AF.Exp, scale=0.125), reads=[r_ps], writes=[r_ex])
                exs.append((nn, ex, r_ex))
            firstc = True
            for (nn, ex, r_ex) in exs:
                for hh in range(4):
                    P.pe.op(lambda e: e.matmul(pOc[:, hh, :], lhsT=ex[:, hh, :], rhs=vov[:, g, nn, :],
                                               start=firstc, stop=False, skip_group_check=True),
                            reads=[r_ex, r_vov], writes=[r_pOc])
                    firstc = False
            firstc = True
            for (nn, ex, r_ex) in exs:
                for hh in range(4):
                    P.pe.op(lambda e: e.matmul(pImp[:, hh, :], lhsT=ex[:, hh, :], rhs=ovT[:, nn, :],
                                               start=firstc, stop=False, skip_group_check=True),
                            reads=[r_ex, r_c4], writes=[r_pImp])
                    firstc = False
            sm, r_sm = sm_r.next()
            P.dve.op(lambda e: e.tensor_scalar(out=sm[:, 0:4], in0=pOc[:, :, 64], scalar1=1e-6, scalar2=None, op0=ALU.max),
                     reads=[r_pOc], writes=[r_sm])
            P.dve.op(lambda e: e.reciprocal(out=sm[:, 4:8], in_=sm[:, 0:4]), reads=[r_sm], writes=[r_sm])
            oc, r_oc = oc_r.next()
            P.dve.op(lambda e: e.tensor_tensor(out=oc[:], in0=pOc[:, :, 0:64], in1=bc(sm[:, 4:8].unsqueeze(2), [128, 4, 64]),
                                               op=ALU.mult), reads=[r_pOc, r_sm], writes=[r_oc])
            st["oc"], st["r_oc"] = oc, r_oc
            imp, r_imp = imp_r.next()
            P.dve.op(lambda e: e.tensor_scalar(out=imp[:], in0=pImp[:, 0, :], scalar1=sm[:, 4:5], scalar2=None, op0=ALU.mult),
                     reads=[r_pImp, r_sm], writes=[r_imp])
            for hh in range(1, 4):
                P.dve.op(lambda e: e.scalar_tensor_tensor(out=imp[:], in0=pImp[:, hh, :], scalar=sm[:, 4 + hh:5 + hh],
                                                          in1=imp[:], op0=ALU.mult, op1=ALU.add),
                         reads=[r_pImp, r_sm, r_imp], writes=[r_imp])
            sc, r_sc = sc_r.next()
            o0 = 62 - 2 * qt
            P.dve.op(lambda e: e.tensor_tensor(out=sc[:], in0=imp[:], in1=rkeep[:, o0:o0 + 64], op=ALU.mult),
                     reads=[r_imp, r_c4], writes=[r_sc])
            P.dve.op(lambda e: e.tensor_tensor(out=sc[:], in0=sc[:], in1=radd[:, o0:o0 + 64], op=ALU.add),
                     reads=[r_sc, r_c4], writes=[r_sc])
            P.dve.op(lambda e: e.memset(sc[:, 0:1], 1e6), reads=[r_sc], writes=[r_sc])
            m8, r_m8 = m8_r.next()
            sc2, r_sc2 = sc2_r.next()
            P.dve.op(lambda e: e.max(out=m8[:, 0:8], in_=sc[:]), reads=[r_sc], writes=[r_m8])
            P.dve.op(lambda e: e.match_replace(out=sc2[:], in_to_replace=m8[:, 0:8], in_values=sc[:], imm_value=-1e30),
                     reads=[r_sc, r_m8], writes=[r_sc2])
            P.dve.op(lambda e: e.max(out=m8[:, 8:16], in_=sc2[:]), reads=[r_sc2, r_m8], writes=[r_m8])
            sel, r_sel = sel_r.next()
            P.dve.op(lambda e: e.tensor_scalar(out=sel[:, 64:128], in0=sc[:], scalar1=m8[:, 15:16], scalar2=-BIG,
                                               op0=ALU.is_lt, op1=ALU.mult),
                     reads=[r_sc, r_m8], writes=[r_sel])
            st["sel"], st["r_sel"] = sel, r_sel
            st["r_sb"] = Res()
            return st

        def prologue_b(g, qt, st):
            sel, r_sel = st["sel"], st["r_sel"]
            P.pe.op(lambda e: e.transpose(out=pMTb[:, 0:128], in_=sel[:], identity=ident_bf[:]),
                    reads=[r_sel, r_ident], writes=[r_pT4])
            r_sb = st["r_sb"]
            P.dve.op(lambda e: e.tensor_copy(out=qs[g][64:128, :, qt * 128:(qt + 1) * 128],
                                             in_=bc(pMTb[64:128, 0:128].unsqueeze(1), [64, 4, 128])),
                     reads=[r_pT4], writes=[r_sb])

        def attend(g, qt, st):
            units = [("s", kb) for kb in range(qt + 1)] + [("w", kb) for kb in range(max(0, qt - 4), qt + 1)]
            pend = []

            def s1(kind, kb):
                ps, r_ps = pSr.next()
                if kind == "s":
                    diag = (kb == qt)
                    P.pe.op(lambda e: e.matmul(ps[:], lhsT=kse[g][:, kb * 128:(kb + 1) * 128],
                                               rhs=qs[g][:, :, qt * 128:(qt + 1) * 128], start=True, stop=not diag),
                            reads=[r_kse[g], r_q[g], st["r_sb"]], writes=[r_ps])
                    if diag:
                        P.pe.op(lambda e: e.matmul(ps[:], lhsT=ident_bf[:], rhs=bc(cb_le[:].unsqueeze(1), [128, 4, 128]),
                                                   start=False, stop=True), reads=[r_ident, r_c4], writes=[r_ps])
                else:
                    m = cb_le if kb == qt else (cb_gt if kb == qt - 4 else None)
                    P.pe.op(lambda e: e.matmul(ps[:], lhsT=kw2[:, g, kb * 128:(kb + 1) * 128],
                                               rhs=qs[g][0:64, :, qt * 128:(qt + 1) * 128], start=True, stop=(m is None)),
                            reads=[r_kw, r_q[g]], writes=[r_ps])
                    if m is not None:
                        P.pe.op(lambda e: e.matmul(ps[:], lhsT=ident_bf[:], rhs=bc(m[:].unsqueeze(1), [128, 4, 128]),
                                                   start=False, stop=True), reads=[r_ident, r_c4], writes=[r_ps])
                ex, r_ex = ex_r.next()
                P.act.op(lambda e: e.activation(out=ex[:], in_=ps[:], func=AF.Exp, scale=0.125), reads=[r_ps], writes=[r_ex])
                return (kind, kb, ex, r_ex)

            def s2(kind, kb, ex, r_ex):
                if kind == "s":
                    po, r_po, v1, r_v1, first = pOs, r_pOs, vs1, r_vs, (kb == 0)
                else:
                    po, r_po, v1, r_v1, first = pOw, r_pOw, vw1, r_vw, (kb == max(0, qt - 4))
                for hh in range(4):
                    P.pe.op(lambda e: e.matmul(po[:, hh, :], lhsT=ex[:, hh, :], rhs=v1[:, g, kb, :],
                                               start=(first and hh == 0), stop=False, skip_group_check=True),
                            reads=[r_ex, r_v1], writes=[r_po])

            for i in range(len(units) + 2):
                if i < len(units):
                    pend.append(s1(*units[i]))
                if 0 <= i - 2 < len(units):
                    s2(*pend[i - 2])

        def combine(g, qt, st):
            gt_, r_gt = st["gt"], st["r_gt"]
            gv = gt_[:, 0:12].rearrange("p (h b) -> p h b", b=3)
            cf, r_cf = cf_r.next()
            P.dve.op(lambda e: e.reciprocal(out=cf[:, 0:4], in_=pOs[:, :, 64]), reads=[r_pOs], writes=[r_cf])
            P.dve.op(lambda e: e.reciprocal(out=cf[:, 4:8], in_=pOw[:, :, 64]), reads=[r_pOw, r_cf], writes=[r_cf])
            P.dve.op(lambda e: e.tensor_tensor(out=cf[:, 8:12], in0=cf[:, 0:4], in1=gv[:, :, 1], op=ALU.mult),
                     reads=[r_cf, r_gt], writes=[r_cf])
            P.dve.op(lambda e: e.tensor_tensor(out=cf[:, 12:16], in0=cf[:, 4:8], in1=gv[:, :, 2], op=ALU.mult),
                     reads=[r_cf, r_gt], writes=[r_cf])
            ta, r_ta = ta_r.next()
            tb, r_tb = tb_r.next()
            on, r_on = on_r.next()
            P.dve.op(lambda e: e.tensor_tensor(out=ta[:], in0=pOs[:, :, 0:64], in1=bc(cf[:, 8:12].unsqueeze(2), [128, 4, 64]),
                                               op=ALU.mult), reads=[r_pOs, r_cf], writes=[r_ta])
            P.dve.op(lambda e: e.tensor_tensor(out=tb[:], in0=pOw[:, :, 0:64], in1=bc(cf[:, 12:16].unsqueeze(2), [128, 4, 64]),
                                               op=ALU.mult), reads=[r_pOw, r_cf], writes=[r_tb])
            P.pool.op(lambda e: e.tensor_tensor(out=on[:], in0=st["oc"][:], in1=bc(gv[:, :, 0:1], [128, 4, 64]), op=ALU.mult),
                      reads=[st["r_oc"], r_gt], writes=[r_on])
            P.pool.op(lambda e: e.tensor_tensor(out=ta[:], in0=ta[:], in1=tb[:], op=ALU.add), reads=[r_ta, r_tb], writes=[r_ta])
            P.pool.op(lambda e: e.tensor_tensor(out=on[:], in0=on[:], in1=ta[:], op=ALU.add), reads=[r_on, r_ta], writes=[r_on])
            P.dma(o_nsa_d[qt * 128:(qt + 1) * 128, 256 * g:256 * (g + 1)], on[:].rearrange("p h d -> p (h d)"), reads=[r_on])

        tiles = [(g, qt) for g in range(2) for qt in range(NT)]
        if nsa_tiles is not None:
            tiles = nsa_tiles
        stn = prologue(*tiles[0])
        prologue_b(*tiles[0], stn)
        for i, (g, qt) in enumerate(tiles):
            stc = stn
            if i + 1 < len(tiles):
                stn = prologue(*tiles[i + 1])
            attend(g, qt, stc)
            if i + 1 < len(tiles):
                prologue_b(*tiles[i + 1], stn)
            combine(g, qt, stc)
        S.close()

    def rstd_ops(st, r_st, c0, n, dim):
        P.act.op(lambda e: e.activation(out=st[:, c0 + n:c0 + 2 * n], in_=st[:, c0:c0 + n], func=AF.Ln,
                                        scale=1.0 / dim, bias=eps_t[:, 0:1]), reads=[r_st, r_eps], writes=[r_st])
        P.act.op(lambda e: e.activation(out=st[:, c0 + 2 * n:c0 + 3 * n], in_=st[:, c0 + n:c0 + 2 * n], func=AF.Exp,
                                        scale=-0.5), reads=[r_st], writes=[r_st])

    def phase5a(l, x_src):
        S = Scope(P)
        wo = S.sbuf("wo_sb", [128, 8, D], BF16)
        r_wo = Res()
        P.dma_group([(wo[:, k, :], w_out_d[l, k * 128:(k + 1) * 128, :], P.pool, None) for k in range(8)], writes=[r_wo])
        gbc = S.sbuf("gbc", [128, D], F32)
        g1bc = S.sbuf("g1bc", [128, D], F32)
        r_bc = Res()
        P.dma_group([(gbc[:, 0:512], sbg_d[l].partition_broadcast(128), None, None),
                     (gbc[:, 512:1024], nsag_d[l].partition_broadcast(128), None, None),
                     (g1bc[:], modrow[l, 2 * D:3 * D].partition_broadcast(128), None, None)], writes=[r_bc])
        oin = Ring([S.sbuf(f"oin{i}", [128, D], F32) for i in range(3)])
        xin = Ring([S.sbuf(f"x5_{i}", [128, D], F32) for i in range(8)])
        junk = S.sbuf("junk5", [128, D], F32)
        r_junk = Res()
        stat = Ring([S.sbuf(f"st5_{i}", [128, 12], F32) for i in range(12)])
        mixed = Ring([S.sbuf(f"mixed{i}", [128, D], BF16) for i in range(3)])
        mixT = Ring([S.sbuf(f"mixT{i}", [128, 8, 128], BF16) for i in range(3)])
        tmp = Ring([S.sbuf(f"tmp5_{i}", [128, D], F32) for i in range(5)])
        xn2 = Ring([S.sbuf(f"xn2_{i}", [128, D], BF16) for i in range(3)])
        h2 = Ring([S.sbuf(f"h2_{i}", [128, 8, 512], BF16) for i in range(2)])
        pT_r = Ring([S.psum(f"pT5{i}", [128, 8, 128], BF16) for i in range(4)], excl=True)
        pa_r = Ring([S.psum(f"pa5{i}", [128, 512], F32) for i in range(4)], excl=True)
        B2 = modT[l][:, 24:32]
        ld = {}

        def load(ti):
            o, r_o = oin.next()
            xt, r_x = xin.next()
            rows = slice(ti * 128, (ti + 1) * 128)
            P.dma_group([(o[:, 0:512], o_sb_d[rows, :], None, None), (o[:, 512:1024], o_nsa_d[rows, :], None, None)],
                        writes=[r_o])
            P.dma(xt[:], x_src[rows, :], writes=[r_x])
            ld[ti] = (o, r_o, xt, r_x)

        def g1(ti, d):
            o, r_o, xt, r_x = ld.pop(ti)
            if ti + 1 < NT:
                load(ti + 1)
            st, r_st = stat.next()
            for hf in range(2):
                P.act.op(lambda e: e.activation(out=junk[:, 0:512], in_=o[:, hf * 512:(hf + 1) * 512], func=AF.Square,
                                                accum_out=st[:, hf:hf + 1]), reads=[r_o], writes=[r_junk, r_st])
            rstd_ops(st, r_st, 0, 2, 512)
            d.update(o=o, r_o=r_o, xt=xt, r_x=r_x, st=st, r_st=r_st)

        def g2(ti, d):
            o, r_o, st, r_st = d["o"], d["r_o"], d["st"], d["r_st"]
            mx, r_mx = mixed.next()
            for hf in range(2):
                P.dve.op(lambda e: e.scalar_tensor_tensor(out=mx[:, hf * 512:(hf + 1) * 512], in0=o[:, hf * 512:(hf + 1) * 512],
                                                          scalar=st[:, 4 + hf:5 + hf], in1=gbc[:, hf * 512:(hf + 1) * 512],
                                                          op0=ALU.mult, op1=ALU.mult),
                         reads=[r_o, r_st, r_bc], writes=[r_mx])
            d.update(mx=mx, r_mx=r_mx)

        def g3(ti, d):
            mx, r_mx = d["mx"], d["r_mx"]
            pT, r_pT = pT_r.next()
            for k in range(8):
                P.pe.op(lambda e: e.transpose(out=pT[:, k, :], in_=mx[:, k * 128:(k + 1) * 128], identity=ident_bf[:]),
                        reads=[r_mx, r_ident], writes=[r_pT])
            d.update(pT=pT, r_pT=r_pT)

        def g4(ti, d):
            mT, r_mT = mixT.next()
            P.act.op(lambda e: e.activation(out=mT[:], in_=d["pT"][:], func=AF.Copy), reads=[d["r_pT"]], writes=[r_mT])
            d.update(mT=mT, r_mT=r_mT)

        def g5(ti, d):
            mT, r_mT = d["mT"], d["r_mT"]
            pas = []
            for hf in range(2):
                pa, r_pa = pa_r.next()
                for k in range(8):
                    P.pe.op(lambda e: e.matmul(pa[:], lhsT=mT[:, k, :], rhs=wo[:, k, hf * 512:(hf + 1) * 512],
                                               start=(k == 0), stop=(k == 7)), reads=[r_mT, r_wo], writes=[r_pa])
                pas.append((pa, r_pa))
            d.update(pas=pas)

        def g6(ti, d):
            tm_, r_tm = tmp.next()
            for hf in range(2):
                pa, r_pa = d["pas"][hf]
                P.dve.op(lambda e: e.tensor_tensor(out=tm_[:, hf * 512:(hf + 1) * 512], in0=pa[:],
                                                   in1=g1bc[:, hf * 512:(hf + 1) * 512], op=ALU.mult),
                         reads=[r_pa, r_bc], writes=[r_tm])
            d.update(tm=tm_, r_tm=r_tm)

        def g7(ti, d):
            rows = slice(ti * 128, (ti + 1) * 128)
            tm_, r_tm, xt, r_x = d["tm"], d["r_tm"], d["xt"], d["r_x"]
            P.pool.op(lambda e: e.tensor_tensor(out=tm_[:], in0=tm_[:], in1=xt[:], op=ALU.add),
                      reads=[r_tm, r_x], writes=[r_tm])
            P.dma(x1_d[rows, :], tm_[:], reads=[r_tm])

        def g8(ti, d):
            tm_, r_tm, st, r_st = d["tm"], d["r_tm"], d["st"], d["r_st"]
            P.act.op(lambda e: e.activation(out=junk[:], in_=tm_[:], func=AF.Square, accum_out=st[:, 6:7]),
                     reads=[r_tm], writes=[r_junk, r_st])
            rstd_ops(st, r_st, 6, 1, D)

        def g9(ti, d):
            tm_, r_tm, st, r_st = d["tm"], d["r_tm"], d["st"], d["r_st"]
            xn, r_xn = xn2.next()
            P.dve.op(lambda e: e.tensor_scalar(out=xn[:], in0=tm_[:], scalar1=st[:, 8:9], scalar2=None, op0=ALU.mult),
                     reads=[r_tm, r_st], writes=[r_xn])
            d.update(xn=xn, r_xn=r_xn)

        def g10(ti, d):
            xn, r_xn = d["xn"], d["r_xn"]
            pT, r_pT = pT_r.next()
            for k in range(8):
                P.pe.op(lambda e: e.transpose(out=pT[:, k, :], in_=xn[:, k * 128:(k + 1) * 128], identity=ident_bf[:]),
                        reads=[r_xn, r_ident], writes=[r_pT])
            d.update(pT=pT, r_pT=r_pT)

        def g11(ti, d):
            pT, r_pT = d["pT"], d["r_pT"]
            if ti % 4 == 0:
                h2cur[0] = h2.next()
            hh4, r_hh = h2cur[0]
            hh = hh4[:, :, (ti % 4) * 128:(ti % 4 + 1) * 128]
            for k in range(8):
                if ti % 2 == 0:
                    P.act.op(lambda e: e.activation(out=hh[:, k, :], in_=pT[:, k, :], func=AF.Identity,
                                                    scale=A2[l][:, k:k + 1], bias=B2[:, k:k + 1]),
                             reads=[r_pT, r_A[l], r_modT[l]], writes=[r_hh])
                else:
                    P.dve.op(lambda e: e.tensor_scalar(out=hh[:, k, :], in0=pT[:, k, :], scalar1=A2[l][:, k:k + 1],
                                                       scalar2=B2[:, k:k + 1], op0=ALU.mult, op1=ALU.add),
                             reads=[r_pT, r_A[l], r_modT[l]], writes=[r_hh])
            if ti % 4 == 3:
                g0 = (ti // 4) * 512
                P.dma(h2T_d[:, g0:g0 + 512].rearrange("(k p) t -> p k t", p=128), hh4[:], reads=[r_hh])

        h2cur = [None]
        load(0)
        stages = [g1, g2, g3, g4, g5, g6, g7, g8, g9, g10, g11]
        sts = {}
        for it in range(NT + len(stages) - 1):
            for si, fn in enumerate(stages):
                ti = it - si
                if 0 <= ti < NT:
                    if si == 0:
                        sts[ti] = {}
                    fn(ti, sts[ti])
                    if si == len(stages) - 1:
                        sts.pop(ti)
        S.close()

    GT = 256
    NG = T // GT

    def phase5b(l, x_dst, final):
        S = Scope(P)
        wf = S.sbuf("wf_sb", [128, 8, 2 * DFF], BF16)
        wd = S.sbuf("wd_sb", [128, 22, D], BF16)
        r_wd = Res()
        r_wfb = [Res() for _ in range(4)]
        for blk in (0, 2, 1, 3):
            c0 = blk * 1408
            P.dma_group([(wf[:, k, c0:c0 + 1408], wffn_d[l, k * 128:(k + 1) * 128, c0:c0 + 1408], P.pool, None)
                         for k in range(8)], writes=[r_wfb[blk]])
            if blk == 2:
                P.dma_group([(wd[:, fc, :], wdn_d[l, fc * 128:(fc + 1) * 128, :], P.pool, None) for fc in range(22)],
                            writes=[r_wd])
        cw = S.sbuf("cw_sb", [128, 44, 3], F32)
        cb = S.sbuf("cb_sb", [128, 44], F32)
        g2bc = S.sbuf("g2bc", [128, D], F32)
        fgbc = S.sbuf("fgbc", [128, D], F32)
        r_cp = Res()
        P.dma_group([(cw[:], cw_d[l], None, None), (cb[:], cb_d[l], None, None),
                     (g2bc[:], modrow[l, 5 * D:6 * D].partition_broadcast(128), None, None),
                     (fgbc[:], fg_d.partition_broadcast(128), None, None)], writes=[r_cp])
        hal = S.sbuf("halo", [128, 44, 2], F32)
        r_hal = Res()
        P.pool.op(lambda e: e.memset(hal[:], 0.0), writes=[r_hal])
        r_hals = [Res() for _ in range(44)]
        for rr in r_hals:
            rr.w = list(r_hal.w)
        r_uhs = [Res() for _ in range(4)]
        h2 = Ring([S.sbuf(f"h2g{i}", [128, 8, GT], BF16) for i in range(2)])
        gT = S.sbuf("gT", [128, 22, GT], BF16)
        r_gT = Res()
        us = Ring([S.sbuf(f"us{i}", [128, GT + 2], F32) for i in range(4)])
        ys = Ring([S.sbuf(f"ys{i}", [128, GT], F32) for i in range(6)])
        sa_r = Ring([S.sbuf(f"sa{i}", [128, GT], F32) for i in range(2)])
        x1t = Ring([S.sbuf(f"x1t{i}", [128, D], F32) for i in range(2)])
        tmp = Ring([S.sbuf(f"tmpb{i}", [128, D], F32) for i in range(2)])
        junk = S.sbuf("junkb", [128, D], F32)
        r_junk = Res()
        stat = Ring([S.sbuf(f"stb{i}", [128, 4], F32) for i in range(2)])
        pu_r = Ring([S.psum(f"pu{i}", [128, GT], F32) for i in range(4)], excl=True)
        pd_r = Ring([S.psum(f"pd{i}", [128, 512], F32) for i in range(4)], excl=True)
        ld = {}

        def load(gi):
            h, r_h = h2.next()
            P.dma(h[:], h2T_d[:, gi * GT:(gi + 1) * GT].rearrange("(k p) t -> p k t", p=128), writes=[r_h])
            ld[gi] = (h, r_h)

        load(0)
        for gi in range(NG):
            h, r_h = ld.pop(gi)
            if gi + 1 < NG:
                load(gi + 1)
            pend_g = None

            def gate(fc_, ys_):
                sa, r_sa = sa_r.next()
                P.act.op(lambda e: e.activation(out=sa[:], in_=ys_[0][0][:], func=AF.Silu), reads=[ys_[0][1]], writes=[r_sa])
                P.dve.op(lambda e: e.tensor_tensor(out=gT[:, fc_, :], in0=sa[:], in1=ys_[1][0][:], op=ALU.mult),
                         reads=[r_sa, ys_[1][1]], writes=[r_gT])

            for fc in range(22):
                ysab = []
                uts = []
                for part in range(2):
                    ci = part * 22 + fc
                    ui = us.i
                    u, r_u = us.next()
                    r_uh = r_uhs[ui]
                    P.pool.op(lambda e: e.tensor_copy(out=u[:, 0:2], in_=hal[:, ci, :]), reads=[r_hals[ci]], writes=[r_uh])
                    uts.append((u, r_u, r_uh))
                for part in range(2):
                    ci = part * 22 + fc
                    pu, r_pu = pu_r.next()
                    for k in range(8):
                        P.pe.op(lambda e: e.matmul(pu[:], lhsT=wf[:, k, ci * 128:(ci + 1) * 128], rhs=h[:, k, :],
                                                   start=(k == 0), stop=(k == 7)),
                                reads=[r_wfb[(ci * 128) // 1408], r_h], writes=[r_pu])
                    u, r_u, r_uh = uts[part]
                    y, r_y = ys.next()
                    P.act.op(lambda e: e.activation(out=u[:, 2:GT + 2], in_=pu[:], func=AF.Copy), reads=[r_pu], writes=[r_u])
                    P.act.op(lambda e: e.activation(out=y[:], in_=pu[:], func=AF.Identity, scale=cw[:, ci, 2:3],
                                                    bias=cb[:, ci:ci + 1]), reads=[r_pu, r_cp], writes=[r_y])
                    P.act.op(lambda e: e.activation(out=hal[:, ci, :], in_=pu[:, GT - 2:GT], func=AF.Copy),
                             reads=[r_pu], writes=[r_hals[ci]])
                    P.dve.op(lambda e: e.scalar_tensor_tensor(out=y[:], in0=u[:, 1:GT + 1], scalar=cw[:, ci, 1:2], in1=y[:],
                                                              op0=ALU.mult, op1=ALU.add),
                             reads=[r_u, r_uh, r_cp, r_y], writes=[r_y])
                    P.dve.op(lambda e: e.scalar_tensor_tensor(out=y[:], in0=u[:, 0:GT], scalar=cw[:, ci, 0:1], in1=y[:],
                                                              op0=ALU.mult, op1=ALU.add),
                             reads=[r_u, r_uh, r_cp, r_y], writes=[r_y])
                    ysab.append((y, r_y))
                if pend_g is not None:
                    gate(*pend_g)
                pend_g = (fc, ysab)
            gate(*pend_g)
            for ts in range(GT // 128):
                ti = gi * (GT // 128) + ts
                rows = slice(ti * 128, (ti + 1) * 128)
                x1, r_x1 = x1t.next()
                P.dma(x1[:], x1_d[rows, :], writes=[r_x1])
                tm_, r_tm = tmp.next()
                for hf in range(2):
                    pd, r_pd = pd_r.next()
                    for fc in range(22):
                        P.pe.op(lambda e: e.matmul(pd[:], lhsT=gT[:, fc, ts * 128:(ts + 1) * 128],
                                                   rhs=wd[:, fc, hf * 512:(hf + 1) * 512], start=(fc == 0), stop=(fc == 21)),
                                reads=[r_gT, r_wd], writes=[r_pd])
                    P.dve.op(lambda e: e.tensor_tensor(out=tm_[:, hf * 512:(hf + 1) * 512], in0=pd[:],
                                                       in1=g2bc[:, hf * 512:(hf + 1) * 512], op=ALU.mult),
                             reads=[r_pd, r_cp], writes=[r_tm])
                P.pool.op(lambda e: e.tensor_tensor(out=tm_[:], in0=tm_[:], in1=x1[:], op=ALU.add),
                          reads=[r_tm, r_x1], writes=[r_tm])
                if not final:
                    P.dma(x_dst[rows, :], tm_[:], reads=[r_tm])
                else:
                    st, r_st = stat.next()
                    P.act.op(lambda e: e.activation(out=junk[:], in_=tm_[:], func=AF.Square, accum_out=st[:, 0:1]),
                             reads=[r_tm], writes=[r_junk, r_st])
                    rstd_ops(st, r_st, 0, 1, D)
                    P.dve.op(lambda e: e.scalar_tensor_tensor(out=x1[:], in0=tm_[:], scalar=st[:, 2:3], in1=fgbc[:],
                                                              op0=ALU.mult, op1=ALU.mult),
                             reads=[r_tm, r_st, r_cp, r_x1], writes=[r_x1])
                    P.dma(x_dst[rows, :], x1[:], reads=[r_x1])
        S.close()

    if stop_after is None:
        for l in range(L):
            x_src = x_in if l == 0 else xbuf_d
            phase0(l)
            P.barrier()
            phase1(l, x_src)
            P.barrier()
            phase3(l)
            P.barrier()
            phase4(l)
            P.barrier()
            phase5a(l, x_src)
            P.barrier()
            phase5b(l, out_d if l == L - 1 else xbuf_d, final=(l == L - 1))
            P.barrier()
    else:
        phase0(0)
        P.barrier()
        phase1(0, x_in)
        P.barrier()
        if stop_after in ("p3", "l0"):
            phase3(0)
            P.barrier()
        if stop_after in ("p4", "l0"):
            phase4(0)
            P.barrier()
        if stop_after == "l0":
            phase5a(0, x_in)
            P.barrier()
            phase5b(0, xbuf_d, final=False)
            P.barrier()
        S = Scope(P)
        z = S.sbuf("zz", [128, D], F32)
        rz = Res()
        P.dve.op(lambda e: e.memset(z[:], 0.0), writes=[rz])
        for ti in range(NT):
            P.dma(out_d[ti * 128:(ti + 1) * 128, :], z[:], reads=[rz])
        S.close()
    P.finish()
    return nc


def host_inputs(inputs):
    cols = _ext_cols()
    f32 = lambda a: np.ascontiguousarray(np.asarray(a, dtype=np.float32))
    w_ext = f32(np.asarray(inputs["w_in"])[:, :, cols])
    shared = {
        "ln1T": f32(np.asarray(inputs["ln1_g"]).reshape(L, 8, 128).transpose(0, 2, 1)),
        "ln2T": f32(np.asarray(inputs["ln2_g"]).reshape(L, 8, 128).transpose(0, 2, 1)),
        "w_ada": f32(inputs["w_ada"]),
        "b_adaT": f32(np.asarray(inputs["b_ada"]).reshape(L, 48, 128).transpose(0, 2, 1)),
        "w_ext": w_ext,
        "cmp_w1_k": f32(inputs["cmp_w1_k"]), "cmp_w1_v": f32(inputs["cmp_w1_v"]),
        "cmp_w2_k": f32(inputs["cmp_w2_k"]), "cmp_w2_v": f32(inputs["cmp_w2_v"]),
        "cmp_w2_k_sw": f32(np.asarray(inputs["cmp_w2_k"])[:, :, _swap64(np.arange(64))]),
        "sb_out_g": f32(inputs["sb_out_g"]), "nsa_out_g": f32(inputs["nsa_out_g"]),
        "w_out": f32(inputs["w_out"]), "ffn_w_in": f32(inputs["ffn_w_in"]), "ffn_w_down": f32(inputs["ffn_w_down"]),
        "conv_w": f32(np.asarray(inputs["ffn_conv_w"]).reshape(L, 3, 44, 128).transpose(0, 3, 2, 1)),
        "conv_b": f32(np.asarray(inputs["ffn_conv_b"]).reshape(L, 44, 128).transpose(0, 2, 1)),
        "final_g": f32(inputs["final_g"]),
        "posTk": f32(np.asarray(inputs["cmp_pos_k"]).transpose(0, 2, 1)),
        "posTv": f32(np.asarray(inputs["cmp_pos_v"]).transpose(0, 2, 1)),
    }
    shared.update(_consts())
    x = np.asarray(inputs["x"])
    c = np.asarray(inputs["c"])
    in_maps = []
    for b in range(8):
        m = dict(shared)
        m["x"] = f32(x[b])
        m["cT"] = f32(c[b].reshape(8, 128).T)
        in_maps.append(m)
    return in_maps


def kernel(**inputs):
    in_maps = host_inputs(inputs)
    nc = build_program()
    res = run_bass_kernel_spmd(nc, in_maps, core_ids=list(range(8)))
    return np.stack([r["out"] for r in res.results], axis=0).astype(np.float32)
```

```python
import numpy as np
import ml_dtypes
import concourse.bass as bass
import concourse.mybir as mybir
from concourse.bass_utils import run_bass_kernel_spmd

F32 = mybir.dt.float32
BF16 = mybir.dt.bfloat16
AF = mybir.ActivationFunctionType
ALU = mybir.AluOpType
AX = mybir.AxisListType

T = 4096
D = 1024
L = 2
NT = T // 128
DFF = 2816
EPS = 1e-6
N_FM = 22 * 128
N_TM = 792
N_EXT = N_FM + N_TM


class Res:
    __slots__ = ("w", "r", "excl")

    def __init__(self, excl=False):
        self.w = []
        self.r = []
        self.excl = excl


class Eng:
    def __init__(self, name, e, sem, is_pe=False):
        self.name = name
        self.e = e
        self.sem = sem
        self.count = 0
        self.waited = {}
        self.is_pe = is_pe

    def _wait(self, tok):
        sem, val = tok
        k = id(sem)
        if self.waited.get(k, 0) >= val:
            return
        self.e.wait_ge(sem, val)
        self.waited[k] = val

    def _deps(self, reads, writes):
        for r in reads:
            for t in r.w:
                if not (self.is_pe and t[0] is self.sem):
                    self._wait(t)
            if r.excl:
                for t in r.r:
                    if t[0] is not self.sem:
                        self._wait(t)
        for r in writes:
            for t in r.w:
                if not (self.is_pe and t[0] is self.sem):
                    self._wait(t)
            for t in r.r:
                if t[0] is self.sem:
                    continue
                self._wait(t)

    def op(self, fn, reads=(), writes=()):
        self._deps(reads, writes)
        ins = fn(self.e)
        self.count += 1
        ins.then_inc(self.sem, 1)
        tok = (self.sem, self.count)
        for r in reads:
            r.r = [t for t in r.r if t[0] is not self.sem] + [tok]
        for r in writes:
            r.w = [tok]
            r.r = []
        return tok


class Prog:
    def __init__(self, nc, n_dma_sems=40):
        self.nc = nc
        self._ctx = []
        mk = lambda name: self._enter(nc.semaphore(name))
        self.pe = Eng("pe", nc.tensor, mk("s_pe"), is_pe=True)
        self.act = Eng("act", nc.scalar, mk("s_act"))
        self.dve = Eng("dve", nc.vector, mk("s_dve"))
        self.pool = Eng("pool", nc.gpsimd, mk("s_pool"))
        self.sp = Eng("sp", nc.sync, mk("s_sp"))
        self.dma_sems = [mk(f"s_dma{i}") for i in range(n_dma_sems)]
        self.dma_vals = [0] * n_dma_sems
        self.dma_rr = 0

    def _enter(self, cm):
        v = cm.__enter__()
        self._ctx.append(cm)
        return v

    def sbuf(self, name, shape, dtype):
        return self._enter(self.nc.sbuf_tensor(name, list(shape), dtype))

    def psum(self, name, shape, dtype=F32):
        return self._enter(self.nc.psum_tensor(name, list(shape), dtype))

    def dma_group(self, items, reads=(), writes=()):
        dep = []
        for r in reads:
            dep += r.w
        for r in writes:
            dep += r.w
            dep += r.r
        toks = []
        for it in items:
            out, in_, q, kw = (list(it) + [None, None])[:4]
            q = q or self.sp
            kw = kw or {}
            i = self.dma_rr
            self.dma_rr = (self.dma_rr + 1) % len(self.dma_sems)
            sem = self.dma_sems[i]
            if self.dma_vals[i] > 0:
                q._wait((sem, self.dma_vals[i]))
            for t in dep:
                q._wait(t)
            ins = q.e.dma_start(out=out, in_=in_, **kw)
            self.dma_vals[i] += 16
            ins.then_inc(sem, 16)
            toks.append((sem, self.dma_vals[i]))
        for r in reads:
            r.r = r.r + toks
        for r in writes:
            r.w = list(toks)
            r.r = []
        return toks

    def dma(self, out, in_, reads=(), writes=(), q=None, **kw):
        return self.dma_group([(out, in_, q, kw)], reads, writes)

    def barrier(self):
        engs = (self.pe, self.act, self.dve, self.pool, self.sp)
        for e in engs:
            for i, sem in enumerate(self.dma_sems):
                if self.dma_vals[i] > 0:
                    e._wait((sem, self.dma_vals[i]))
            for o in engs:
                if o is not e and o.count > 0:
                    e._wait((o.sem, o.count))

    def finish(self):
        self.barrier()
        for cm in reversed(self._ctx):
            cm.__exit__(None, None, None)
        self._ctx = []


class Scope:
    _uid = [0]

    def __init__(self, P):
        self.P = P
        self.cms = []
        Scope._uid[0] += 1
        self.sfx = f"_s{Scope._uid[0]}"

    def sbuf(self, name, shape, dtype):
        cm = self.P.nc.sbuf_tensor(name + self.sfx, list(shape), dtype)
        v = cm.__enter__()
        self.cms.append(cm)
        return v

    def psum(self, name, shape, dtype=F32):
        isz = 4 if dtype == F32 else 2
        cm = self.P.nc.psum_tensor(name + self.sfx, [128, 2048 // isz], dtype)
        v = cm.__enter__()
        self.cms.append(cm)
        n = int(np.prod(shape[1:]))
        view = v[0:shape[0], 0:n]
        if len(shape) == 3:
            view = view.rearrange("p (a b) -> p a b", b=shape[2])
        return view

    def close(self):
        for cm in reversed(self.cms):
            cm.__exit__(None, None, None)
        self.cms = []


class Ring:
    def __init__(self, tiles, excl=False):
        self.tiles = tiles
        self.res = [Res(excl) for _ in tiles]
        self.i = 0

    def next(self):
        t, r = self.tiles[self.i], self.res[self.i]
        self.i = (self.i + 1) % len(self.tiles)
        return t, r


def _swap64(cols):
    cols = np.asarray(cols).reshape(-1, 64)
    return np.concatenate([cols[:, 32:], cols[:, :32]], axis=1).reshape(-1)


def _ext_cols():
    r = lambda a, b: np.arange(a, b)
    sbq, sbk, sbv = r(0, 512), r(512, 1024), r(1024, 1536)
    nq, kc, vc = r(1536, 2048), r(2048, 2176), r(2176, 2304)
    ks, vs, kw, vw, gl = r(2304, 2432), r(2432, 2560), r(2560, 2688), r(2688, 2816), r(2816, 2840)
    fm = [sbq, sbk]
    for c in range(4):
        fm += [nq[c * 128:(c + 1) * 128], _swap64(nq[c * 128:(c + 1) * 128])]
    fm += [ks, _swap64(ks), kw, _swap64(kw), kc, vc]
    tm = [sbv, vs, vw, gl]
    cols = np.concatenate(fm + tm)
    assert cols.shape[0] == N_EXT
    return cols


def _consts():
    half = 32
    inv = (10000.0 ** (-np.arange(half, dtype=np.float32) / half)).astype(np.float32)
    pos = np.arange(T, dtype=np.float32)
    d = np.arange(128) % 64
    ang = (pos[None, :] * inv[d % 32][:, None]).astype(np.float32)
    cos = np.cos(ang).astype(np.float32)
    sin = np.sin(ang).astype(np.float32)
    sgn = np.where(d < 32, -1.0, 1.0).astype(np.float32)[:, None]
    c = {
        "rope_cos": cos, "rope_sin": (sin * sgn).astype(np.float32),
        "ident_bf": np.eye(128).astype(ml_dtypes.bfloat16),
        "ident_f32": np.eye(128).astype(np.float32),
    }
    j = np.arange(128)
    bf = ml_dtypes.bfloat16
    c["tri_ge"] = (j[:, None] >= j[None, :]).astype(bf)
    c["tri_lt"] = (j[:, None] < j[None, :]).astype(bf)
    c["tri_le"] = (j[:, None] <= j[None, :]).astype(bf)
    c["tri_gt"] = (j[:, None] > j[None, :]).astype(bf)
    n = np.arange(256, dtype=np.float32)
    angc = ((16.0 * n + 31.0)[None, :] * inv[d % 32][:, None]).astype(np.float32)
    c["cmp_cos"] = np.cos(angc).astype(np.float32)
    c["cmp_sin"] = (np.sin(angc).astype(np.float32) * sgn).astype(np.float32)
    nn = np.arange(128)[:, None, None]
    qq = np.arange(17)[None, :, None]
    tt = np.arange(128)[None, None, :]
    BIG = 240000.0
    c["cmaskb"] = np.where(16 * nn + 31 <= 128 * qq + tt, 0.0, -BIG).astype(bf)
    c["cb_le"] = np.where(j[:, None] <= j[None, :], 0.0, -BIG).astype(bf)
    c["cb_gt"] = np.where(j[:, None] > j[None, :], 0.0, -BIG).astype(bf)
    ncmp = np.arange(256)[:, None]
    jj = np.arange(64)[None, :]
    ov = np.clip(np.minimum(16 * ncmp + 32, 64 * jj + 64) - np.maximum(16 * ncmp, 64 * jj), 0, None) / 32.0
    ov[255, :] = 0.0
    c["ovT"] = np.ascontiguousarray(ov.reshape(2, 128, 64).transpose(1, 0, 2)).astype(bf)
    sidx = np.arange(T)[None, :]
    c["eexp"] = (sidx // 64 == np.arange(64)[:, None]).astype(bf)
    ttq = np.arange(128)[:, None]
    rel = np.arange(128)[None, :] - 62
    dist = (ttq >= 64).astype(np.int64) - rel
    c["rkeep"] = (dist >= 2).astype(np.float32)
    c["radd"] = np.where(dist < 0, -1.0, np.where(dist <= 1, 1e6, 0.0)).astype(np.float32)
    return c


def build_program(dbg=None, stop_after=None, nsa_tiles=None, dbg_tile=None):
    nc = bass.Bass("TRN2", target_bir_lowering=False)
    dbg = dbg or []

    def din(name, shape, dt=F32):
        return nc.dram_tensor(name, list(shape), dt, kind="ExternalInput").ap()

    def dscr(name, shape, dt):
        kind = "ExternalOutput" if name in dbg else "Internal"
        return nc.dram_tensor(name, list(shape), dt, kind=kind).ap()

    x_in = din("x", [T, D])
    cT_in = din("cT", [128, 8])
    ln1T = din("ln1T", [L, 128, 8])
    ln2T = din("ln2T", [L, 128, 8])
    w_ada = din("w_ada", [L, D, 6 * D])
    b_adaT = din("b_adaT", [L, 128, 48])
    w_ext = din("w_ext", [L, D, N_EXT])
    rope_cos = din("rope_cos", [128, T])
    rope_sin = din("rope_sin", [128, T])
    ident_bf_d = din("ident_bf", [128, 128], BF16)
    ident_f_d = din("ident_f32", [128, 128])
    tri_d = {n: din(n, [128, 128], BF16) for n in ("tri_ge", "tri_lt", "tri_le", "tri_gt")}
    w1k_d = din("cmp_w1_k", [L, 2048, 256])
    w1v_d = din("cmp_w1_v", [L, 2048, 256])
    w2k_d = din("cmp_w2_k", [L, 256, 64])
    w2ksw_d = din("cmp_w2_k_sw", [L, 256, 64])
    w2v_d = din("cmp_w2_v", [L, 256, 64])
    posTk_d = din("posTk", [L, 64, 32])
    posTv_d = din("posTv", [L, 64, 32])
    sbg_d = din("sb_out_g", [L, 512])
    nsag_d = din("nsa_out_g", [L, 512])
    w_out_d = din("w_out", [L, D, D])
    wffn_d = din("ffn_w_in", [L, D, 2 * DFF])
    wdn_d = din("ffn_w_down", [L, DFF, D])
    cw_d = din("conv_w", [L, 128, 44, 3])
    cb_d = din("conv_b", [L, 128, 44])
    fg_d = din("final_g", [D])
    cmp_cos_d = din("cmp_cos", [128, 256])
    cmp_sin_d = din("cmp_sin", [128, 256])
    cmaskb_d = din("cmaskb", [128, 17, 128], BF16)
    cb_le_d = din("cb_le", [128, 128], BF16)
    cb_gt_d = din("cb_gt", [128, 128], BF16)
    ovT_d = din("ovT", [128, 2, 64], BF16)
    eexp_d = din("eexp", [64, T], BF16)
    rkeep_d = din("rkeep", [128, 128])
    radd_d = din("radd", [128, 128])
    out_d = nc.dram_tensor("out", [T, D], F32, kind="ExternalOutput").ap()

    o_sb_d = dscr("o_sb", [T, 512], F32)
    o_nsa_d = dscr("o_nsa", [T, 512], F32)
    xbuf_d = dscr("xbuf", [T, D], F32)
    x1_d = dscr("x1buf", [T, D], F32)
    h2T_d = dscr("h2T", [D, T], BF16)
    fmT = dscr("fmT", [2048, T], BF16)
    tmv = dscr("tmv", [T, 768], BF16)
    gates_d = dscr("gates", [T, 24], F32)
    modrow = dscr("modrow", [L, 6 * D], F32)

    P = Prog(nc)
    def dump(name, ap, res):
        if name not in dbg:
            return
        shp = [int(v) for v in ap.shape]
        dt_ = ap.dtype
        d = nc.dram_tensor(name, shp, dt_, kind="ExternalOutput").ap()
        P.dma(d, ap, reads=[res])

    ident_bf = P.sbuf("ident_bf_sb", [128, 128], BF16)
    r_ident = Res()
    P.dma(ident_bf[:], ident_bf_d[:, :], writes=[r_ident])
    ident_f = P.sbuf("ident_f_sb", [128, 128], F32)
    r_identf = Res()
    P.dma(ident_f[:], ident_f_d[:, :], writes=[r_identf])
    tri = {}
    r_tri = Res()
    for n in tri_d:
        tri[n] = P.sbuf(n + "_sb", [128, 128], BF16)
    P.dma_group([(tri[n][:], tri_d[n][:, :], None, None) for n in tri_d], writes=[r_tri])
    eps_t = P.sbuf("eps_t", [128, 1], F32)
    r_eps = Res()
    P.dve.op(lambda e: e.memset(eps_t[:], EPS), writes=[r_eps])
    cT = P.sbuf("cT_sb", [128, 8], F32)
    r_cT = Res()
    P.dma(cT[:], cT_in[:, :], writes=[r_cT])
    siluc = P.sbuf("siluc", [128, 8], F32)
    r_siluc = Res()
    P.act.op(lambda e: e.activation(out=siluc[:], in_=cT[:], func=AF.Silu), reads=[r_cT], writes=[r_siluc])
    modT = [P.sbuf(f"modT{l}", [128, 48], F32) for l in range(L)]
    r_modT = [Res() for _ in range(L)]
    A1 = [P.sbuf(f"A1_{l}", [128, 8], F32) for l in range(L)]
    A2 = [P.sbuf(f"A2_{l}", [128, 8], F32) for l in range(L)]
    r_A = [Res() for _ in range(L)]
    r_modrow = [Res() for _ in range(L)]

    def phase0(l):
        S = Scope(P)
        wbuf = Ring([S.sbuf(f"wada{i}", [128, 6 * D], F32) for i in range(2)])
        pm = S.psum("pmod", [128, 48], F32)
        r_pm = Res(True)
        lnt = S.sbuf("lnt", [128, 16], F32)
        r_lnt = Res()
        bT = S.sbuf("bT", [128, 48], F32)
        r_bT = Res()
        P.dma(lnt[:, 0:8], ln1T[l], writes=[r_lnt])
        P.dma(lnt[:, 8:16], ln2T[l], writes=[r_lnt])
        P.dma(bT[:], b_adaT[l], writes=[r_bT])
        for k in range(8):
            wt, rw = wbuf.next()
            P.dma_group([(wt[:, hf * 3072:(hf + 1) * 3072],
                          w_ada[l, k * 128:(k + 1) * 128, hf * 3072:(hf + 1) * 3072], None, None)
                         for hf in range(2)], writes=[rw])
            for m in range(48):
                P.pe.op(lambda e: e.matmul(pm[:, m:m + 1], lhsT=wt[:, m * 128:(m + 1) * 128], rhs=siluc[:, k:k + 1],
                                           start=(k == 0 and m == 0), stop=(k == 7 and m == 47),
                                           skip_group_check=True),
                        reads=[rw, r_siluc], writes=[r_pm])
        P.dve.op(lambda e: e.tensor_tensor(out=modT[l][:], in0=pm[:], in1=bT[:], op=ALU.add),
                 reads=[r_pm, r_bT], writes=[r_modT[l]])
        P.dve.op(lambda e: e.scalar_tensor_tensor(out=A1[l][:], in0=modT[l][:, 8:16], scalar=1.0, in1=lnt[:, 0:8],
                                                  op0=ALU.add, op1=ALU.mult),
                 reads=[r_modT[l], r_lnt], writes=[r_A[l]])
        P.dve.op(lambda e: e.scalar_tensor_tensor(out=A2[l][:], in0=modT[l][:, 32:40], scalar=1.0, in1=lnt[:, 8:16],
                                                  op0=ALU.add, op1=ALU.mult),
                 reads=[r_modT[l], r_lnt], writes=[r_A[l]])
        pmt = S.psum("pmt", [48, 128], F32)
        r_pmt = Res(True)
        P.pe.op(lambda e: e.transpose(out=pmt[:], in_=modT[l][:], identity=ident_f[:]),
                reads=[r_modT[l], r_identf], writes=[r_pmt])
        mrow = S.sbuf("mrow", [48, 128], F32)
        r_mrow = Res()
        P.dve.op(lambda e: e.tensor_copy(out=mrow[:], in_=pmt[:]), reads=[r_pmt], writes=[r_mrow])
        P.dma(modrow[l].rearrange("(m p) -> m p", p=128), mrow[:], reads=[r_mrow])
        S.close()

    def phase1(l, x_src):
        S = Scope(P)
        wsb = S.sbuf("w1sb", [128, 8, N_EXT], BF16)
        blocks = [(N_FM, N_EXT), (0, 1024), (1024, 2048), (2048, N_FM)]
        r_wb = [Res() for _ in blocks]
        for (c0, c1), rw in zip(blocks, r_wb):
            P.dma_group([(wsb[:, k, c0:c1], w_ext[l, k * 128:(k + 1) * 128, c0:c1], P.pool, None) for k in range(8)],
                        writes=[rw])

        def r_wcols(c0):
            for (b0, b1), rw in zip(blocks, r_wb):
                if b0 <= c0 < b1:
                    return rw
            raise AssertionError

        cosb = S.sbuf("cosb", [128, T], F32)
        sinb = S.sbuf("sinb", [128, T], F32)
        r_rope = Res()
        P.dma(cosb[:], rope_cos[:, :], writes=[r_rope])
        r_rope2 = Res()
        P.dma(sinb[:], rope_sin[:, :], writes=[r_rope2])
        xt = Ring([S.sbuf(f"xt{i}", [128, D], F32) for i in range(3)])
        junk = S.sbuf("junk", [128, D], F32)
        r_junk = Res()
        stat = Ring([S.sbuf(f"stat{i}", [128, 4], F32) for i in range(4)])
        xn = Ring([S.sbuf(f"xn{i}", [128, D], BF16) for i in range(3)])
        hT = Ring([S.sbuf(f"hT{i}", [128, 8, 512], BF16) for i in range(2)])
        stg = Ring([S.sbuf(f"stg{i}", [128, 512], BF16) for i in range(6)])
        stgtm = Ring([S.sbuf(f"stgtm{i}", [128, 768], BF16) for i in range(2)])
        gst = Ring([S.sbuf(f"gst{i}", [128, 24], F32) for i in range(2)])
        rt1 = Ring([S.sbuf(f"rt1_{i}", [128, 512], F32) for i in range(2)])
        rt2 = Ring([S.sbuf(f"rt2_{i}", [128, 512], F32) for i in range(2)])
        pT_r = Ring([S.psum(f"pT{i}", [128, 8, 128], BF16) for i in range(2)], excl=True)
        ptm_r = Ring([S.psum(f"ptm{i}", [128, 512], F32) for i in range(2)], excl=True)
        pfm = Ring([S.psum(f"pfm{i}", [128, 512], F32) for i in range(4)], excl=True)
        B1 = modT[l][:, 0:8]
        xl = {}
        hcur = {}

        def ldx(ti):
            xtile, r_x = xt.next()
            P.dma(xtile[:], x_src[ti * 128:(ti + 1) * 128, :], writes=[r_x])
            xl[ti] = (xtile, r_x)

        def g1(ti, d):
            xtile, r_x = xl.pop(ti)
            if ti + 1 < NT:
                ldx(ti + 1)
            st, r_st = stat.next()
            P.act.op(lambda e: e.activation(out=junk[:], in_=xtile[:], func=AF.Square, accum_out=st[:, 0:1]),
                     reads=[r_x], writes=[r_junk, r_st])
            rstd_ops(st, r_st, 0, 1, D)
            d.update(xtile=xtile, r_x=r_x, st=st, r_st=r_st)

        def g2(ti, d):
            xnt, r_xn = xn.next()
            P.dve.op(lambda e: e.tensor_scalar(out=xnt[:], in0=d["xtile"][:], scalar1=d["st"][:, 2:3], scalar2=None,
                                               op0=ALU.mult), reads=[d["r_x"], d["r_st"]], writes=[r_xn])
            d.update(xnt=xnt, r_xn=r_xn)

        def g3(ti, d):
            xnt, r_xn = d["xnt"], d["r_xn"]
            pT, r_pT = pT_r.next()
            for k in range(8):
                P.pe.op(lambda e: e.transpose(out=pT[:, k, :], in_=xnt[:, k * 128:(k + 1) * 128], identity=ident_bf[:]),
                        reads=[r_xn, r_ident], writes=[r_pT])
            d.update(pT=pT, r_pT=r_pT)

        def g4(ti, d):
            grp, tt = divmod(ti, 4)
            if tt == 0:
                hcur[grp] = hT.next()
            h, r_h = hcur[grp]
            pT, r_pT = d["pT"], d["r_pT"]
            for k in range(8):
                dst = h[:, k, tt * 128:(tt + 1) * 128]
                if ti % 2 == 0:
                    P.act.op(lambda e: e.activation(out=dst, in_=pT[:, k, :], func=AF.Identity,
                                                    scale=A1[l][:, k:k + 1], bias=B1[:, k:k + 1]),
                             reads=[r_pT, r_A[l], r_modT[l]], writes=[r_h])
                else:
                    P.dve.op(lambda e: e.tensor_scalar(out=dst, in0=pT[:, k, :], scalar1=A1[l][:, k:k + 1],
                                                       scalar2=B1[:, k:k + 1], op0=ALU.mult, op1=ALU.add),
                             reads=[r_pT, r_A[l], r_modT[l]], writes=[r_h])

        def g5(ti, d):
            grp, tt = divmod(ti, 4)
            h, r_h = hcur[grp]
            p0, r_p0 = ptm_r.next()
            p1, r_p1 = ptm_r.next()
            for k in range(8):
                lhsT = h[:, k, tt * 128:(tt + 1) * 128]
                P.pe.op(lambda e: e.matmul(p0[:], lhsT=lhsT, rhs=wsb[:, k, N_FM:N_FM + 512],
                                           start=(k == 0), stop=(k == 7)), reads=[r_h, r_wb[0]], writes=[r_p0])
            for k in range(8):
                lhsT = h[:, k, tt * 128:(tt + 1) * 128]
                P.pe.op(lambda e: e.matmul(p1[:, 0:280], lhsT=lhsT, rhs=wsb[:, k, N_FM + 512:N_EXT],
                                           start=(k == 0), stop=(k == 7)), reads=[r_h, r_wb[0]], writes=[r_p1])
            d.update(p0=p0, r_p0=r_p0, p1=p1, r_p1=r_p1)

        def g6(ti, d):
            p0, r_p0, p1, r_p1 = d["p0"], d["r_p0"], d["p1"], d["r_p1"]
            sg, r_sg = stgtm.next()
            P.act.op(lambda e: e.activation(out=sg[:, 0:512], in_=p0[:], func=AF.Copy), reads=[r_p0], writes=[r_sg])
            P.dve.op(lambda e: e.tensor_copy(out=sg[:, 512:768], in_=p1[:, 0:256]), reads=[r_p1], writes=[r_sg])
            gs, r_gs = gst.next()
            P.act.op(lambda e: e.activation(out=gs[:], in_=p1[:, 256:280], func=AF.Sigmoid), reads=[r_p1], writes=[r_gs])
            P.dma(tmv[ti * 128:(ti + 1) * 128, :], sg[:], reads=[r_sg])
            P.dma(gates_d[ti * 128:(ti + 1) * 128, :], gs[:], reads=[r_gs])

        evc = [0]

        def fm_group(grp):
            h, r_h = hcur[grp]
            tsl = slice(grp * 512, (grp + 1) * 512)

            def fm_mm(ch):
                pf, r_pf = pfm.next()
                rw = r_wcols(ch * 128)
                for k in range(8):
                    P.pe.op(lambda e: e.matmul(pf[:], lhsT=wsb[:, k, ch * 128:(ch + 1) * 128], rhs=h[:, k, :],
                                               start=(k == 0), stop=(k == 7)), reads=[r_h, rw], writes=[r_pf])
                return pf, r_pf

            for ch in range(8):
                pf, r_pf = fm_mm(ch)
                s_, r_s = stg.next()
                if evc[0] % 2 == 0:
                    P.act.op(lambda e: e.activation(out=s_[:], in_=pf[:], func=AF.Copy), reads=[r_pf], writes=[r_s])
                else:
                    P.dve.op(lambda e: e.tensor_copy(out=s_[:], in_=pf[:]), reads=[r_pf], writes=[r_s])
                evc[0] += 1
                P.dma(fmT[ch * 128:(ch + 1) * 128, tsl], s_[:], reads=[r_s])
            for pr in range(6):
                pa, r_pa = fm_mm(8 + 2 * pr)
                pb, r_pb = fm_mm(9 + 2 * pr)
                t1, r_t1 = rt1.next()
                t2, r_t2 = rt2.next()
                P.dve.op(lambda e: e.tensor_tensor(out=t1[:], in0=pa[:], in1=cosb[:, tsl], op=ALU.mult),
                         reads=[r_pa, r_rope], writes=[r_t1])
                P.dve.op(lambda e: e.tensor_tensor(out=t2[:], in0=pb[:], in1=sinb[:, tsl], op=ALU.mult),
                         reads=[r_pb, r_rope2], writes=[r_t2])
                s_, r_s = stg.next()
                P.pool.op(lambda e: e.tensor_tensor(out=s_[:], in0=t1[:], in1=t2[:], op=ALU.add),
                          reads=[r_t1, r_t2], writes=[r_s])
                P.dma(fmT[1024 + pr * 128:1024 + (pr + 1) * 128, tsl], s_[:], reads=[r_s])
            for j in range(2):
                pf, r_pf = fm_mm(20 + j)
                s_, r_s = stg.next()
                P.act.op(lambda e: e.activation(out=s_[:], in_=pf[:], func=AF.Copy), reads=[r_pf], writes=[r_s])
                P.dma(fmT[1792 + j * 128:1792 + (j + 1) * 128, tsl], s_[:], reads=[r_s])

        ldx(0)
        stages = [g1, g2, g3, g4, g5, g6]
        sts = {}
        for it in range(NT + len(stages) - 1):
            fm_after = None
            for si, fn in reversed(list(enumerate(stages))):
                ti = it - si
                if 0 <= ti < NT:
                    if si == 0:
                        sts[ti] = {}
                    fn(ti, sts[ti])
                    if si == 3 and ti % 4 == 3:
                        fm_after = ti // 4
                    if si == len(stages) - 1:
                        sts.pop(ti)
            if fm_after is not None:
                fm_group(fm_after)
        S.close()

    def phase3(l):
        S = Scope(P)
        qT = Ring([S.sbuf(f"sbq{i}", [64, T], BF16) for i in range(2)])
        kT = Ring([S.sbuf(f"sbk{i}", [64, T], BF16) for i in range(2)])
        vt = Ring([S.sbuf(f"sbv{i}", [128, NT, 64], BF16) for i in range(2)])
        e_r = Ring([S.sbuf(f"e{i}", [128, 512], F32) for i in range(3)])
        sp_r = Ring([S.sbuf(f"sp{i}", [128, 512], BF16) for i in range(4)])
        w_r = Ring([S.sbuf(f"w{i}", [128, 512], BF16) for i in range(4)])
        o_r = Ring([S.sbuf(f"osb{i}", [128, 4, 64], F32) for i in range(2)])
        pS = Ring([S.psum(f"pS{i}", [128, 512], F32) for i in range(2)], excl=True)
        p2 = Ring([S.psum(f"p2{i}", [128, 512], F32) for i in range(2)], excl=True)
        pD = Ring([S.psum(f"pD{i}", [128, 512], F32) for i in range(2)], excl=True)
        pacc = Ring([S.psum(f"pacc{i}", [128, 4, 64], F32) for i in range(2)], excl=True)
        heads = {}

        def ldh(h):
            q, rq = qT.next()
            k, rk = kT.next()
            v, rv = vt.next()
            P.dma(q[:], fmT[h * 64:(h + 1) * 64, :], writes=[rq])
            P.dma(k[:], fmT[512 + h * 64:512 + (h + 1) * 64, :], writes=[rk])
            P.dma(v[:], tmv[:, h * 64:(h + 1) * 64].rearrange("(kb p) d -> p kb d", p=128), writes=[rv])
            heads[h] = (q, rq, k, rk, v, rv)

        units = []
        for h in range(8):
            for c in range(8):
                for kb in range(4 * c + 3, -1, -1):
                    units.append(dict(h=h, c=c, kb=kb, first=(kb == 4 * c + 3), last=(kb == 0)))

        def stage1(u):
            h, c, kb = u["h"], u["c"], u["kb"]
            if u["first"] and c == 0:
                if h == 0:
                    ldh(0)
                if h + 1 < 8:
                    ldh(h + 1)
            q, rq, k, rk, v, rv = heads[h]
            i = kb - 4 * c
            q0 = 128 * i if i > 0 else 0
            u["q0"] = q0
            ps, r_ps = pS.next()
            P.pe.op(lambda e: e.matmul(ps[:, q0:512], lhsT=k[:, kb * 128:(kb + 1) * 128],
                                       rhs=q[:, c * 512 + q0:(c + 1) * 512], start=True, stop=True),
                    reads=[rq, rk], writes=[r_ps])
            et, r_e = e_r.next()
            P.act.op(lambda e: e.activation(out=et[:, q0:512], in_=ps[:, q0:512], func=AF.Exp, scale=0.125),
                     reads=[r_ps], writes=[r_e])
            if i >= 0:
                P.dve.op(lambda e: e.tensor_tensor(out=et[:, q0:q0 + 128], in0=et[:, q0:q0 + 128],
                                                   in1=tri["tri_lt"][:], op=ALU.mult),
                         reads=[r_e, r_tri], writes=[r_e])
            spt, r_sp = sp_r.next()
            P.act.op(lambda e: e.activation(out=spt[:, q0:512], in_=et[:, q0:512], func=AF.Ln, bias=1.0, scale=1.0),
                     reads=[r_e], writes=[r_sp])
            u.update(et=et, r_e=r_e, spt=spt, r_sp=r_sp)

        chain = {}

        def stage2(u):
            q0 = u["q0"]
            if u["first"]:
                chain["p2"], chain["r_p2"] = p2.next()
                chain["prev"] = None
            pp, r_pp = chain["p2"], chain["r_p2"]
            prev = chain["prev"]
            if prev is not None:
                pq0 = prev["q0"]
                P.pe.op(lambda e: e.matmul(pp[:, pq0:512], lhsT=tri["tri_lt"][:], rhs=prev["spt"][:, pq0:512],
                                           start=False, stop=False, skip_group_check=True),
                        reads=[prev["r_sp"], r_tri], writes=[r_pp])
            P.pe.op(lambda e: e.matmul(pp[:, q0:512], lhsT=tri["tri_ge"][:], rhs=u["spt"][:, q0:512],
                                       start=(prev is None), stop=True, skip_group_check=True),
                    reads=[u["r_sp"], r_tri], writes=[r_pp])
            chain["prev"] = u
            pd, r_pd = pD.next()
            P.act.op(lambda e: e.activation(out=pd[:, q0:512], in_=pp[:, q0:512], func=AF.Exp, scale=-1.0),
                     reads=[r_pp], writes=[r_pd])
            wt, r_w = w_r.next()
            P.dve.op(lambda e: e.tensor_tensor(out=wt[:, q0:512], in0=u["et"][:, q0:512], in1=pd[:, q0:512],
                                               op=ALU.mult),
                     reads=[u["r_e"], r_pd], writes=[r_w])
            u.update(wt=wt, r_w=r_w)

        accs = {}

        def stage3(u):
            h, c, kb, q0 = u["h"], u["c"], u["kb"], u["q0"]
            q, rq, k, rk, v, rv = heads[h]
            if u["first"]:
                accs["a"], accs["r"] = pacc.next()
            acc, r_acc = accs["a"], accs["r"]
            for ts in range(q0 // 128, 4):
                P.pe.op(lambda e: e.matmul(acc[:, ts, :], lhsT=u["wt"][:, ts * 128:(ts + 1) * 128], rhs=v[:, kb, :],
                                           start=(u["first"] and ts == q0 // 128), stop=(u["last"] and ts == 3),
                                           skip_group_check=True),
                        reads=[u["r_w"], rv], writes=[r_acc])
            if u["last"]:
                ot, r_o = o_r.next()
                P.dve.op(lambda e: e.tensor_copy(out=ot[:], in_=acc[:]), reads=[r_acc], writes=[r_o])
                P.dma(o_sb_d[c * 512:(c + 1) * 512, h * 64:(h + 1) * 64].rearrange("(s p) d -> p s d", p=128),
                      ot[:], reads=[r_o])

        n = len(units)
        for it in range(n + 3):
            if 0 <= it - 3 < n:
                stage3(units[it - 3])
            if it < n:
                stage1(units[it])
            if 0 <= it - 1 < n:
                stage2(units[it - 1])
        S.close()


    def phase4(l):
        S = Scope(P)
        qs = [S.sbuf(f"qs{g}", [128, 4, T], BF16) for g in range(2)]
        kse = [S.sbuf(f"kse{g}", [128, T], BF16) for g in range(2)]
        kw2 = S.sbuf("kw2", [64, 2, T], BF16)
        vs1 = S.sbuf("vs1", [128, 2, NT, 65], BF16)
        vw1 = S.sbuf("vw1", [128, 2, NT, 65], BF16)
        kcr = S.sbuf("kcr", [64, 2, 256], BF16)
        vov = S.sbuf("vov", [128, 2, 2, 65], BF16)
        ovT = S.sbuf("ovT_sb", [128, 2, 64], BF16)
        cmaskb = S.sbuf("cmaskb_sb", [128, 17, 128], BF16)
        cb_le = S.sbuf("cb_le_sb", [128, 128], BF16)
        cb_gt = S.sbuf("cb_gt_sb", [128, 128], BF16)
        rkeep = S.sbuf("rkeep_sb", [128, 128], F32)
        radd = S.sbuf("radd_sb", [128, 128], F32)
        r_q = [Res(), Res()]
        r_kse = [Res(), Res()]
        r_kw, r_vs, r_vw, r_kcr, r_vov, r_c4 = (Res() for _ in range(6))
        for g in range(2):
            P.dma(qs[g][0:64, :, :], fmT[1024 + 256 * g:1024 + 256 * (g + 1), :].rearrange("(hh d) t -> d hh t", d=64),
                  writes=[r_q[g]])
            P.dma_group([(kse[g][0:64, :], fmT[1536 + 64 * g:1536 + 64 * (g + 1), :], None, None),
                         (kse[g][64:128, :], eexp_d[:, :], None, None)], writes=[r_kse[g]])
        P.dma_group([(kw2[:, g, :], fmT[1664 + 64 * g:1664 + 64 * (g + 1), :], None, None) for g in range(2)], writes=[r_kw])
        P.pool.op(lambda e: e.memset(vs1[:], 1.0), writes=[r_vs])
        P.pool.op(lambda e: e.memset(vw1[:], 1.0), writes=[r_vw])
        P.pool.op(lambda e: e.memset(vov[:], 1.0), writes=[r_vov])
        P.dma_group([(vs1[:, g, :, 0:64],
                      tmv[:, 512 + 64 * g:512 + 64 * (g + 1)].rearrange("(kb p) d -> p kb d", p=128), None, None)
                     for g in range(2)], writes=[r_vs])
        P.dma_group([(vw1[:, g, :, 0:64],
                      tmv[:, 640 + 64 * g:640 + 64 * (g + 1)].rearrange("(kb p) d -> p kb d", p=128), None, None)
                     for g in range(2)], writes=[r_vw])
        P.dma_group([(ovT[:], ovT_d[:, :, :], None, None), (cmaskb[:], cmaskb_d[:, :, :], None, None),
                     (cb_le[:], cb_le_d[:, :], None, None), (cb_gt[:], cb_gt_d[:, :], None, None),
                     (rkeep[:], rkeep_d[:, :], None, None), (radd[:], radd_d[:, :], None, None)], writes=[r_c4])

        S2 = Scope(P)
        kcT = S2.sbuf("kcT", [128, T], BF16)
        vcT = S2.sbuf("vcT", [128, T], BF16)
        w1 = {"k": S2.sbuf("w1k", [128, 32, 256], BF16), "v": S2.sbuf("w1v", [128, 32, 256], BF16)}
        w2k = S2.sbuf("w2k", [128, 2, 64], BF16)
        w2ks = S2.sbuf("w2ks", [128, 2, 64], BF16)
        w2v = S2.sbuf("w2v", [128, 2, 64], BF16)
        posT = {"k": S2.sbuf("posTk_sb", [64, 32], BF16), "v": S2.sbuf("posTv_sb", [64, 32], BF16)}
        ccos = S2.sbuf("ccos", [128, 256], F32)
        csin = S2.sbuf("csin", [128, 256], F32)
        bias_sb = S2.sbuf("cbias", [128, 4], F32)
        r_x2, r_w1, r_w2, r_pos, r_cc, r_bias = (Res() for _ in range(6))
        P.dma_group([(kcT[:], fmT[1792:1920, :], None, None), (vcT[:], fmT[1920:2048, :], None, None)], writes=[r_x2])
        srcs = {"k": w1k_d, "v": w1v_d}
        P.dma_group([(w1[kd][64 * hf:64 * hf + 64, :, :], srcs[kd][l].rearrange("(l d) h -> d l h", d=64), P.pool, None)
                     for kd in ("k", "v") for hf in range(2)], writes=[r_w1])
        P.dma_group([(w2k[:], w2k_d[l].rearrange("(hc p) d -> p hc d", p=128), P.pool, None),
                     (w2ks[:], w2ksw_d[l].rearrange("(hc p) d -> p hc d", p=128), P.pool, None),
                     (w2v[:], w2v_d[l].rearrange("(hc p) d -> p hc d", p=128), P.pool, None)], writes=[r_w2])
        P.dma_group([(posT["k"][:], posTk_d[l], P.pool, None), (posT["v"][:], posTv_d[l], P.pool, None)], writes=[r_pos])
        P.dma_group([(ccos[:], cmp_cos_d[:, :], None, None), (csin[:], cmp_sin_d[:, :], None, None)], writes=[r_cc])
        pb = S2.psum("pb", [128, 4], F32)
        r_pb = Res(True)
        first = True
        for ki, kd in enumerate(("k", "v")):
            for hc in range(2):
                for ll in range(32):
                    P.pe.op(lambda e: e.matmul(pb[:, 2 * ki + hc:2 * ki + hc + 1],
                                               lhsT=w1[kd][0:64, ll, hc * 128:(hc + 1) * 128],
                                               rhs=posT[kd][:, ll:ll + 1], start=first, stop=False,
                                               skip_group_check=True),
                            reads=[r_w1, r_pos], writes=[r_pb])
                    first = False
        P.dve.op(lambda e: e.tensor_copy(out=bias_sb[:], in_=pb[:]), reads=[r_pb], writes=[r_bias])
        ph = Ring([S2.psum(f"ph{i}", [128, 256], F32) for i in range(2)], excl=True)
        pk = Ring([S2.psum(f"pk{i}", [128, 256], F32) for i in range(2)], excl=True)
        u_r = Ring([S2.sbuf(f"cu{i}", [128, 256], F32) for i in range(2)])
        t_r = Ring([S2.sbuf(f"ct{i}", [128, 256], F32) for i in range(2)])
        g_r = [S2.sbuf(f"cg{i}", [128, 256], BF16) for i in range(4)]
        r_g = [Res() for _ in range(4)]
        for i in range(4):
            P.pool.op(lambda e: e.memset(g_r[i][:], 0.0), writes=[r_g[i]])
        gi = 0
        xsrc = {"k": kcT, "v": vcT}
        for ki, kd in enumerate(("k", "v")):
            for g in range(2):
                gts = []
                for hc in range(2):
                    pht, r_ph = ph.next()
                    for ll in range(32):
                        P.pe.op(lambda e: e.matmul(pht[:, 0:255], lhsT=w1[kd][64 * g:64 * g + 64, ll, hc * 128:(hc + 1) * 128],
                                                   rhs=xsrc[kd][64 * g:64 * g + 64, ll:ll + 16 * 254 + 1:16],
                                                   start=(ll == 0), stop=(ll == 31)),
                                reads=[r_w1, r_x2], writes=[r_ph])
                    ut, r_u = u_r.next()
                    P.act.op(lambda e: e.activation(out=ut[:, 0:255], in_=pht[:, 0:255], func=AF.Identity,
                                                    bias=bias_sb[:, 2 * ki + hc:2 * ki + hc + 1], scale=1.0),
                             reads=[r_ph, r_bias], writes=[r_u])
                    tt_, r_t = t_r.next()
                    P.dve.op(lambda e: e.tensor_tensor(out=tt_[:, 0:255], in0=ut[:, 0:255], in1=ut[:, 0:255], op=ALU.mult),
                             reads=[r_u], writes=[r_t])
                    P.dve.op(lambda e: e.tensor_scalar(out=tt_[:, 0:255], in0=tt_[:, 0:255], scalar1=0.044715, scalar2=1.0,
                                                       op0=ALU.mult, op1=ALU.add), reads=[r_t], writes=[r_t])
                    P.dve.op(lambda e: e.tensor_tensor(out=tt_[:, 0:255], in0=tt_[:, 0:255], in1=ut[:, 0:255], op=ALU.mult),
                             reads=[r_t, r_u], writes=[r_t])
                    P.act.op(lambda e: e.activation(out=tt_[:, 0:255], in_=tt_[:, 0:255], func=AF.Sigmoid,
                                                    scale=1.5957691216057308), reads=[r_t], writes=[r_t])
                    gt_, r_gt = g_r[gi % 4], r_g[gi % 4]
                    gi += 1
                    P.dve.op(lambda e: e.tensor_tensor(out=gt_[:, 0:255], in0=ut[:, 0:255], in1=tt_[:, 0:255], op=ALU.mult),
                             reads=[r_t, r_u], writes=[r_gt])
                    gts.append((gt_, r_gt))
                if kd == "k":
                    pa, r_pa = pk.next()
                    pb2, r_pb2 = pk.next()
                    for hc in range(2):
                        P.pe.op(lambda e: e.matmul(pa[0:64, :], lhsT=w2k[:, hc, :], rhs=gts[hc][0][:], start=(hc == 0), stop=(hc == 1)),
                                reads=[r_w2, gts[hc][1]], writes=[r_pa])
                    for hc in range(2):
                        P.pe.op(lambda e: e.matmul(pb2[0:64, :], lhsT=w2ks[:, hc, :], rhs=gts[hc][0][:], start=(hc == 0), stop=(hc == 1)),
                                reads=[r_w2, gts[hc][1]], writes=[r_pb2])
                    t1, r_t1 = u_r.next()
                    t2, r_t2 = t_r.next()
                    P.dve.op(lambda e: e.tensor_tensor(out=t1[0:64, :], in0=pa[0:64, :], in1=ccos[0:64, :], op=ALU.mult),
                             reads=[r_pa, r_cc], writes=[r_t1])
                    P.dve.op(lambda e: e.tensor_tensor(out=t2[0:64, :], in0=pb2[0:64, :], in1=csin[0:64, :], op=ALU.mult),
                             reads=[r_pb2, r_cc], writes=[r_t2])
                    P.dve.op(lambda e: e.tensor_tensor(out=kcr[:, g, :], in0=t1[0:64, :], in1=t2[0:64, :], op=ALU.add),
                             reads=[r_t1, r_t2], writes=[r_kcr])
                else:
                    for nn in range(2):
                        pv, r_pv = pk.next()
                        for hc in range(2):
                            P.pe.op(lambda e: e.matmul(pv[:, 0:64], lhsT=gts[hc][0][:, nn * 128:(nn + 1) * 128],
                                                       rhs=w2v[:, hc, :], start=(hc == 0), stop=(hc == 1)),
                                    reads=[r_w2, gts[hc][1]], writes=[r_pv])
                        P.dve.op(lambda e: e.tensor_copy(out=vov[:, g, nn, 0:64], in_=pv[:, 0:64]),
                                 reads=[r_pv], writes=[r_vov])
        S2.close()
        P.barrier()

        pSr = Ring([S.psum(f"pSn{i}", [128, 4, 128], F32) for i in range(3)], excl=True)
        pOc = S.psum("pOc", [128, 4, 65], F32)
        pImp = S.psum("pImp", [128, 4, 64], F32)
        pOs = S.psum("pOs", [128, 4, 65], F32)
        pOw = S.psum("pOw", [128, 4, 65], F32)
        pMTb = S.psum("pMT", [128, 1024], BF16)
        r_pOc, r_pImp, r_pOs, r_pOw, r_pT4 = (Res(True) for _ in range(5))
        ex_r = Ring([S.sbuf(f"ex{i}", [128, 4, 128], BF16) for i in range(4)])
        sm_r = Ring([S.sbuf(f"sm{i}", [128, 32], F32) for i in range(2)])
        oc_r = Ring([S.sbuf(f"oc{i}", [128, 4, 64], F32) for i in range(2)])
        imp_r = Ring([S.sbuf(f"imp{i}", [128, 64], F32) for i in range(2)])
        sc_r = Ring([S.sbuf(f"sc{i}", [128, 64], F32) for i in range(2)])
        sc2_r = Ring([S.sbuf(f"sc2{i}", [128, 64], F32) for i in range(2)])
        m8_r = Ring([S.sbuf(f"m8{i}", [128, 16], F32) for i in range(2)])
        sel_r = Ring([S.sbuf(f"sel{i}", [128, 128], BF16) for i in range(2)])
        for i in range(2):
            P.pool.op(lambda e: e.memset(sel_r.tiles[i][:], 0.0), writes=[sel_r.res[i]])
        gt_r = Ring([S.sbuf(f"gt{i}", [128, 12], F32) for i in range(2)])
        cf_r = Ring([S.sbuf(f"cf{i}", [128, 16], F32) for i in range(2)])
        ta_r = Ring([S.sbuf(f"ta{i}", [128, 4, 64], F32) for i in range(2)])
        tb_r = Ring([S.sbuf(f"tb{i}", [128, 4, 64], F32) for i in range(2)])
        on_r = Ring([S.sbuf(f"on{i}", [128, 4, 64], F32) for i in range(2)])
        BIG = 240000.0

        def bc(ap, shape):
            return ap.to_broadcast(shape)

        def prologue(g, qt):
            st = {}
            qsl = qs[g][0:64, :, qt * 128:(qt + 1) * 128]
            gt_, r_gt = gt_r.next()
            P.dma(gt_[:], gates_d[qt * 128:(qt + 1) * 128, 12 * g:12 * (g + 1)], writes=[r_gt])
            st["gt"], st["r_gt"] = gt_, r_gt
            nns = [0] if qt < 16 else [0, 1]
            exs = []
            for nn in nns:
                ps, r_ps = pSr.next()
                qp = qt - 16 * nn
                msk = qp <= 16
                P.pe.op(lambda e: e.matmul(ps[:], lhsT=kcr[:, g, nn * 128:(nn + 1) * 128], rhs=qsl, start=True, stop=not msk),
                        reads=[r_kcr, r_q[g]], writes=[r_ps])
                if msk:
                    P.pe.op(lambda e: e.matmul(ps[:], lhsT=ident_bf[:], rhs=bc(cmaskb[:, qp:qp + 1, :], [128, 4, 128]),
                                               start=False, stop=True), reads=[r_ident, r_c4], writes=[r_ps])
                ex, r_ex = ex_r.next()
                P.act.op(lambda e: e.activation(out=ex[:], in_=ps[:], func=AF.Exp, scale=0.125), reads=[r_ps], writes=[r_ex])
                exs.append((nn, ex, r_ex))
            firstc = True
            for (nn, ex, r_ex) in exs:
                for hh in range(4):
                    P.pe.op(lambda e: e.matmul(pOc[:, hh, :], lhsT=ex[:, hh, :], rhs=vov[:, g, nn, :],
                                               start=firstc, stop=False, skip_group_check=True),
                            reads=[r_ex, r_vov], writes=[r_pOc])
                    firstc = False
            firstc = True
            for (nn, ex, r_ex) in exs:
                for hh in range(4):
                    P.pe.op(lambda e: e.matmul(pImp[:, hh, :], lhsT=ex[:, hh, :], rhs=ovT[:, nn, :],
                                               start=firstc, stop=False, skip_group_check=True),
                            reads=[r_ex, r_c4], writes=[r_pImp])
                    firstc = False
            sm, r_sm = sm_r.next()
            P.dve.op(lambda e: e.tensor_scalar(out=sm[:, 0:4], in0=pOc[:, :, 64], scalar1=1e-6, scalar2=None, op0=ALU.max),
                     reads=[r_pOc], writes=[r_sm])
            P.dve.op(lambda e: e.reciprocal(out=sm[:, 4:8], in_=sm[:, 0:4]), reads=[r_sm], writes=[r_sm])
            oc, r_oc = oc_r.next()
            P.dve.op(lambda e: e.tensor_tensor(out=oc[:], in0=pOc[:, :, 0:64], in1=bc(sm[:, 4:8].unsqueeze(2), [128, 4, 64]),
                                               op=ALU.mult), reads=[r_pOc, r_sm], writes=[r_oc])
            st["oc"], st["r_oc"] = oc, r_oc
            imp, r_imp = imp_r.next()
            P.dve.op(lambda e: e.tensor_scalar(out=imp[:], in0=pImp[:, 0, :], scalar1=sm[:, 4:5], scalar2=None, op0=ALU.mult),
                     reads=[r_pImp, r_sm], writes=[r_imp])
            for hh in range(1, 4):
                P.dve.op(lambda e: e.scalar_tensor_tensor(out=imp[:], in0=pImp[:, hh, :], scalar=sm[:, 4 + hh:5 + hh],
                                                          in1=imp[:], op0=ALU.mult, op1=ALU.add),
                         reads=[r_pImp, r_sm, r_imp], writes=[r_imp])
            sc, r_sc = sc_r.next()
            o0 = 62 - 2 * qt
            P.dve.op(lambda e: e.tensor_tensor(out=sc[:], in0=imp[:], in1=rkeep[:, o0:o0 + 64], op=ALU.mult),
                     reads=[r_imp, r_c4], writes=[r_sc])
            P.dve.op(lambda e: e.tensor_tensor(out=sc[:], in0=sc[:], in1=radd[:, o0:o0 + 64], op=ALU.add),
                     reads=[r_sc, r_c4], writes=[r_sc])
            P.dve.op(lambda e: e.memset(sc[:, 0:1], 1e6), reads=[r_sc], writes=[r_sc])
            m8, r_m8 = m8_r.next()
            sc2, r_sc2 = sc2_r.next()
            P.dve.op(lambda e: e.max(out=m8[:, 0:8], in_=sc[:]), reads=[r_sc], writes=[r_m8])
            P.dve.op(lambda e: e.match_replace(out=sc2[:], in_to_replace=m8[:, 0:8], in_values=sc[:], imm_value=-1e30),
                     reads=[r_sc, r_m8], writes=[r_sc2])
            P.dve.op(lambda e: e.max(out=m8[:, 8:16], in_=sc2[:]), reads=[r_sc2, r_m8], writes=[r_m8])
            sel, r_sel = sel_r.next()
            P.dve.op(lambda e: e.tensor_scalar(out=sel[:, 64:128], in0=sc[:], scalar1=m8[:, 15:16], scalar2=-BIG,
                                               op0=ALU.is_lt, op1=ALU.mult),
                     reads=[r_sc, r_m8], writes=[r_sel])
            st["sel"], st["r_sel"] = sel, r_sel
            st["r_sb"] = Res()
            return st

        def prologue_b(g, qt, st):
            sel, r_sel = st["sel"], st["r_sel"]
            P.pe.op(lambda e: e.transpose(out=pMTb[:, 0:128], in_=sel[:], identity=ident_bf[:]),
                    reads=[r_sel, r_ident], writes=[r_pT4])
            r_sb = st["r_sb"]
            P.dve.op(lambda e: e.tensor_copy(out=qs[g][64:128, :, qt * 128:(qt + 1) * 128],
                                             in_=bc(pMTb[64:128, 0:128].unsqueeze(1), [64, 4, 128])),
                     reads=[r_pT4], writes=[r_sb])

        def attend(g, qt, st):
            units = [("s", kb) for kb in range(qt + 1)] + [("w", kb) for kb in range(max(0, qt - 4), qt + 1)]
            pend = []

            def s1(kind, kb):
                ps, r_ps = pSr.next()
                if kind == "s":
                    diag = (kb == qt)
                    P.pe.op(lambda e: e.matmul(ps[:], lhsT=kse[g][:, kb * 128:(kb + 1) * 128],
                                               rhs=qs[g][:, :, qt * 128:(qt + 1) * 128], start=True, stop=not diag),
                            reads=[r_kse[g], r_q[g], st["r_sb"]], writes=[r_ps])
                    if diag:
                        P.pe.op(lambda e: e.matmul(ps[:], lhsT=ident_bf[:], rhs=bc(cb_le[:].unsqueeze(1), [128, 4, 128]),
                                                   start=False, stop=True), reads=[r_ident, r_c4], writes=[r_ps])
                else:
                    m = cb_le if kb == qt else (cb_gt if kb == qt - 4 else None)
                    P.pe.op(lambda e: e.matmul(ps[:], lhsT=kw2[:, g, kb * 128:(kb + 1) * 128],
                                               rhs=qs[g][0:64, :, qt * 128:(qt + 1) * 128], start=True, stop=(m is None)),
                            reads=[r_kw, r_q[g]], writes=[r_ps])
                    if m is not None:
                        P.pe.op(lambda e: e.matmul(ps[:], lhsT=ident_bf[:], rhs=bc(m[:].unsqueeze(1), [128, 4, 128]),
                                                   start=False, stop=True), reads=[r_ident, r_c4], writes=[r_ps])
                ex, r_ex = ex_r.next()
                P.act.op(lambda e: e.activation(out=ex[:], in_=ps[:], func=AF.Exp, scale=0.125), reads=[r_ps], writes=[r_ex])
                return (kind, kb, ex, r_ex)

            def s2(kind, kb, ex, r_ex):
                if kind == "s":
                    po, r_po, v1, r_v1, first = pOs, r_pOs, vs1, r_vs, (kb == 0)
                else:
                    po, r_po, v1, r_v1, first = pOw, r_pOw, vw1, r_vw, (kb == max(0, qt - 4))
                for hh in range(4):
                    P.pe.op(lambda e: e.matmul(po[:, hh, :], lhsT=ex[:, hh, :], rhs=v1[:, g, kb, :],
                                               start=(first and hh == 0), stop=False, skip_group_check=True),
                            reads=[r_ex, r_v1], writes=[r_po])

            for i in range(len(units) + 2):
                if i < len(units):
                    pend.append(s1(*units[i]))
                if 0 <= i - 2 < len(units):
                    s2(*pend[i - 2])

        def combine(g, qt, st):
            gt_, r_gt = st["gt"], st["r_gt"]
            gv = gt_[:, 0:12].rearrange("p (h b) -> p h b", b=3)
            cf, r_cf = cf_r.next()
            P.dve.op(lambda e: e.reciprocal(out=cf[:, 0:4], in_=pOs[:, :, 64]), reads=[r_pOs], writes=[r_cf])
            P.dve.op(lambda e: e.reciprocal(out=cf[:, 4:8], in_=pOw[:, :, 64]), reads=[r_pOw, r_cf], writes=[r_cf])
            P.dve.op(lambda e: e.tensor_tensor(out=cf[:, 8:12], in0=cf[:, 0:4], in1=gv[:, :, 1], op=ALU.mult),
                     reads=[r_cf, r_gt], writes=[r_cf])
            P.dve.op(lambda e: e.tensor_tensor(out=cf[:, 12:16], in0=cf[:, 4:8], in1=gv[:, :, 2], op=ALU.mult),
                     reads=[r_cf, r_gt], writes=[r_cf])
            ta, r_ta = ta_r.next()
            tb, r_tb = tb_r.next()
            on, r_on = on_r.next()
            P.dve.op(lambda e: e.tensor_tensor(out=ta[:], in0=pOs[:, :, 0:64], in1=bc(cf[:, 8:12].unsqueeze(2), [128, 4, 64]),
                                               op=ALU.mult), reads=[r_pOs, r_cf], writes=[r_ta])
            P.dve.op(lambda e: e.tensor_tensor(out=tb[:], in0=pOw[:, :, 0:64], in1=bc(cf[:, 12:16].unsqueeze(2), [128, 4, 64]),
                                               op=ALU.mult), reads=[r_pOw, r_cf], writes=[r_tb])
            P.pool.op(lambda e: e.tensor_tensor(out=on[:], in0=st["oc"][:], in1=bc(gv[:, :, 0:1], [128, 4, 64]), op=ALU.mult),
                      reads=[st["r_oc"], r_gt], writes=[r_on])
            P.pool.op(lambda e: e.tensor_tensor(out=ta[:], in0=ta[:], in1=tb[:], op=ALU.add), reads=[r_ta, r_tb], writes=[r_ta])
            P.pool.op(lambda e: e.tensor_tensor(out=on[:], in0=on[:], in1=ta[:], op=ALU.add), reads=[r_on, r_ta], writes=[r_on])
            P.dma(o_nsa_d[qt * 128:(qt + 1) * 128, 256 * g:256 * (g + 1)], on[:].rearrange("p h d -> p (h d)"), reads=[r_on])

        tiles = [(g, qt) for g in range(2) for qt in range(NT)]
        if nsa_tiles is not None:
            tiles = nsa_tiles
        stn = prologue(*tiles[0])
        prologue_b(*tiles[0], stn)
        for i, (g, qt) in enumerate(tiles):
            stc = stn
            if i + 1 < len(tiles):
                stn = prologue(*tiles[i + 1])
            attend(g, qt, stc)
            if i + 1 < len(tiles):
                prologue_b(*tiles[i + 1], stn)
            combine(g, qt, stc)
        S.close()

    def rstd_ops(st, r_st, c0, n, dim):
        P.act.op(lambda e: e.activation(out=st[:, c0 + n:c0 + 2 * n], in_=st[:, c0:c0 + n], func=AF.Ln,
                                        scale=1.0 / dim, bias=eps_t[:, 0:1]), reads=[r_st, r_eps], writes=[r_st])
        P.act.op(lambda e: e.activation(out=st[:, c0 + 2 * n:c0 + 3 * n], in_=st[:, c0 + n:c0 + 2 * n], func=AF.Exp,
                                        scale=-0.5), reads=[r_st], writes=[r_st])

    def phase5a(l, x_src):
        S = Scope(P)
        wo = S.sbuf("wo_sb", [128, 8, D], BF16)
        r_wo = Res()
        P.dma_group([(wo[:, k, :], w_out_d[l, k * 128:(k + 1) * 128, :], P.pool, None) for k in range(8)], writes=[r_wo])
        gbc = S.sbuf("gbc", [128, D], F32)
        g1bc = S.sbuf("g1bc", [128, D], F32)
        r_bc = Res()
        P.dma_group([(gbc[:, 0:512], sbg_d[l].partition_broadcast(128), None, None),
                     (gbc[:, 512:1024], nsag_d[l].partition_broadcast(128), None, None),
                     (g1bc[:], modrow[l, 2 * D:3 * D].partition_broadcast(128), None, None)], writes=[r_bc])
        oin = Ring([S.sbuf(f"oin{i}", [128, D], F32) for i in range(3)])
        xin = Ring([S.sbuf(f"x5_{i}", [128, D], F32) for i in range(8)])
        junk = S.sbuf("junk5", [128, D], F32)
        r_junk = Res()
        stat = Ring([S.sbuf(f"st5_{i}", [128, 12], F32) for i in range(12)])
        mixed = Ring([S.sbuf(f"mixed{i}", [128, D], BF16) for i in range(3)])
        mixT = Ring([S.sbuf(f"mixT{i}", [128, 8, 128], BF16) for i in range(3)])
        tmp = Ring([S.sbuf(f"tmp5_{i}", [128, D], F32) for i in range(5)])
        xn2 = Ring([S.sbuf(f"xn2_{i}", [128, D], BF16) for i in range(3)])
        h2 = Ring([S.sbuf(f"h2_{i}", [128, 8, 512], BF16) for i in range(2)])
        pT_r = Ring([S.psum(f"pT5{i}", [128, 8, 128], BF16) for i in range(4)], excl=True)
        pa_r = Ring([S.psum(f"pa5{i}", [128, 512], F32) for i in range(4)], excl=True)
        B2 = modT[l][:, 24:32]
        ld = {}

        def load(ti):
            o, r_o = oin.next()
            xt, r_x = xin.next()
            rows = slice(ti * 128, (ti + 1) * 128)
            P.dma_group([(o[:, 0:512], o_sb_d[rows, :], None, None), (o[:, 512:1024], o_nsa_d[rows, :], None, None)],
                        writes=[r_o])
            P.dma(xt[:], x_src[rows, :], writes=[r_x])
            ld[ti] = (o, r_o, xt, r_x)

        def g1(ti, d):
            o, r_o, xt, r_x = ld.pop(ti)
            if ti + 1 < NT:
                load(ti + 1)
            st, r_st = stat.next()
            for hf in range(2):
                P.act.op(lambda e: e.activation(out=junk[:, 0:512], in_=o[:, hf * 512:(hf + 1) * 512], func=AF.Square,
                                                accum_out=st[:, hf:hf + 1]), reads=[r_o], writes=[r_junk, r_st])
            rstd_ops(st, r_st, 0, 2, 512)
            d.update(o=o, r_o=r_o, xt=xt, r_x=r_x, st=st, r_st=r_st)

        def g2(ti, d):
            o, r_o, st, r_st = d["o"], d["r_o"], d["st"], d["r_st"]
            mx, r_mx = mixed.next()
            for hf in range(2):
                P.dve.op(lambda e: e.scalar_tensor_tensor(out=mx[:, hf * 512:(hf + 1) * 512], in0=o[:, hf * 512:(hf + 1) * 512],
                                                          scalar=st[:, 4 + hf:5 + hf], in1=gbc[:, hf * 512:(hf + 1) * 512],
                                                          op0=ALU.mult, op1=ALU.mult),
                         reads=[r_o, r_st, r_bc], writes=[r_mx])
            d.update(mx=mx, r_mx=r_mx)

        def g3(ti, d):
            mx, r_mx = d["mx"], d["r_mx"]
            pT, r_pT = pT_r.next()
            for k in range(8):
                P.pe.op(lambda e: e.transpose(out=pT[:, k, :], in_=mx[:, k * 128:(k + 1) * 128], identity=ident_bf[:]),
                        reads=[r_mx, r_ident], writes=[r_pT])
            d.update(pT=pT, r_pT=r_pT)

        def g4(ti, d):
            mT, r_mT = mixT.next()
            P.act.op(lambda e: e.activation(out=mT[:], in_=d["pT"][:], func=AF.Copy), reads=[d["r_pT"]], writes=[r_mT])
            d.update(mT=mT, r_mT=r_mT)

        def g5(ti, d):
            mT, r_mT = d["mT"], d["r_mT"]
            pas = []
            for hf in range(2):
                pa, r_pa = pa_r.next()
                for k in range(8):
                    P.pe.op(lambda e: e.matmul(pa[:], lhsT=mT[:, k, :], rhs=wo[:, k, hf * 512:(hf + 1) * 512],
                                               start=(k == 0), stop=(k == 7)), reads=[r_mT, r_wo], writes=[r_pa])
                pas.append((pa, r_pa))
            d.update(pas=pas)

        def g6(ti, d):
            tm_, r_tm = tmp.next()
            for hf in range(2):
                pa, r_pa = d["pas"][hf]
                P.dve.op(lambda e: e.tensor_tensor(out=tm_[:, hf * 512:(hf + 1) * 512], in0=pa[:],
                                                   in1=g1bc[:, hf * 512:(hf + 1) * 512], op=ALU.mult),
                         reads=[r_pa, r_bc], writes=[r_tm])
            d.update(tm=tm_, r_tm=r_tm)

        def g7(ti, d):
            rows = slice(ti * 128, (ti + 1) * 128)
            tm_, r_tm, xt, r_x = d["tm"], d["r_tm"], d["xt"], d["r_x"]
            P.pool.op(lambda e: e.tensor_tensor(out=tm_[:], in0=tm_[:], in1=xt[:], op=ALU.add),
                      reads=[r_tm, r_x], writes=[r_tm])
            P.dma(x1_d[rows, :], tm_[:], reads=[r_tm])

        def g8(ti, d):
            tm_, r_tm, st, r_st = d["tm"], d["r_tm"], d["st"], d["r_st"]
            P.act.op(lambda e: e.activation(out=junk[:], in_=tm_[:], func=AF.Square, accum_out=st[:, 6:7]),
                     reads=[r_tm], writes=[r_junk, r_st])
            rstd_ops(st, r_st, 6, 1, D)

        def g9(ti, d):
            tm_, r_tm, st, r_st = d["tm"], d["r_tm"], d["st"], d["r_st"]
            xn, r_xn = xn2.next()
            P.dve.op(lambda e: e.tensor_scalar(out=xn[:], in0=tm_[:], scalar1=st[:, 8:9], scalar2=None, op0=ALU.mult),
                     reads=[r_tm, r_st], writes=[r_xn])
            d.update(xn=xn, r_xn=r_xn)

        def g10(ti, d):
            xn, r_xn = d["xn"], d["r_xn"]
            pT, r_pT = pT_r.next()
            for k in range(8):
                P.pe.op(lambda e: e.transpose(out=pT[:, k, :], in_=xn[:, k * 128:(k + 1) * 128], identity=ident_bf[:]),
                        reads=[r_xn, r_ident], writes=[r_pT])
            d.update(pT=pT, r_pT=r_pT)

        def g11(ti, d):
            pT, r_pT = d["pT"], d["r_pT"]
            if ti % 4 == 0:
                h2cur[0] = h2.next()
            hh4, r_hh = h2cur[0]
            hh = hh4[:, :, (ti % 4) * 128:(ti % 4 + 1) * 128]
            for k in range(8):
                if ti % 2 == 0:
                    P.act.op(lambda e: e.activation(out=hh[:, k, :], in_=pT[:, k, :], func=AF.Identity,
                                                    scale=A2[l][:, k:k + 1], bias=B2[:, k:k + 1]),
                             reads=[r_pT, r_A[l], r_modT[l]], writes=[r_hh])
                else:
                    P.dve.op(lambda e: e.tensor_scalar(out=hh[:, k, :], in0=pT[:, k, :], scalar1=A2[l][:, k:k + 1],
                                                       scalar2=B2[:, k:k + 1], op0=ALU.mult, op1=ALU.add),
                             reads=[r_pT, r_A[l], r_modT[l]], writes=[r_hh])
            if ti % 4 == 3:
                g0 = (ti // 4) * 512
                P.dma(h2T_d[:, g0:g0 + 512].rearrange("(k p) t -> p k t", p=128), hh4[:], reads=[r_hh])

        h2cur = [None]
        load(0)
        stages = [g1, g2, g3, g4, g5, g6, g7, g8, g9, g10, g11]
        sts = {}
        for it in range(NT + len(stages) - 1):
            for si, fn in enumerate(stages):
                ti = it - si
                if 0 <= ti < NT:
                    if si == 0:
                        sts[ti] = {}
                    fn(ti, sts[ti])
                    if si == len(stages) - 1:
                        sts.pop(ti)
        S.close()

    GT = 256
    NG = T // GT

    def phase5b(l, x_dst, final):
        S = Scope(P)
        wf = S.sbuf("wf_sb", [128, 8, 2 * DFF], BF16)
        wd = S.sbuf("wd_sb", [128, 22, D], BF16)
        r_wd = Res()
        r_wfb = [Res() for _ in range(4)]
        for blk in (0, 2, 1, 3):
            c0 = blk * 1408
            P.dma_group([(wf[:, k, c0:c0 + 1408], wffn_d[l, k * 128:(k + 1) * 128, c0:c0 + 1408], P.pool, None)
                         for k in range(8)], writes=[r_wfb[blk]])
            if blk == 2:
                P.dma_group([(wd[:, fc, :], wdn_d[l, fc * 128:(fc + 1) * 128, :], P.pool, None) for fc in range(22)],
                            writes=[r_wd])
        cw = S.sbuf("cw_sb", [128, 44, 3], F32)
        cb = S.sbuf("cb_sb", [128, 44], F32)
        g2bc = S.sbuf("g2bc", [128, D], F32)
        fgbc = S.sbuf("fgbc", [128, D], F32)
        r_cp = Res()
        P.dma_group([(cw[:], cw_d[l], None, None), (cb[:], cb_d[l], None, None),
                     (g2bc[:], modrow[l, 5 * D:6 * D].partition_broadcast(128), None, None),
                     (fgbc[:], fg_d.partition_broadcast(128), None, None)], writes=[r_cp])
        hal = S.sbuf("halo", [128, 44, 2], F32)
        r_hal = Res()
        P.pool.op(lambda e: e.memset(hal[:], 0.0), writes=[r_hal])
        r_hals = [Res() for _ in range(44)]
        for rr in r_hals:
            rr.w = list(r_hal.w)
        r_uhs = [Res() for _ in range(4)]
        h2 = Ring([S.sbuf(f"h2g{i}", [128, 8, GT], BF16) for i in range(2)])
        gT = S.sbuf("gT", [128, 22, GT], BF16)
        r_gT = Res()
        us = Ring([S.sbuf(f"us{i}", [128, GT + 2], F32) for i in range(4)])
        ys = Ring([S.sbuf(f"ys{i}", [128, GT], F32) for i in range(6)])
        sa_r = Ring([S.sbuf(f"sa{i}", [128, GT], F32) for i in range(2)])
        x1t = Ring([S.sbuf(f"x1t{i}", [128, D], F32) for i in range(2)])
        tmp = Ring([S.sbuf(f"tmpb{i}", [128, D], F32) for i in range(2)])
        junk = S.sbuf("junkb", [128, D], F32)
        r_junk = Res()
        stat = Ring([S.sbuf(f"stb{i}", [128, 4], F32) for i in range(2)])
        pu_r = Ring([S.psum(f"pu{i}", [128, GT], F32) for i in range(4)], excl=True)
        pd_r = Ring([S.psum(f"pd{i}", [128, 512], F32) for i in range(4)], excl=True)
        ld = {}

        def load(gi):
            h, r_h = h2.next()
            P.dma(h[:], h2T_d[:, gi * GT:(gi + 1) * GT].rearrange("(k p) t -> p k t", p=128), writes=[r_h])
            ld[gi] = (h, r_h)

        load(0)
        for gi in range(NG):
            h, r_h = ld.pop(gi)
            if gi + 1 < NG:
                load(gi + 1)
            pend_g = None

            def gate(fc_, ys_):
                sa, r_sa = sa_r.next()
                P.act.op(lambda e: e.activation(out=sa[:], in_=ys_[0][0][:], func=AF.Silu), reads=[ys_[0][1]], writes=[r_sa])
                P.dve.op(lambda e: e.tensor_tensor(out=gT[:, fc_, :], in0=sa[:], in1=ys_[1][0][:], op=ALU.mult),
                         reads=[r_sa, ys_[1][1]], writes=[r_gT])

            for fc in range(22):
                ysab = []
                uts = []
                for part in range(2):
                    ci = part * 22 + fc
                    ui = us.i
                    u, r_u = us.next()
                    r_uh = r_uhs[ui]
                    P.pool.op(lambda e: e.tensor_copy(out=u[:, 0:2], in_=hal[:, ci, :]), reads=[r_hals[ci]], writes=[r_uh])
                    uts.append((u, r_u, r_uh))
                for part in range(2):
                    ci = part * 22 + fc
                    pu, r_pu = pu_r.next()
                    for k in range(8):
                        P.pe.op(lambda e: e.matmul(pu[:], lhsT=wf[:, k, ci * 128:(ci + 1) * 128], rhs=h[:, k, :],
                                                   start=(k == 0), stop=(k == 7)),
                                reads=[r_wfb[(ci * 128) // 1408], r_h], writes=[r_pu])
                    u, r_u, r_uh = uts[part]
                    y, r_y = ys.next()
                    P.act.op(lambda e: e.activation(out=u[:, 2:GT + 2], in_=pu[:], func=AF.Copy), reads=[r_pu], writes=[r_u])
                    P.act.op(lambda e: e.activation(out=y[:], in_=pu[:], func=AF.Identity, scale=cw[:, ci, 2:3],
                                                    bias=cb[:, ci:ci + 1]), reads=[r_pu, r_cp], writes=[r_y])
                    P.act.op(lambda e: e.activation(out=hal[:, ci, :], in_=pu[:, GT - 2:GT], func=AF.Copy),
                             reads=[r_pu], writes=[r_hals[ci]])
                    P.dve.op(lambda e: e.scalar_tensor_tensor(out=y[:], in0=u[:, 1:GT + 1], scalar=cw[:, ci, 1:2], in1=y[:],
                                                              op0=ALU.mult, op1=ALU.add),
                             reads=[r_u, r_uh, r_cp, r_y], writes=[r_y])
                    P.dve.op(lambda e: e.scalar_tensor_tensor(out=y[:], in0=u[:, 0:GT], scalar=cw[:, ci, 0:1], in1=y[:],
                                                              op0=ALU.mult, op1=ALU.add),
                             reads=[r_u, r_uh, r_cp, r_y], writes=[r_y])
                    ysab.append((y, r_y))
                if pend_g is not None:
                    gate(*pend_g)
                pend_g = (fc, ysab)
            gate(*pend_g)
            for ts in range(GT // 128):
                ti = gi * (GT // 128) + ts
                rows = slice(ti * 128, (ti + 1) * 128)
                x1, r_x1 = x1t.next()
                P.dma(x1[:], x1_d[rows, :], writes=[r_x1])
                tm_, r_tm = tmp.next()
                for hf in range(2):
                    pd, r_pd = pd_r.next()
                    for fc in range(22):
                        P.pe.op(lambda e: e.matmul(pd[:], lhsT=gT[:, fc, ts * 128:(ts + 1) * 128],
                                                   rhs=wd[:, fc, hf * 512:(hf + 1) * 512], start=(fc == 0), stop=(fc == 21)),
                                reads=[r_gT, r_wd], writes=[r_pd])
                    P.dve.op(lambda e: e.tensor_tensor(out=tm_[:, hf * 512:(hf + 1) * 512], in0=pd[:],
                                                       in1=g2bc[:, hf * 512:(hf + 1) * 512], op=ALU.mult),
                             reads=[r_pd, r_cp], writes=[r_tm])
                P.pool.op(lambda e: e.tensor_tensor(out=tm_[:], in0=tm_[:], in1=x1[:], op=ALU.add),
                          reads=[r_tm, r_x1], writes=[r_tm])
                if not final:
                    P.dma(x_dst[rows, :], tm_[:], reads=[r_tm])
                else:
                    st, r_st = stat.next()
                    P.act.op(lambda e: e.activation(out=junk[:], in_=tm_[:], func=AF.Square, accum_out=st[:, 0:1]),
                             reads=[r_tm], writes=[r_junk, r_st])
                    rstd_ops(st, r_st, 0, 1, D)
                    P.dve.op(lambda e: e.scalar_tensor_tensor(out=x1[:], in0=tm_[:], scalar=st[:, 2:3], in1=fgbc[:],
                                                              op0=ALU.mult, op1=ALU.mult),
                             reads=[r_tm, r_st, r_cp, r_x1], writes=[r_x1])
                    P.dma(x_dst[rows, :], x1[:], reads=[r_x1])
        S.close()

    if stop_after is None:
        for l in range(L):
            x_src = x_in if l == 0 else xbuf_d
            phase0(l)
            P.barrier()
            phase1(l, x_src)
            P.barrier()
            phase3(l)
            P.barrier()
            phase4(l)
            P.barrier()
            phase5a(l, x_src)
            P.barrier()
            phase5b(l, out_d if l == L - 1 else xbuf_d, final=(l == L - 1))
            P.barrier()
    else:
        phase0(0)
        P.barrier()
        phase1(0, x_in)
        P.barrier()
        if stop_after in ("p3", "l0"):
            phase3(0)
            P.barrier()
        if stop_after in ("p4", "l0"):
            phase4(0)
            P.barrier()
        if stop_after == "l0":
            phase5a(0, x_in)
            P.barrier()
            phase5b(0, xbuf_d, final=False)
            P.barrier()
        S = Scope(P)
        z = S.sbuf("zz", [128, D], F32)
        rz = Res()
        P.dve.op(lambda e: e.memset(z[:], 0.0), writes=[rz])
        for ti in range(NT):
            P.dma(out_d[ti * 128:(ti + 1) * 128, :], z[:], reads=[rz])
        S.close()
    P.finish()
    return nc


def host_inputs(inputs):
    cols = _ext_cols()
    f32 = lambda a: np.ascontiguousarray(np.asarray(a, dtype=np.float32))
    w_ext = f32(np.asarray(inputs["w_in"])[:, :, cols])
    shared = {
        "ln1T": f32(np.asarray(inputs["ln1_g"]).reshape(L, 8, 128).transpose(0, 2, 1)),
        "ln2T": f32(np.asarray(inputs["ln2_g"]).reshape(L, 8, 128).transpose(0, 2, 1)),
        "w_ada": f32(inputs["w_ada"]),
        "b_adaT": f32(np.asarray(inputs["b_ada"]).reshape(L, 48, 128).transpose(0, 2, 1)),
        "w_ext": w_ext,
        "cmp_w1_k": f32(inputs["cmp_w1_k"]), "cmp_w1_v": f32(inputs["cmp_w1_v"]),
        "cmp_w2_k": f32(inputs["cmp_w2_k"]), "cmp_w2_v": f32(inputs["cmp_w2_v"]),
        "cmp_w2_k_sw": f32(np.asarray(inputs["cmp_w2_k"])[:, :, _swap64(np.arange(64))]),
        "sb_out_g": f32(inputs["sb_out_g"]), "nsa_out_g": f32(inputs["nsa_out_g"]),
        "w_out": f32(inputs["w_out"]), "ffn_w_in": f32(inputs["ffn_w_in"]), "ffn_w_down": f32(inputs["ffn_w_down"]),
        "conv_w": f32(np.asarray(inputs["ffn_conv_w"]).reshape(L, 3, 44, 128).transpose(0, 3, 2, 1)),
        "conv_b": f32(np.asarray(inputs["ffn_conv_b"]).reshape(L, 44, 128).transpose(0, 2, 1)),
        "final_g": f32(inputs["final_g"]),
        "posTk": f32(np.asarray(inputs["cmp_pos_k"]).transpose(0, 2, 1)),
        "posTv": f32(np.asarray(inputs["cmp_pos_v"]).transpose(0, 2, 1)),
    }
    shared.update(_consts())
    x = np.asarray(inputs["x"])
    c = np.asarray(inputs["c"])
    in_maps = []
    for b in range(8):
        m = dict(shared)
        m["x"] = f32(x[b])
        m["cT"] = f32(c[b].reshape(8, 128).T)
        in_maps.append(m)
    return in_maps


def kernel(**inputs):
    in_maps = host_inputs(inputs)
    nc = build_program()
    res = run_bass_kernel_spmd(nc, in_maps, core_ids=list(range(8)))
    return np.stack([r["out"] for r in res.results], axis=0).astype(np.float32)
```

```python
import numpy as np
import ml_dtypes
import concourse.bass as bass
import concourse.mybir as mybir
from concourse.bass_utils import run_bass_kernel_spmd

F32 = mybir.dt.float32
BF16 = mybir.dt.bfloat16
AF = mybir.ActivationFunctionType
ALU = mybir.AluOpType
AX = mybir.AxisListType

T = 4096
D = 1024
L = 2
NT = T // 128
DFF = 2816
EPS = 1e-6
N_FM = 22 * 128
N_TM = 792
N_EXT = N_FM + N_TM


class Res:
    __slots__ = ("w", "r", "excl")

    def __init__(self, excl=False):
        self.w = []
        self.r = []
        self.excl = excl


class Eng:
    def __init__(self, name, e, sem, is_pe=False):
        self.name = name
        self.e = e
        self.sem = sem
        self.count = 0
        self.waited = {}
        self.is_pe = is_pe

    def _wait(self, tok):
        sem, val = tok
        k = id(sem)
        if self.waited.get(k, 0) >= val:
            return
        self.e.wait_ge(sem, val)
        self.waited[k] = val

    def _deps(self, reads, writes):
        for r in reads:
            for t in r.w:
                if not (self.is_pe and t[0] is self.sem):
                    self._wait(t)
            if r.excl:
                for t in r.r:
                    if t[0] is not self.sem:
                        self._wait(t)
        for r in writes:
            for t in r.w:
                if not (self.is_pe and t[0] is self.sem):
                    self._wait(t)
            for t in r.r:
                if t[0] is self.sem:
                    continue
                self._wait(t)

    def op(self, fn, reads=(), writes=()):
        self._deps(reads, writes)
        ins = fn(self.e)
        self.count += 1
        ins.then_inc(self.sem, 1)
        tok = (self.sem, self.count)
        for r in reads:
            r.r = [t for t in r.r if t[0] is not self.sem] + [tok]
        for r in writes:
            r.w = [tok]
            r.r = []
        return tok


class Prog:
    def __init__(self, nc, n_dma_sems=40):
        self.nc = nc
        self._ctx = []
        mk = lambda name: self._enter(nc.semaphore(name))
        self.pe = Eng("pe", nc.tensor, mk("s_pe"), is_pe=True)
        self.act = Eng("act", nc.scalar, mk("s_act"))
        self.dve = Eng("dve", nc.vector, mk("s_dve"))
        self.pool = Eng("pool", nc.gpsimd, mk("s_pool"))
        self.sp = Eng("sp", nc.sync, mk("s_sp"))
        self.dma_sems = [mk(f"s_dma{i}") for i in range(n_dma_sems)]
        self.dma_vals = [0] * n_dma_sems
        self.dma_rr = 0

    def _enter(self, cm):
        v = cm.__enter__()
        self._ctx.append(cm)
        return v

    def sbuf(self, name, shape, dtype):
        return self._enter(self.nc.sbuf_tensor(name, list(shape), dtype))

    def psum(self, name, shape, dtype=F32):
        return self._enter(self.nc.psum_tensor(name, list(shape), dtype))

    def dma_group(self, items, reads=(), writes=()):
        dep = []
        for r in reads:
            dep += r.w
        for r in writes:
            dep += r.w
            dep += r.r
        toks = []
        for it in items:
            out, in_, q, kw = (list(it) + [None, None])[:4]
            q = q or self.sp
            kw = kw or {}
            i = self.dma_rr
            self.dma_rr = (self.dma_rr + 1) % len(self.dma_sems)
            sem = self.dma_sems[i]
            if self.dma_vals[i] > 0:
                q._wait((sem, self.dma_vals[i]))
            for t in dep:
                q._wait(t)
            ins = q.e.dma_start(out=out, in_=in_, **kw)
            self.dma_vals[i] += 16
            ins.then_inc(sem, 16)
            toks.append((sem, self.dma_vals[i]))
        for r in reads:
            r.r = r.r + toks
        for r in writes:
            r.w = list(toks)
            r.r = []
        return toks

    def dma(self, out, in_, reads=(), writes=(), q=None, **kw):
        return self.dma_group([(out, in_, q, kw)], reads, writes)

    def barrier(self):
        engs = (self.pe, self.act, self.dve, self.pool, self.sp)
        for e in engs:
            for i, sem in enumerate(self.dma_sems):
                if self.dma_vals[i] > 0:
                    e._wait((sem, self.dma_vals[i]))
            for o in engs:
                if o is not e and o.count > 0:
                    e._wait((o.sem, o.count))

    def finish(self):
        self.barrier()
        for cm in reversed(self._ctx):
            cm.__exit__(None, None, None)
        self._ctx = []


class Scope:
    _uid = [0]

    def __init__(self, P):
        self.P = P
        self.cms = []
        Scope._uid[0] += 1
        self.sfx = f"_s{Scope._uid[0]}"

    def sbuf(self, name, shape, dtype):
        cm = self.P.nc.sbuf_tensor(name + self.sfx, list(shape), dtype)
        v = cm.__enter__()
        self.cms.append(cm)
        return v

    def psum(self, name, shape, dtype=F32):
        isz = 4 if dtype == F32 else 2
        cm = self.P.nc.psum_tensor(name + self.sfx, [128, 2048 // isz], dtype)
        v = cm.__enter__()
        self.cms.append(cm)
        n = int(np.prod(shape[1:]))
        view = v[0:shape[0], 0:n]
        if len(shape) == 3:
            view = view.rearrange("p (a b) -> p a b", b=shape[2])
        return view

    def close(self):
        for cm in reversed(self.cms):
            cm.__exit__(None, None, None)
        self.cms = []


class Ring:
    def __init__(self, tiles, excl=False):
        self.tiles = tiles
        self.res = [Res(excl) for _ in tiles]
        self.i = 0

    def next(self):
        t, r = self.tiles[self.i], self.res[self.i]
        self.i = (self.i + 1) % len(self.tiles)
        return t, r


def _swap64(cols):
    cols = np.asarray(cols).reshape(-1, 64)
    return np.concatenate([cols[:, 32:], cols[:, :32]], axis=1).reshape(-1)


def _ext_cols():
    r = lambda a, b: np.arange(a, b)
    sbq, sbk, sbv = r(0, 512), r(512, 1024), r(1024, 1536)
    nq, kc, vc = r(1536, 2048), r(2048, 2176), r(2176, 2304)
    ks, vs, kw, vw, gl = r(2304, 2432), r(2432, 2560), r(2560, 2688), r(2688, 2816), r(2816, 2840)
    fm = [sbq, sbk]
    for c in range(4):
        fm += [nq[c * 128:(c + 1) * 128], _swap64(nq[c * 128:(c + 1) * 128])]
    fm += [ks, _swap64(ks), kw, _swap64(kw), kc, vc]
    tm = [sbv, vs, vw, gl]
    cols = np.concatenate(fm + tm)
    assert cols.shape[0] == N_EXT
    return cols


def _consts():
    half = 32
    inv = (10000.0 ** (-np.arange(half, dtype=np.float32) / half)).astype(np.float32)
    pos = np.arange(T, dtype=np.float32)
    d = np.arange(128) % 64
    ang = (pos[None, :] * inv[d % 32][:, None]).astype(np.float32)
    cos = np.cos(ang).astype(np.float32)
    sin = np.sin(ang).astype(np.float32)
    sgn = np.where(d < 32, -1.0, 1.0).astype(np.float32)[:, None]
    c = {
        "rope_cos": cos, "rope_sin": (sin * sgn).astype(np.float32),
        "ident_bf": np.eye(128).astype(ml_dtypes.bfloat16),
        "ident_f32": np.eye(128).astype(np.float32),
    }
    j = np.arange(128)
    bf = ml_dtypes.bfloat16
    c["tri_ge"] = (j[:, None] >= j[None, :]).astype(bf)
    c["tri_lt"] = (j[:, None] < j[None, :]).astype(bf)
    c["tri_le"] = (j[:, None] <= j[None, :]).astype(bf)
    c["tri_gt"] = (j[:, None] > j[None, :]).astype(bf)
    n = np.arange(256, dtype=np.float32)
    angc = ((16.0 * n + 31.0)[None, :] * inv[d % 32][:, None]).astype(np.float32)
    c["cmp_cos"] = np.cos(angc).astype(np.float32)
    c["cmp_sin"] = (np.sin(angc).astype(np.float32) * sgn).astype(np.float32)
    nn = np.arange(128)[:, None, None]
    qq = np.arange(17)[None, :, None]
    tt = np.arange(128)[None, None, :]
    BIG = 240000.0
    c["cmaskb"] = np.where(16 * nn + 31 <= 128 * qq + tt, 0.0, -BIG).astype(bf)
    c["cb_le"] = np.where(j[:, None] <= j[None, :], 0.0, -BIG).astype(bf)
    c["cb_lt"] = np.where(j[:, None] < j[None, :], 0.0, -BIG).astype(bf)
    c["cb_gt"] = np.where(j[:, None] > j[None, :], 0.0, -BIG).astype(bf)
    ncmp = np.arange(256)[:, None]
    jj = np.arange(64)[None, :]
    ov = np.clip(np.minimum(16 * ncmp + 32, 64 * jj + 64) - np.maximum(16 * ncmp, 64 * jj), 0, None) / 32.0
    ov[255, :] = 0.0
    c["ovT"] = np.ascontiguousarray(ov.reshape(2, 128, 64).transpose(1, 0, 2)).astype(bf)
    sidx = np.arange(T)[None, :]
    c["eexp"] = (sidx // 64 == np.arange(64)[:, None]).astype(bf)
    ttq = np.arange(128)[:, None]
    rel = np.arange(128)[None, :] - 62
    dist = (ttq >= 64).astype(np.int64) - rel
    c["rkeep"] = (dist >= 2).astype(np.float32)
    c["radd"] = np.where(dist < 0, -1.0, np.where(dist <= 1, 1e6, 0.0)).astype(np.float32)
    return c


def build_program(dbg=None, stop_after=None, nsa_tiles=None, dbg_tile=None):
    nc = bass.Bass("TRN2", target_bir_lowering=False)
    dbg = dbg or []

    def din(name, shape, dt=F32):
        return nc.dram_tensor(name, list(shape), dt, kind="ExternalInput").ap()

    def dscr(name, shape, dt):
        kind = "ExternalOutput" if name in dbg else "Internal"
        return nc.dram_tensor(name, list(shape), dt, kind=kind).ap()

    x_in = din("x", [T, D])
    cT_in = din("cT", [128, 8])
    ln1T = din("ln1T", [L, 128, 8])
    ln2T = din("ln2T", [L, 128, 8])
    w_ada = din("w_ada", [L, D, 6 * D])
    b_adaT = din("b_adaT", [L, 128, 48])
    w_ext = din("w_ext", [L, D, N_EXT])
    rope_cos = din("rope_cos", [128, T])
    rope_sin = din("rope_sin", [128, T])
    ident_bf_d = din("ident_bf", [128, 128], BF16)
    ident_f_d = din("ident_f32", [128, 128])
    tri_d = {n: din(n, [128, 128], BF16) for n in ("tri_ge", "tri_lt", "tri_le", "tri_gt", "cb_lt")}
    w1k_d = din("cmp_w1_k", [L, 2048, 256])
    w1v_d = din("cmp_w1_v", [L, 2048, 256])
    w2k_d = din("cmp_w2_k", [L, 256, 64])
    w2ksw_d = din("cmp_w2_k_sw", [L, 256, 64])
    w2v_d = din("cmp_w2_v", [L, 256, 64])
    posTk_d = din("posTk", [L, 64, 32])
    posTv_d = din("posTv", [L, 64, 32])
    sbg_d = din("sb_out_g", [L, 512])
    nsag_d = din("nsa_out_g", [L, 512])
    w_out_d = din("w_out", [L, D, D])
    wffn_d = din("ffn_w_in", [L, D, 2 * DFF])
    wdn_d = din("ffn_w_down", [L, DFF, D])
    cw_d = din("conv_w", [L, 128, 44, 3])
    cb_d = din("conv_b", [L, 128, 44])
    fg_d = din("final_g", [D])
    cmp_cos_d = din("cmp_cos", [128, 256])
    cmp_sin_d = din("cmp_sin", [128, 256])
    cmaskb_d = din("cmaskb", [128, 17, 128], BF16)
    cb_le_d = din("cb_le", [128, 128], BF16)
    cb_gt_d = din("cb_gt", [128, 128], BF16)
    ovT_d = din("ovT", [128, 2, 64], BF16)
    eexp_d = din("eexp", [64, T], BF16)
    rkeep_d = din("rkeep", [128, 128])
    radd_d = din("radd", [128, 128])
    out_d = nc.dram_tensor("out", [T, D], F32, kind="ExternalOutput").ap()

    o_sb_d = dscr("o_sb", [T, 512], F32)
    o_nsa_d = dscr("o_nsa", [T, 512], F32)
    xbuf_d = dscr("xbuf", [T, D], F32)
    x1_d = dscr("x1buf", [T, D], F32)
    h2T_d = dscr("h2T", [D, T], BF16)
    fmT = dscr("fmT", [2048, T], BF16)
    tmv = dscr("tmv", [T, 768], BF16)
    gates_d = dscr("gates", [T, 24], F32)
    modrow = dscr("modrow", [L, 6 * D], F32)

    P = Prog(nc)
    def dump(name, ap, res):
        if name not in dbg:
            return
        shp = [int(v) for v in ap.shape]
        dt_ = ap.dtype
        d = nc.dram_tensor(name, shp, dt_, kind="ExternalOutput").ap()
        P.dma(d, ap, reads=[res])

    ident_bf = P.sbuf("ident_bf_sb", [128, 128], BF16)
    r_ident = Res()
    P.dma(ident_bf[:], ident_bf_d[:, :], writes=[r_ident])
    ident_f = P.sbuf("ident_f_sb", [128, 128], F32)
    r_identf = Res()
    P.dma(ident_f[:], ident_f_d[:, :], writes=[r_identf])
    tri = {}
    r_tri = Res()
    for n in tri_d:
        tri[n] = P.sbuf(n + "_sb", [128, 128], BF16)
    P.dma_group([(tri[n][:], tri_d[n][:, :], None, None) for n in tri_d], writes=[r_tri])
    eps_t = P.sbuf("eps_t", [128, 1], F32)
    r_eps = Res()
    P.dve.op(lambda e: e.memset(eps_t[:], EPS), writes=[r_eps])
    cT = P.sbuf("cT_sb", [128, 8], F32)
    r_cT = Res()
    P.dma(cT[:], cT_in[:, :], writes=[r_cT])
    siluc = P.sbuf("siluc", [128, 8], F32)
    r_siluc = Res()
    P.act.op(lambda e: e.activation(out=siluc[:], in_=cT[:], func=AF.Silu), reads=[r_cT], writes=[r_siluc])
    modT = [P.sbuf(f"modT{l}", [128, 48], F32) for l in range(L)]
    r_modT = [Res() for _ in range(L)]
    A1 = [P.sbuf(f"A1_{l}", [128, 8], F32) for l in range(L)]
    A2 = [P.sbuf(f"A2_{l}", [128, 8], F32) for l in range(L)]
    r_A = [Res() for _ in range(L)]
    r_modrow = [Res() for _ in range(L)]

    def phase0(l):
        S = Scope(P)
        wbuf = Ring([S.sbuf(f"wada{i}", [128, 6 * D], F32) for i in range(2)])
        pm = S.psum("pmod", [128, 48], F32)
        r_pm = Res(True)
        lnt = S.sbuf("lnt", [128, 16], F32)
        r_lnt = Res()
        bT = S.sbuf("bT", [128, 48], F32)
        r_bT = Res()
        P.dma(lnt[:, 0:8], ln1T[l], writes=[r_lnt])
        P.dma(lnt[:, 8:16], ln2T[l], writes=[r_lnt])
        P.dma(bT[:], b_adaT[l], writes=[r_bT])
        for k in range(8):
            wt, rw = wbuf.next()
            P.dma_group([(wt[:, hf * 3072:(hf + 1) * 3072],
                          w_ada[l, k * 128:(k + 1) * 128, hf * 3072:(hf + 1) * 3072], None, None)
                         for hf in range(2)], writes=[rw])
            for m in range(48):
                P.pe.op(lambda e: e.matmul(pm[:, m:m + 1], lhsT=wt[:, m * 128:(m + 1) * 128], rhs=siluc[:, k:k + 1],
                                           start=(k == 0 and m == 0), stop=(k == 7 and m == 47),
                                           skip_group_check=True),
                        reads=[rw, r_siluc], writes=[r_pm])
        P.dve.op(lambda e: e.tensor_tensor(out=modT[l][:], in0=pm[:], in1=bT[:], op=ALU.add),
                 reads=[r_pm, r_bT], writes=[r_modT[l]])
        P.dve.op(lambda e: e.scalar_tensor_tensor(out=A1[l][:], in0=modT[l][:, 8:16], scalar=1.0, in1=lnt[:, 0:8],
                                                  op0=ALU.add, op1=ALU.mult),
                 reads=[r_modT[l], r_lnt], writes=[r_A[l]])
        P.dve.op(lambda e: e.scalar_tensor_tensor(out=A2[l][:], in0=modT[l][:, 32:40], scalar=1.0, in1=lnt[:, 8:16],
                                                  op0=ALU.add, op1=ALU.mult),
                 reads=[r_modT[l], r_lnt], writes=[r_A[l]])
        pmt = S.psum("pmt", [48, 128], F32)
        r_pmt = Res(True)
        P.pe.op(lambda e: e.transpose(out=pmt[:], in_=modT[l][:], identity=ident_f[:]),
                reads=[r_modT[l], r_identf], writes=[r_pmt])
        mrow = S.sbuf("mrow", [48, 128], F32)
        r_mrow = Res()
        P.dve.op(lambda e: e.tensor_copy(out=mrow[:], in_=pmt[:]), reads=[r_pmt], writes=[r_mrow])
        P.dma(modrow[l].rearrange("(m p) -> m p", p=128), mrow[:], reads=[r_mrow])
        S.close()

    def phase1(l, x_src):
        S = Scope(P)
        wsb = S.sbuf("w1sb", [128, 8, N_EXT], BF16)
        blocks = [(N_FM, N_EXT), (0, 1024), (1024, 2048), (2048, N_FM)]
        r_wb = [Res() for _ in blocks]
        for (c0, c1), rw in zip(blocks, r_wb):
            P.dma_group([(wsb[:, k, c0:c1], w_ext[l, k * 128:(k + 1) * 128, c0:c1], P.pool, None) for k in range(8)],
                        writes=[rw])

        def r_wcols(c0):
            for (b0, b1), rw in zip(blocks, r_wb):
                if b0 <= c0 < b1:
                    return rw
            raise AssertionError

        cosb = S.sbuf("cosb", [128, T], F32)
        sinb = S.sbuf("sinb", [128, T], F32)
        r_rope = Res()
        P.dma(cosb[:], rope_cos[:, :], writes=[r_rope])
        r_rope2 = Res()
        P.dma(sinb[:], rope_sin[:, :], writes=[r_rope2])
        xt = Ring([S.sbuf(f"xt{i}", [128, D], F32) for i in range(3)])
        junk = S.sbuf("junk", [128, D], F32)
        r_junk = Res()
        stat = Ring([S.sbuf(f"stat{i}", [128, 4], F32) for i in range(4)])
        xn = Ring([S.sbuf(f"xn{i}", [128, D], BF16) for i in range(3)])
        hT = Ring([S.sbuf(f"hT{i}", [128, 8, 512], BF16) for i in range(2)])
        stg = Ring([S.sbuf(f"stg{i}", [128, 512], BF16) for i in range(6)])
        stgtm = Ring([S.sbuf(f"stgtm{i}", [128, 768], BF16) for i in range(2)])
        gst = Ring([S.sbuf(f"gst{i}", [128, 24], F32) for i in range(2)])
        rt1 = Ring([S.sbuf(f"rt1_{i}", [128, 512], F32) for i in range(2)])
        rt2 = Ring([S.sbuf(f"rt2_{i}", [128, 512], F32) for i in range(2)])
        pT_r = Ring([S.psum(f"pT{i}", [128, 8, 128], BF16) for i in range(2)], excl=True)
        ptm_r = Ring([S.psum(f"ptm{i}", [128, 512], F32) for i in range(2)], excl=True)
        pfm = Ring([S.psum(f"pfm{i}", [128, 512], F32) for i in range(4)], excl=True)
        B1 = modT[l][:, 0:8]
        xl = {}
        hcur = {}

        def ldx(ti):
            xtile, r_x = xt.next()
            P.dma(xtile[:], x_src[ti * 128:(ti + 1) * 128, :], writes=[r_x])
            xl[ti] = (xtile, r_x)

        def g1(ti, d):
            xtile, r_x = xl.pop(ti)
            if ti + 1 < NT:
                ldx(ti + 1)
            st, r_st = stat.next()
            P.act.op(lambda e: e.activation(out=junk[:], in_=xtile[:], func=AF.Square, accum_out=st[:, 0:1]),
                     reads=[r_x], writes=[r_junk, r_st])
            rstd_ops(st, r_st, 0, 1, D)
            d.update(xtile=xtile, r_x=r_x, st=st, r_st=r_st)

        def g2(ti, d):
            xnt, r_xn = xn.next()
            P.dve.op(lambda e: e.tensor_scalar(out=xnt[:], in0=d["xtile"][:], scalar1=d["st"][:, 2:3], scalar2=None,
                                               op0=ALU.mult), reads=[d["r_x"], d["r_st"]], writes=[r_xn])
            d.update(xnt=xnt, r_xn=r_xn)

        def g3(ti, d):
            xnt, r_xn = d["xnt"], d["r_xn"]
            pT, r_pT = pT_r.next()
            for k in range(8):
                P.pe.op(lambda e: e.transpose(out=pT[:, k, :], in_=xnt[:, k * 128:(k + 1) * 128], identity=ident_bf[:]),
                        reads=[r_xn, r_ident], writes=[r_pT])
            d.update(pT=pT, r_pT=r_pT)

        def g4(ti, d):
            grp, tt = divmod(ti, 4)
            if tt == 0:
                hcur[grp] = hT.next()
            h, r_h = hcur[grp]
            pT, r_pT = d["pT"], d["r_pT"]
            for k in range(8):
                dst = h[:, k, tt * 128:(tt + 1) * 128]
                if ti % 2 == 0:
                    P.act.op(lambda e: e.activation(out=dst, in_=pT[:, k, :], func=AF.Identity,
                                                    scale=A1[l][:, k:k + 1], bias=B1[:, k:k + 1]),
                             reads=[r_pT, r_A[l], r_modT[l]], writes=[r_h])
                else:
                    P.dve.op(lambda e: e.tensor_scalar(out=dst, in0=pT[:, k, :], scalar1=A1[l][:, k:k + 1],
                                                       scalar2=B1[:, k:k + 1], op0=ALU.mult, op1=ALU.add),
                             reads=[r_pT, r_A[l], r_modT[l]], writes=[r_h])

        def g5(ti, d):
            grp, tt = divmod(ti, 4)
            h, r_h = hcur[grp]
            p0, r_p0 = ptm_r.next()
            p1, r_p1 = ptm_r.next()
            for k in range(8):
                lhsT = h[:, k, tt * 128:(tt + 1) * 128]
                P.pe.op(lambda e: e.matmul(p0[:], lhsT=lhsT, rhs=wsb[:, k, N_FM:N_FM + 512],
                                           start=(k == 0), stop=(k == 7)), reads=[r_h, r_wb[0]], writes=[r_p0])
            for k in range(8):
                lhsT = h[:, k, tt * 128:(tt + 1) * 128]
                P.pe.op(lambda e: e.matmul(p1[:, 0:280], lhsT=lhsT, rhs=wsb[:, k, N_FM + 512:N_EXT],
                                           start=(k == 0), stop=(k == 7)), reads=[r_h, r_wb[0]], writes=[r_p1])
            d.update(p0=p0, r_p0=r_p0, p1=p1, r_p1=r_p1)

        def g6(ti, d):
            p0, r_p0, p1, r_p1 = d["p0"], d["r_p0"], d["p1"], d["r_p1"]
            sg, r_sg = stgtm.next()
            P.act.op(lambda e: e.activation(out=sg[:, 0:512], in_=p0[:], func=AF.Copy), reads=[r_p0], writes=[r_sg])
            P.dve.op(lambda e: e.tensor_copy(out=sg[:, 512:768], in_=p1[:, 0:256]), reads=[r_p1], writes=[r_sg])
            gs, r_gs = gst.next()
            P.act.op(lambda e: e.activation(out=gs[:], in_=p1[:, 256:280], func=AF.Sigmoid), reads=[r_p1], writes=[r_gs])
            P.dma(tmv[ti * 128:(ti + 1) * 128, :], sg[:], reads=[r_sg])
            P.dma(gates_d[ti * 128:(ti + 1) * 128, :], gs[:], reads=[r_gs])

        evc = [0]

        def fm_group(grp):
            h, r_h = hcur[grp]
            tsl = slice(grp * 512, (grp + 1) * 512)

            def fm_mm(ch):
                pf, r_pf = pfm.next()
                rw = r_wcols(ch * 128)
                for k in range(8):
                    P.pe.op(lambda e: e.matmul(pf[:], lhsT=wsb[:, k, ch * 128:(ch + 1) * 128], rhs=h[:, k, :],
                                               start=(k == 0), stop=(k == 7)), reads=[r_h, rw], writes=[r_pf])
                return pf, r_pf

            for ch in range(8):
                pf, r_pf = fm_mm(ch)
                s_, r_s = stg.next()
                if evc[0] % 2 == 0:
                    P.act.op(lambda e: e.activation(out=s_[:], in_=pf[:], func=AF.Copy), reads=[r_pf], writes=[r_s])
                else:
                    P.dve.op(lambda e: e.tensor_copy(out=s_[:], in_=pf[:]), reads=[r_pf], writes=[r_s])
                evc[0] += 1
                P.dma(fmT[ch * 128:(ch + 1) * 128, tsl], s_[:], reads=[r_s])
            for pr in range(6):
                pa, r_pa = fm_mm(8 + 2 * pr)
                pb, r_pb = fm_mm(9 + 2 * pr)
                t1, r_t1 = rt1.next()
                t2, r_t2 = rt2.next()
                P.dve.op(lambda e: e.tensor_tensor(out=t1[:], in0=pa[:], in1=cosb[:, tsl], op=ALU.mult),
                         reads=[r_pa, r_rope], writes=[r_t1])
                P.dve.op(lambda e: e.tensor_tensor(out=t2[:], in0=pb[:], in1=sinb[:, tsl], op=ALU.mult),
                         reads=[r_pb, r_rope2], writes=[r_t2])
                s_, r_s = stg.next()
                P.pool.op(lambda e: e.tensor_tensor(out=s_[:], in0=t1[:], in1=t2[:], op=ALU.add),
                          reads=[r_t1, r_t2], writes=[r_s])
                P.dma(fmT[1024 + pr * 128:1024 + (pr + 1) * 128, tsl], s_[:], reads=[r_s])
            for j in range(2):
                pf, r_pf = fm_mm(20 + j)
                s_, r_s = stg.next()
                P.act.op(lambda e: e.activation(out=s_[:], in_=pf[:], func=AF.Copy), reads=[r_pf], writes=[r_s])
                P.dma(fmT[1792 + j * 128:1792 + (j + 1) * 128, tsl], s_[:], reads=[r_s])

        ldx(0)
        stages = [g1, g2, g3, g4, g5, g6]
        sts = {}
        for it in range(NT + len(stages) - 1):
            fm_after = None
            for si, fn in reversed(list(enumerate(stages))):
                ti = it - si
                if 0 <= ti < NT:
                    if si == 0:
                        sts[ti] = {}
                    fn(ti, sts[ti])
                    if si == 3 and ti % 4 == 3:
                        fm_after = ti // 4
                    if si == len(stages) - 1:
                        sts.pop(ti)
            if fm_after is not None:
                fm_group(fm_after)
        S.close()

    def phase3(l):
        S = Scope(P)
        qT = Ring([S.sbuf(f"sbq{i}", [64, T], BF16) for i in range(2)])
        kT = Ring([S.sbuf(f"sbk{i}", [64, T], BF16) for i in range(2)])
        vt = Ring([S.sbuf(f"sbv{i}", [128, NT, 64], BF16) for i in range(2)])
        e_r = Ring([S.sbuf(f"e{i}", [128, 512], F32) for i in range(3)])
        sp_r = Ring([S.sbuf(f"sp{i}", [128, 512], BF16) for i in range(4)])
        w_r = Ring([S.sbuf(f"w{i}", [128, 512], BF16) for i in range(4)])
        o_r = Ring([S.sbuf(f"osb{i}", [128, 4, 64], F32) for i in range(2)])
        pS = Ring([S.psum(f"pS{i}", [128, 512], F32) for i in range(2)], excl=True)
        p2 = Ring([S.psum(f"p2{i}", [128, 512], F32) for i in range(2)], excl=True)
        pD = Ring([S.psum(f"pD{i}", [128, 512], F32) for i in range(2)], excl=True)
        pacc = Ring([S.psum(f"pacc{i}", [128, 4, 64], F32) for i in range(2)], excl=True)
        heads = {}

        def ldh(h):
            q, rq = qT.next()
            k, rk = kT.next()
            v, rv = vt.next()
            P.dma(q[:], fmT[h * 64:(h + 1) * 64, :], writes=[rq])
            P.dma(k[:], fmT[512 + h * 64:512 + (h + 1) * 64, :], writes=[rk])
            P.dma(v[:], tmv[:, h * 64:(h + 1) * 64].rearrange("(kb p) d -> p kb d", p=128), writes=[rv])
            heads[h] = (q, rq, k, rk, v, rv)

        units = []
        for h in range(8):
            for c in range(8):
                for kb in range(4 * c + 3, -1, -1):
                    units.append(dict(h=h, c=c, kb=kb, first=(kb == 4 * c + 3), last=(kb == 0)))

        def stage1(u):
            h, c, kb = u["h"], u["c"], u["kb"]
            if u["first"] and c == 0:
                if h == 0:
                    ldh(0)
                if h + 1 < 8:
                    ldh(h + 1)
            q, rq, k, rk, v, rv = heads[h]
            i = kb - 4 * c
            q0 = 128 * i if i > 0 else 0
            u["q0"] = q0
            ps, r_ps = pS.next()
            P.pe.op(lambda e: e.matmul(ps[:, q0:512], lhsT=k[:, kb * 128:(kb + 1) * 128],
                                       rhs=q[:, c * 512 + q0:(c + 1) * 512], start=True, stop=(i < 0)),
                    reads=[rq, rk], writes=[r_ps])
            if i >= 0:
                P.pe.op(lambda e: e.matmul(ps[:, q0:q0 + 128], lhsT=ident_bf[:], rhs=tri["cb_lt"][:],
                                           start=False, stop=True),
                        reads=[r_ident, r_tri], writes=[r_ps])
            et, r_e = e_r.next()
            P.act.op(lambda e: e.activation(out=et[:, q0:512], in_=ps[:, q0:512], func=AF.Exp, scale=0.125),
                     reads=[r_ps], writes=[r_e])
            spt, r_sp = sp_r.next()
            P.act.op(lambda e: e.activation(out=spt[:, q0:512], in_=et[:, q0:512], func=AF.Ln, bias=1.0, scale=1.0),
                     reads=[r_e], writes=[r_sp])
            u.update(et=et, r_e=r_e, spt=spt, r_sp=r_sp)

        chain = {}

        def stage2(u):
            q0 = u["q0"]
            if u["first"]:
                chain["p2"], chain["r_p2"] = p2.next()
                chain["prev"] = None
            pp, r_pp = chain["p2"], chain["r_p2"]
            prev = chain["prev"]
            if prev is not None:
                pq0 = prev["q0"]
                P.pe.op(lambda e: e.matmul(pp[:, pq0:512], lhsT=tri["tri_lt"][:], rhs=prev["spt"][:, pq0:512],
                                           start=False, stop=False, skip_group_check=True),
                        reads=[prev["r_sp"], r_tri], writes=[r_pp])
            P.pe.op(lambda e: e.matmul(pp[:, q0:512], lhsT=tri["tri_ge"][:], rhs=u["spt"][:, q0:512],
                                       start=(prev is None), stop=True, skip_group_check=True),
                    reads=[u["r_sp"], r_tri], writes=[r_pp])
            chain["prev"] = u
            pd, r_pd = pD.next()
            P.act.op(lambda e: e.activation(out=pd[:, q0:512], in_=pp[:, q0:512], func=AF.Exp, scale=-1.0),
                     reads=[r_pp], writes=[r_pd])
            wt, r_w = w_r.next()
            P.dve.op(lambda e: e.tensor_tensor(out=wt[:, q0:512], in0=u["et"][:, q0:512], in1=pd[:, q0:512],
                                               op=ALU.mult),
                     reads=[u["r_e"], r_pd], writes=[r_w])
            u.update(wt=wt, r_w=r_w)

        accs = {}

        def stage3(u):
            h, c, kb, q0 = u["h"], u["c"], u["kb"], u["q0"]
            q, rq, k, rk, v, rv = heads[h]
            if u["first"]:
                accs["a"], accs["r"] = pacc.next()
            acc, r_acc = accs["a"], accs["r"]
            for ts in range(q0 // 128, 4):
                P.pe.op(lambda e: e.matmul(acc[:, ts, :], lhsT=u["wt"][:, ts * 128:(ts + 1) * 128], rhs=v[:, kb, :],
                                           start=(u["first"] and ts == q0 // 128), stop=(u["last"] and ts == 3),
                                           skip_group_check=True),
                        reads=[u["r_w"], rv], writes=[r_acc])
            if u["last"]:
                ot, r_o = o_r.next()
                P.dve.op(lambda e: e.tensor_copy(out=ot[:], in_=acc[:]), reads=[r_acc], writes=[r_o])
                P.dma(o_sb_d[c * 512:(c + 1) * 512, h * 64:(h + 1) * 64].rearrange("(s p) d -> p s d", p=128),
                      ot[:], reads=[r_o])

        n = len(units)
        for it in range(n + 3):
            if 0 <= it - 3 < n:
                stage3(units[it - 3])
            if it < n:
                stage1(units[it])
            if 0 <= it - 1 < n:
                stage2(units[it - 1])
        S.close()


    def phase4(l):
        S = Scope(P)
        qs = [S.sbuf(f"qs{g}", [128, 4, T], BF16) for g in range(2)]
        kse = [S.sbuf(f"kse{g}", [128, T], BF16) for g in range(2)]
        kw2 = S.sbuf("kw2", [64, 2, T], BF16)
        vs1 = S.sbuf("vs1", [128, 2, NT, 65], BF16)
        vw1 = S.sbuf("vw1", [128, 2, NT, 65], BF16)
        kcr = S.sbuf("kcr", [64, 2, 256], BF16)
        vov = S.sbuf("vov", [128, 2, 2, 65], BF16)
        ovT = S.sbuf("ovT_sb", [128, 2, 64], BF16)
        cmaskb = S.sbuf("cmaskb_sb", [128, 17, 128], BF16)
        cb_le = S.sbuf("cb_le_sb", [128, 128], BF16)
        cb_gt = S.sbuf("cb_gt_sb", [128, 128], BF16)
        rkeep = S.sbuf("rkeep_sb", [128, 128], F32)
        radd = S.sbuf("radd_sb", [128, 128], F32)
        r_q = [Res(), Res()]
        r_kse = [Res(), Res()]
        r_kw, r_vs, r_vw, r_kcr, r_vov, r_c4 = (Res() for _ in range(6))
        for g in range(2):
            P.dma(qs[g][0:64, :, :], fmT[1024 + 256 * g:1024 + 256 * (g + 1), :].rearrange("(hh d) t -> d hh t", d=64),
                  writes=[r_q[g]])
            P.dma_group([(kse[g][0:64, :], fmT[1536 + 64 * g:1536 + 64 * (g + 1), :], None, None),
                         (kse[g][64:128, :], eexp_d[:, :], None, None)], writes=[r_kse[g]])
        P.dma_group([(kw2[:, g, :], fmT[1664 + 64 * g:1664 + 64 * (g + 1), :], None, None) for g in range(2)], writes=[r_kw])
        P.pool.op(lambda e: e.memset(vs1[:], 1.0), writes=[r_vs])
        P.pool.op(lambda e: e.memset(vw1[:], 1.0), writes=[r_vw])
        P.pool.op(lambda e: e.memset(vov[:], 1.0), writes=[r_vov])
        P.dma_group([(vs1[:, g, :, 0:64],
                      tmv[:, 512 + 64 * g:512 + 64 * (g + 1)].rearrange("(kb p) d -> p kb d", p=128), None, None)
                     for g in range(2)], writes=[r_vs])
        P.dma_group([(vw1[:, g, :, 0:64],
                      tmv[:, 640 + 64 * g:640 + 64 * (g + 1)].rearrange("(kb p) d -> p kb d", p=128), None, None)
                     for g in range(2)], writes=[r_vw])
        P.dma_group([(ovT[:], ovT_d[:, :, :], None, None), (cmaskb[:], cmaskb_d[:, :, :], None, None),
                     (cb_le[:], cb_le_d[:, :], None, None), (cb_gt[:], cb_gt_d[:, :], None, None),
                     (rkeep[:], rkeep_d[:, :], None, None), (radd[:], radd_d[:, :], None, None)], writes=[r_c4])

        S2 = Scope(P)
        kcT = S2.sbuf("kcT", [128, T], BF16)
        vcT = S2.sbuf("vcT", [128, T], BF16)
        w1 = {"k": S2.sbuf("w1k", [128, 32, 256], BF16), "v": S2.sbuf("w1v", [128, 32, 256], BF16)}
        w2k = S2.sbuf("w2k", [128, 2, 64], BF16)
        w2ks = S2.sbuf("w2ks", [128, 2, 64], BF16)
        w2v = S2.sbuf("w2v", [128, 2, 64], BF16)
        posT = {"k": S2.sbuf("posTk_sb", [64, 32], BF16), "v": S2.sbuf("posTv_sb", [64, 32], BF16)}
        ccos = S2.sbuf("ccos", [128, 256], F32)
        csin = S2.sbuf("csin", [128, 256], F32)
        bias_sb = S2.sbuf("cbias", [128, 4], F32)
        r_x2, r_w1, r_w2, r_pos, r_cc, r_bias = (Res() for _ in range(6))
        P.dma_group([(kcT[:], fmT[1792:1920, :], None, None), (vcT[:], fmT[1920:2048, :], None, None)], writes=[r_x2])
        srcs = {"k": w1k_d, "v": w1v_d}
        P.dma_group([(w1[kd][64 * hf:64 * hf + 64, :, :], srcs[kd][l].rearrange("(l d) h -> d l h", d=64), P.pool, None)
                     for kd in ("k", "v") for hf in range(2)], writes=[r_w1])
        P.dma_group([(w2k[:], w2k_d[l].rearrange("(hc p) d -> p hc d", p=128), P.pool, None),
                     (w2ks[:], w2ksw_d[l].rearrange("(hc p) d -> p hc d", p=128), P.pool, None),
                     (w2v[:], w2v_d[l].rearrange("(hc p) d -> p hc d", p=128), P.pool, None)], writes=[r_w2])
        P.dma_group([(posT["k"][:], posTk_d[l], P.pool, None), (posT["v"][:], posTv_d[l], P.pool, None)], writes=[r_pos])
        P.dma_group([(ccos[:], cmp_cos_d[:, :], None, None), (csin[:], cmp_sin_d[:, :], None, None)], writes=[r_cc])
        pb = S2.psum("pb", [128, 4], F32)
        r_pb = Res(True)
        first = True
        for ki, kd in enumerate(("k", "v")):
            for hc in range(2):
                for ll in range(32):
                    P.pe.op(lambda e: e.matmul(pb[:, 2 * ki + hc:2 * ki + hc + 1],
                                               lhsT=w1[kd][0:64, ll, hc * 128:(hc + 1) * 128],
                                               rhs=posT[kd][:, ll:ll + 1], start=first, stop=False,
                                               skip_group_check=True),
                            reads=[r_w1, r_pos], writes=[r_pb])
                    first = False
        P.dve.op(lambda e: e.tensor_copy(out=bias_sb[:], in_=pb[:]), reads=[r_pb], writes=[r_bias])
        ph = Ring([S2.psum(f"ph{i}", [128, 256], F32) for i in range(2)], excl=True)
        pk = Ring([S2.psum(f"pk{i}", [128, 256], F32) for i in range(2)], excl=True)
        u_r = Ring([S2.sbuf(f"cu{i}", [128, 256], F32) for i in range(2)])
        t_r = Ring([S2.sbuf(f"ct{i}", [128, 256], F32) for i in range(2)])
        g_r = [S2.sbuf(f"cg{i}", [128, 256], BF16) for i in range(4)]
        r_g = [Res() for _ in range(4)]
        for i in range(4):
            P.pool.op(lambda e: e.memset(g_r[i][:], 0.0), writes=[r_g[i]])
        gi = 0
        xsrc = {"k": kcT, "v": vcT}
        for ki, kd in enumerate(("k", "v")):
            for g in range(2):
                gts = []
                for hc in range(2):
                    pht, r_ph = ph.next()
                    for ll in range(32):
                        P.pe.op(lambda e: e.matmul(pht[:, 0:255], lhsT=w1[kd][64 * g:64 * g + 64, ll, hc * 128:(hc + 1) * 128],
                                                   rhs=xsrc[kd][64 * g:64 * g + 64, ll:ll + 16 * 254 + 1:16],
                                                   start=(ll == 0), stop=(ll == 31)),
                                reads=[r_w1, r_x2], writes=[r_ph])
                    ut, r_u = u_r.next()
                    P.act.op(lambda e: e.activation(out=ut[:, 0:255], in_=pht[:, 0:255], func=AF.Identity,
                                                    bias=bias_sb[:, 2 * ki + hc:2 * ki + hc + 1], scale=1.0),
                             reads=[r_ph, r_bias], writes=[r_u])
                    tt_, r_t = t_r.next()
                    P.dve.op(lambda e: e.tensor_tensor(out=tt_[:, 0:255], in0=ut[:, 0:255], in1=ut[:, 0:255], op=ALU.mult),
                             reads=[r_u], writes=[r_t])
                    P.dve.op(lambda e: e.tensor_scalar(out=tt_[:, 0:255], in0=tt_[:, 0:255], scalar1=0.044715, scalar2=1.0,
                                                       op0=ALU.mult, op1=ALU.add), reads=[r_t], writes=[r_t])
                    P.dve.op(lambda e: e.tensor_tensor(out=tt_[:, 0:255], in0=tt_[:, 0:255], in1=ut[:, 0:255], op=ALU.mult),
                             reads=[r_t, r_u], writes=[r_t])
                    P.act.op(lambda e: e.activation(out=tt_[:, 0:255], in_=tt_[:, 0:255], func=AF.Sigmoid,
                                                    scale=1.5957691216057308), reads=[r_t], writes=[r_t])
                    gt_, r_gt = g_r[gi % 4], r_g[gi % 4]
                    gi += 1
                    P.dve.op(lambda e: e.tensor_tensor(out=gt_[:, 0:255], in0=ut[:, 0:255], in1=tt_[:, 0:255], op=ALU.mult),
                             reads=[r_t, r_u], writes=[r_gt])
                    gts.append((gt_, r_gt))
                if kd == "k":
                    pa, r_pa = pk.next()
                    pb2, r_pb2 = pk.next()
                    for hc in range(2):
                        P.pe.op(lambda e: e.matmul(pa[0:64, :], lhsT=w2k[:, hc, :], rhs=gts[hc][0][:], start=(hc == 0), stop=(hc == 1)),
                                reads=[r_w2, gts[hc][1]], writes=[r_pa])
                    for hc in range(2):
                        P.pe.op(lambda e: e.matmul(pb2[0:64, :], lhsT=w2ks[:, hc, :], rhs=gts[hc][0][:], start=(hc == 0), stop=(hc == 1)),
                                reads=[r_w2, gts[hc][1]], writes=[r_pb2])
                    t1, r_t1 = u_r.next()
                    t2, r_t2 = t_r.next()
                    P.dve.op(lambda e: e.tensor_tensor(out=t1[0:64, :], in0=pa[0:64, :], in1=ccos[0:64, :], op=ALU.mult),
                             reads=[r_pa, r_cc], writes=[r_t1])
                    P.dve.op(lambda e: e.tensor_tensor(out=t2[0:64, :], in0=pb2[0:64, :], in1=csin[0:64, :], op=ALU.mult),
                             reads=[r_pb2, r_cc], writes=[r_t2])
                    P.dve.op(lambda e: e.tensor_tensor(out=kcr[:, g, :], in0=t1[0:64, :], in1=t2[0:64, :], op=ALU.add),
                             reads=[r_t1, r_t2], writes=[r_kcr])
                else:
                    for nn in range(2):
                        pv, r_pv = pk.next()
                        for hc in range(2):
                            P.pe.op(lambda e: e.matmul(pv[:, 0:64], lhsT=gts[hc][0][:, nn * 128:(nn + 1) * 128],
                                                       rhs=w2v[:, hc, :], start=(hc == 0), stop=(hc == 1)),
                                    reads=[r_w2, gts[hc][1]], writes=[r_pv])
                        P.dve.op(lambda e: e.tensor_copy(out=vov[:, g, nn, 0:64], in_=pv[:, 0:64]),
                                 reads=[r_pv], writes=[r_vov])
        S2.close()
        P.barrier()

        pSr = Ring([S.psum(f"pSn{i}", [128, 4, 128], F32) for i in range(3)], excl=True)
        pOc = S.psum("pOc", [128, 4, 65], F32)
        pImp = S.psum("pImp", [128, 4, 64], F32)
        pOs = S.psum("pOs", [128, 4, 65], F32)
        pOw = S.psum("pOw", [128, 4, 65], F32)
        pMTb = S.psum("pMT", [128, 1024], BF16)
        r_pOc, r_pImp, r_pOs, r_pOw, r_pT4 = (Res(True) for _ in range(5))
        ex_r = Ring([S.sbuf(f"ex{i}", [128, 4, 128], BF16) for i in range(4)])
        sm_r = Ring([S.sbuf(f"sm{i}", [128, 32], F32) for i in range(2)])
        oc_r = Ring([S.sbuf(f"oc{i}", [128, 4, 64], F32) for i in range(2)])
        imp_r = Ring([S.sbuf(f"imp{i}", [128, 64], F32) for i in range(2)])
        sc_r = Ring([S.sbuf(f"sc{i}", [128, 64], F32) for i in range(2)])
        sc2_r = Ring([S.sbuf(f"sc2{i}", [128, 64], F32) for i in range(2)])
        m8_r = Ring([S.sbuf(f"m8{i}", [128, 16], F32) for i in range(2)])
        sel_r = Ring([S.sbuf(f"sel{i}", [128, 128], BF16) for i in range(2)])
        for i in range(2):
            P.pool.op(lambda e: e.memset(sel_r.tiles[i][:], 0.0), writes=[sel_r.res[i]])
        gt_r = Ring([S.sbuf(f"gt{i}", [128, 12], F32) for i in range(2)])
        cf_r = Ring([S.sbuf(f"cf{i}", [128, 16], F32) for i in range(2)])
        ta_r = Ring([S.sbuf(f"ta{i}", [128, 4, 64], F32) for i in range(2)])
        tb_r = Ring([S.sbuf(f"tb{i}", [128, 4, 64], F32) for i in range(2)])
        on_r = Ring([S.sbuf(f"on{i}", [128, 4, 64], F32) for i in range(2)])
        BIG = 240000.0

        def bc(ap, shape):
            return ap.to_broadcast(shape)

        def prologue(g, qt):
            st = {}
            qsl = qs[g][0:64, :, qt * 128:(qt + 1) * 128]
            gt_, r_gt = gt_r.next()
            P.dma(gt_[:], gates_d[qt * 128:(qt + 1) * 128, 12 * g:12 * (g + 1)], writes=[r_gt])
            st["gt"], st["r_gt"] = gt_, r_gt
            nns = [0] if qt < 16 else [0, 1]
            exs = []
            for nn in nns:
                ps, r_ps = pSr.next()
                qp = qt - 16 * nn
                msk = qp <= 16
                P.pe.op(lambda e: e.matmul(ps[:], lhsT=kcr[:, g, nn * 128:(nn + 1) * 128], rhs=qsl, start=True, stop=not msk),
                        reads=[r_kcr, r_q[g]], writes=[r_ps])
                if msk:
                    P.pe.op(lambda e: e.matmul(ps[:], lhsT=ident_bf[:], rhs=bc(cmaskb[:, qp:qp + 1, :], [128, 4, 128]),
                                               start=False, stop=True), reads=[r_ident, r_c4], writes=[r_ps])
                ex, r_ex = ex_r.next()
                P.act.op(lambda e: e.activation(out=ex[:], in_=ps[:], func=AF.Exp, scale=0.125), reads=[r_ps], writes=[r_ex])
                exs.append((nn, ex, r_ex))
            firstc = True
            for (nn, ex, r_ex) in exs:
                for hh in range(4):
                    P.pe.op(lambda e: e.matmul(pOc[:, hh, :], lhsT=ex[:, hh, :], rhs=vov[:, g, nn, :],
                                               start=firstc, stop=False, skip_group_check=True),
                            reads=[r_ex, r_vov], writes=[r_pOc])
                    firstc = False
            firstc = True
            for (nn, ex, r_ex) in exs:
                for hh in range(4):
                    P.pe.op(lambda e: e.matmul(pImp[:, hh, :], lhsT=ex[:, hh, :], rhs=ovT[:, nn, :],
                                               start=firstc, stop=False, skip_group_check=True),
                            reads=[r_ex, r_c4], writes=[r_pImp])
                    firstc = False
            sm, r_sm = sm_r.next()
            P.dve.op(lambda e: e.tensor_scalar(out=sm[:, 0:4], in0=pOc[:, :, 64], scalar1=1e-6, scalar2=None, op0=ALU.max),
                     reads=[r_pOc], writes=[r_sm])
            P.dve.op(lambda e: e.reciprocal(out=sm[:, 4:8], in_=sm[:, 0:4]), reads=[r_sm], writes=[r_sm])
            oc, r_oc = oc_r.next()
            P.dve.op(lambda e: e.tensor_tensor(out=oc[:], in0=pOc[:, :, 0:64], in1=bc(sm[:, 4:8].unsqueeze(2), [128, 4, 64]),
                                               op=ALU.mult), reads=[r_pOc, r_sm], writes=[r_oc])
            st["oc"], st["r_oc"] = oc, r_oc
            imp, r_imp = imp_r.next()
            P.dve.op(lambda e: e.tensor_scalar(out=imp[:], in0=pImp[:, 0, :], scalar1=sm[:, 4:5], scalar2=None, op0=ALU.mult),
                     reads=[r_pImp, r_sm], writes=[r_imp])
            for hh in range(1, 4):
                P.dve.op(lambda e: e.scalar_tensor_tensor(out=imp[:], in0=pImp[:, hh, :], scalar=sm[:, 4 + hh:5 + hh],
                                                          in1=imp[:], op0=ALU.mult, op1=ALU.add),
                         reads=[r_pImp, r_sm, r_imp], writes=[r_imp])
            sc, r_sc = sc_r.next()
            o0 = 62 - 2 * qt
            P.dve.op(lambda e: e.tensor_tensor(out=sc[:], in0=imp[:], in1=rkeep[:, o0:o0 + 64], op=ALU.mult),
                     reads=[r_imp, r_c4], writes=[r_sc])
            P.dve.op(lambda e: e.tensor_tensor(out=sc[:], in0=sc[:], in1=radd[:, o0:o0 + 64], op=ALU.add),
                     reads=[r_sc, r_c4], writes=[r_sc])
            P.dve.op(lambda e: e.memset(sc[:, 0:1], 1e6), reads=[r_sc], writes=[r_sc])
            m8, r_m8 = m8_r.next()
            sc2, r_sc2 = sc2_r.next()
            P.dve.op(lambda e: e.max(out=m8[:, 0:8], in_=sc[:]), reads=[r_sc], writes=[r_m8])
            P.dve.op(lambda e: e.match_replace(out=sc2[:], in_to_replace=m8[:, 0:8], in_values=sc[:], imm_value=-1e30),
                     reads=[r_sc, r_m8], writes=[r_sc2])
            P.dve.op(lambda e: e.max(out=m8[:, 8:16], in_=sc2[:]), reads=[r_sc2, r_m8], writes=[r_m8])
            sel, r_sel = sel_r.next()
            P.dve.op(lambda e: e.tensor_scalar(out=sel[:, 64:128], in0=sc[:], scalar1=m8[:, 15:16], scalar2=-BIG,
                                               op0=ALU.is_lt, op1=ALU.mult),
                     reads=[r_sc, r_m8], writes=[r_sel])
            st["sel"], st["r_sel"] = sel, r_sel
            st["r_sb"] = Res()
            return st

        def prologue_b(g, qt, st):
            sel, r_sel = st["sel"], st["r_sel"]
            P.pe.op(lambda e: e.transpose(out=pMTb[:, 0:128], in_=sel[:], identity=ident_bf[:]),
                    reads=[r_sel, r_ident], writes=[r_pT4])
            r_sb = st["r_sb"]
            P.dve.op(lambda e: e.tensor_copy(out=qs[g][64:128, :, qt * 128:(qt + 1) * 128],
                                             in_=bc(pMTb[64:128, 0:128].unsqueeze(1), [64, 4, 128])),
                     reads=[r_pT4], writes=[r_sb])

        def attend(g, qt, st):
            units = [("s", kb) for kb in range(qt + 1)] + [("w", kb) for kb in range(max(0, qt - 4), qt + 1)]
            pend = []

            def s1(kind, kb):
                ps, r_ps = pSr.next()
                if kind == "s":
                    diag = (kb == qt)
                    P.pe.op(lambda e: e.matmul(ps[:], lhsT=kse[g][:, kb * 128:(kb + 1) * 128],
                                               rhs=qs[g][:, :, qt * 128:(qt + 1) * 128], start=True, stop=not diag),
                            reads=[r_kse[g], r_q[g], st["r_sb"]], writes=[r_ps])
                    if diag:
                        P.pe.op(lambda e: e.matmul(ps[:], lhsT=ident_bf[:], rhs=bc(cb_le[:].unsqueeze(1), [128, 4, 128]),
                                                   start=False, stop=True), reads=[r_ident, r_c4], writes=[r_ps])
                else:
                    m = cb_le if kb == qt else (cb_gt if kb == qt - 4 else None)
                    P.pe.op(lambda e: e.matmul(ps[:], lhsT=kw2[:, g, kb * 128:(kb + 1) * 128],
                                               rhs=qs[g][0:64, :, qt * 128:(qt + 1) * 128], start=True, stop=(m is None)),
                            reads=[r_kw, r_q[g]], writes=[r_ps])
                    if m is not None:
                        P.pe.op(lambda e: e.matmul(ps[:], lhsT=ident_bf[:], rhs=bc(m[:].unsqueeze(1), [128, 4, 128]),
                                                   start=False, stop=True), reads=[r_ident, r_c4], writes=[r_ps])
                ex, r_ex = ex_r.next()
                P.act.op(lambda e: e.activation(out=ex[:], in_=ps[:], func=AF.Exp, scale=0.125), reads=[r_ps], writes=[r_ex])
                return (kind, kb, ex, r_ex)

            def s2(kind, kb, ex, r_ex):
                if kind == "s":
                    po, r_po, v1, r_v1, first = pOs, r_pOs, vs1, r_vs, (kb == 0)
                else:
                    po, r_po, v1, r_v1, first = pOw, r_pOw, vw1, r_vw, (kb == max(0, qt - 4))
                for hh in range(4):
                    P.pe.op(lambda e: e.matmul(po[:, hh, :], lhsT=ex[:, hh, :], rhs=v1[:, g, kb, :],
                                               start=(first and hh == 0), stop=False, skip_group_check=True),
                            reads=[r_ex, r_v1], writes=[r_po])

            for i in range(len(units) + 2):
                if i < len(units):
                    pend.append(s1(*units[i]))
                if 0 <= i - 2 < len(units):
                    s2(*pend[i - 2])

        def combine(g, qt, st):
            gt_, r_gt = st["gt"], st["r_gt"]
            gv = gt_[:, 0:12].rearrange("p (h b) -> p h b", b=3)
            cf, r_cf = cf_r.next()
            P.dve.op(lambda e: e.reciprocal(out=cf[:, 0:4], in_=pOs[:, :, 64]), reads=[r_pOs], writes=[r_cf])
            P.dve.op(lambda e: e.reciprocal(out=cf[:, 4:8], in_=pOw[:, :, 64]), reads=[r_pOw, r_cf], writes=[r_cf])
            P.dve.op(lambda e: e.tensor_tensor(out=cf[:, 8:12], in0=cf[:, 0:4], in1=gv[:, :, 1], op=ALU.mult),
                     reads=[r_cf, r_gt], writes=[r_cf])
            P.dve.op(lambda e: e.tensor_tensor(out=cf[:, 12:16], in0=cf[:, 4:8], in1=gv[:, :, 2], op=ALU.mult),
                     reads=[r_cf, r_gt], writes=[r_cf])
            ta, r_ta = ta_r.next()
            tb, r_tb = tb_r.next()
            on, r_on = on_r.next()
            P.dve.op(lambda e: e.tensor_tensor(out=ta[:], in0=pOs[:, :, 0:64], in1=bc(cf[:, 8:12].unsqueeze(2), [128, 4, 64]),
                                               op=ALU.mult), reads=[r_pOs, r_cf], writes=[r_ta])
            P.dve.op(lambda e: e.tensor_tensor(out=tb[:], in0=pOw[:, :, 0:64], in1=bc(cf[:, 12:16].unsqueeze(2), [128, 4, 64]),
                                               op=ALU.mult), reads=[r_pOw, r_cf], writes=[r_tb])
            P.pool.op(lambda e: e.tensor_tensor(out=on[:], in0=st["oc"][:], in1=bc(gv[:, :, 0:1], [128, 4, 64]), op=ALU.mult),
                      reads=[st["r_oc"], r_gt], writes=[r_on])
            P.pool.op(lambda e: e.tensor_tensor(out=ta[:], in0=ta[:], in1=tb[:], op=ALU.add), reads=[r_ta, r_tb], writes=[r_ta])
            P.pool.op(lambda e: e.tensor_tensor(out=on[:], in0=on[:], in1=ta[:], op=ALU.add), reads=[r_on, r_ta], writes=[r_on])
            P.dma(o_nsa_d[qt * 128:(qt + 1) * 128, 256 * g:256 * (g + 1)], on[:].rearrange("p h d -> p (h d)"), reads=[r_on])

        tiles = [(g, qt) for g in range(2) for qt in range(NT)]
        if nsa_tiles is not None:
            tiles = nsa_tiles
        stn = prologue(*tiles[0])
        prologue_b(*tiles[0], stn)
        for i, (g, qt) in enumerate(tiles):
            stc = stn
            if i + 1 < len(tiles):
                stn = prologue(*tiles[i + 1])
            attend(g, qt, stc)
            if i + 1 < len(tiles):
                prologue_b(*tiles[i + 1], stn)
            combine(g, qt, stc)
        S.close()

    def rstd_ops(st, r_st, c0, n, dim):
        P.act.op(lambda e: e.activation(out=st[:, c0 + n:c0 + 2 * n], in_=st[:, c0:c0 + n], func=AF.Ln,
                                        scale=1.0 / dim, bias=eps_t[:, 0:1]), reads=[r_st, r_eps], writes=[r_st])
        P.act.op(lambda e: e.activation(out=st[:, c0 + 2 * n:c0 + 3 * n], in_=st[:, c0 + n:c0 + 2 * n], func=AF.Exp,
                                        scale=-0.5), reads=[r_st], writes=[r_st])

    def phase5a(l, x_src):
        S = Scope(P)
        wo = S.sbuf("wo_sb", [128, 8, D], BF16)
        r_wo = Res()
        P.dma_group([(wo[:, k, :], w_out_d[l, k * 128:(k + 1) * 128, :], P.pool, None) for k in range(8)], writes=[r_wo])
        gbc = S.sbuf("gbc", [128, D], F32)
        g1bc = S.sbuf("g1bc", [128, D], F32)
        r_bc = Res()
        P.dma_group([(gbc[:, 0:512], sbg_d[l].partition_broadcast(128), None, None),
                     (gbc[:, 512:1024], nsag_d[l].partition_broadcast(128), None, None),
                     (g1bc[:], modrow[l, 2 * D:3 * D].partition_broadcast(128), None, None)], writes=[r_bc])
        oin = Ring([S.sbuf(f"oin{i}", [128, D], F32) for i in range(3)])
        xin = Ring([S.sbuf(f"x5_{i}", [128, D], F32) for i in range(8)])
        junk = S.sbuf("junk5", [128, D], F32)
        r_junk = Res()
        stat = Ring([S.sbuf(f"st5_{i}", [128, 12], F32) for i in range(12)])
        mixed = Ring([S.sbuf(f"mixed{i}", [128, D], BF16) for i in range(3)])
        mixT = Ring([S.sbuf(f"mixT{i}", [128, 8, 128], BF16) for i in range(3)])
        tmp = Ring([S.sbuf(f"tmp5_{i}", [128, D], F32) for i in range(5)])
        xn2 = Ring([S.sbuf(f"xn2_{i}", [128, D], BF16) for i in range(3)])
        h2 = Ring([S.sbuf(f"h2_{i}", [128, 8, 512], BF16) for i in range(2)])
        pT_r = Ring([S.psum(f"pT5{i}", [128, 8, 128], BF16) for i in range(4)], excl=True)
        pa_r = Ring([S.psum(f"pa5{i}", [128, 512], F32) for i in range(4)], excl=True)
        B2 = modT[l][:, 24:32]
        ld = {}

        def load(ti):
            o, r_o = oin.next()
            xt, r_x = xin.next()
            rows = slice(ti * 128, (ti + 1) * 128)
            P.dma_group([(o[:, 0:512], o_sb_d[rows, :], None, None), (o[:, 512:1024], o_nsa_d[rows, :], None, None)],
                        writes=[r_o])
            P.dma(xt[:], x_src[rows, :], writes=[r_x])
            ld[ti] = (o, r_o, xt, r_x)

        def g1(ti, d):
            o, r_o, xt, r_x = ld.pop(ti)
            if ti + 1 < NT:
                load(ti + 1)
            st, r_st = stat.next()
            for hf in range(2):
                P.act.op(lambda e: e.activation(out=junk[:, 0:512], in_=o[:, hf * 512:(hf + 1) * 512], func=AF.Square,
                                                accum_out=st[:, hf:hf + 1]), reads=[r_o], writes=[r_junk, r_st])
            rstd_ops(st, r_st, 0, 2, 512)
            d.update(o=o, r_o=r_o, xt=xt, r_x=r_x, st=st, r_st=r_st)

        def g2(ti, d):
            o, r_o, st, r_st = d["o"], d["r_o"], d["st"], d["r_st"]
            mx, r_mx = mixed.next()
            for hf in range(2):
                P.dve.op(lambda e: e.scalar_tensor_tensor(out=mx[:, hf * 512:(hf + 1) * 512], in0=o[:, hf * 512:(hf + 1) * 512],
                                                          scalar=st[:, 4 + hf:5 + hf], in1=gbc[:, hf * 512:(hf + 1) * 512],
                                                          op0=ALU.mult, op1=ALU.mult),
                         reads=[r_o, r_st, r_bc], writes=[r_mx])
            d.update(mx=mx, r_mx=r_mx)

        def g3(ti, d):
            mx, r_mx = d["mx"], d["r_mx"]
            pT, r_pT = pT_r.next()
            for k in range(8):
                P.pe.op(lambda e: e.transpose(out=pT[:, k, :], in_=mx[:, k * 128:(k + 1) * 128], identity=ident_bf[:]),
                        reads=[r_mx, r_ident], writes=[r_pT])
            d.update(pT=pT, r_pT=r_pT)

        def g4(ti, d):
            mT, r_mT = mixT.next()
            P.act.op(lambda e: e.activation(out=mT[:], in_=d["pT"][:], func=AF.Copy), reads=[d["r_pT"]], writes=[r_mT])
            d.update(mT=mT, r_mT=r_mT)

        def g5(ti, d):
            mT, r_mT = d["mT"], d["r_mT"]
            pas = []
            for hf in range(2):
                pa, r_pa = pa_r.next()
                for k in range(8):
                    P.pe.op(lambda e: e.matmul(pa[:], lhsT=mT[:, k, :], rhs=wo[:, k, hf * 512:(hf + 1) * 512],
                                               start=(k == 0), stop=(k == 7)), reads=[r_mT, r_wo], writes=[r_pa])
                pas.append((pa, r_pa))
            d.update(pas=pas)

        def g6(ti, d):
            tm_, r_tm = tmp.next()
            for hf in range(2):
                pa, r_pa = d["pas"][hf]
                P.dve.op(lambda e: e.tensor_tensor(out=tm_[:, hf * 512:(hf + 1) * 512], in0=pa[:],
                                                   in1=g1bc[:, hf * 512:(hf + 1) * 512], op=ALU.mult),
                         reads=[r_pa, r_bc], writes=[r_tm])
            d.update(tm=tm_, r_tm=r_tm)

        def g7(ti, d):
            rows = slice(ti * 128, (ti + 1) * 128)
            tm_, r_tm, xt, r_x = d["tm"], d["r_tm"], d["xt"], d["r_x"]
            P.pool.op(lambda e: e.tensor_tensor(out=tm_[:], in0=tm_[:], in1=xt[:], op=ALU.add),
                      reads=[r_tm, r_x], writes=[r_tm])
            P.dma(x1_d[rows, :], tm_[:], reads=[r_tm])

        def g8(ti, d):
            tm_, r_tm, st, r_st = d["tm"], d["r_tm"], d["st"], d["r_st"]
            P.act.op(lambda e: e.activation(out=junk[:], in_=tm_[:], func=AF.Square, accum_out=st[:, 6:7]),
                     reads=[r_tm], writes=[r_junk, r_st])
            rstd_ops(st, r_st, 6, 1, D)

        def g9(ti, d):
            tm_, r_tm, st, r_st = d["tm"], d["r_tm"], d["st"], d["r_st"]
            xn, r_xn = xn2.next()
            P.dve.op(lambda e: e.tensor_scalar(out=xn[:], in0=tm_[:], scalar1=st[:, 8:9], scalar2=None, op0=ALU.mult),
                     reads=[r_tm, r_st], writes=[r_xn])
            d.update(xn=xn, r_xn=r_xn)

        def g10(ti, d):
            xn, r_xn = d["xn"], d["r_xn"]
            pT, r_pT = pT_r.next()
            for k in range(8):
                P.pe.op(lambda e: e.transpose(out=pT[:, k, :], in_=xn[:, k * 128:(k + 1) * 128], identity=ident_bf[:]),
                        reads=[r_xn, r_ident], writes=[r_pT])
            d.update(pT=pT, r_pT=r_pT)

        def g11(ti, d):
            pT, r_pT = d["pT"], d["r_pT"]
            if ti % 4 == 0:
                h2cur[0] = h2.next()
            hh4, r_hh = h2cur[0]
            hh = hh4[:, :, (ti % 4) * 128:(ti % 4 + 1) * 128]
            for k in range(8):
                if ti % 2 == 0:
                    P.act.op(lambda e: e.activation(out=hh[:, k, :], in_=pT[:, k, :], func=AF.Identity,
                                                    scale=A2[l][:, k:k + 1], bias=B2[:, k:k + 1]),
                             reads=[r_pT, r_A[l], r_modT[l]], writes=[r_hh])
                else:
                    P.dve.op(lambda e: e.tensor_scalar(out=hh[:, k, :], in0=pT[:, k, :], scalar1=A2[l][:, k:k + 1],
                                                       scalar2=B2[:, k:k + 1], op0=ALU.mult, op1=ALU.add),
                             reads=[r_pT, r_A[l], r_modT[l]], writes=[r_hh])
            if ti % 4 == 3:
                g0 = (ti // 4) * 512
                P.dma(h2T_d[:, g0:g0 + 512].rearrange("(k p) t -> p k t", p=128), hh4[:], reads=[r_hh])

        h2cur = [None]
        load(0)
        stages = [g1, g2, g3, g4, g5, g6, g7, g8, g9, g10, g11]
        sts = {}
        for it in range(NT + len(stages) - 1):
            for si, fn in enumerate(stages):
                ti = it - si
                if 0 <= ti < NT:
                    if si == 0:
                        sts[ti] = {}
                    fn(ti, sts[ti])
                    if si == len(stages) - 1:
                        sts.pop(ti)
        S.close()

    GT = 256
    NG = T // GT

    def phase5b(l, x_dst, final):
        S = Scope(P)
        wf = S.sbuf("wf_sb", [128, 8, 2 * DFF], BF16)
        wd = S.sbuf("wd_sb", [128, 22, D], BF16)
        r_wd = Res()
        r_wfb = [Res() for _ in range(4)]
        for blk in (0, 2, 1, 3):
            c0 = blk * 1408
            P.dma_group([(wf[:, k, c0:c0 + 1408], wffn_d[l, k * 128:(k + 1) * 128, c0:c0 + 1408], P.pool, None)
                         for k in range(8)], writes=[r_wfb[blk]])
            if blk == 2:
                P.dma_group([(wd[:, fc, :], wdn_d[l, fc * 128:(fc + 1) * 128, :], P.pool, None) for fc in range(22)],
                            writes=[r_wd])
        cw = S.sbuf("cw_sb", [128, 44, 3], F32)
        cb = S.sbuf("cb_sb", [128, 44], F32)
        g2bc = S.sbuf("g2bc", [128, D], F32)
        fgbc = S.sbuf("fgbc", [128, D], F32)
        r_cp = Res()
        P.dma_group([(cw[:], cw_d[l], None, None), (cb[:], cb_d[l], None, None),
                     (g2bc[:], modrow[l, 5 * D:6 * D].partition_broadcast(128), None, None),
                     (fgbc[:], fg_d.partition_broadcast(128), None, None)], writes=[r_cp])
        hal = S.sbuf("halo", [128, 44, 2], F32)
        r_hal = Res()
        P.pool.op(lambda e: e.memset(hal[:], 0.0), writes=[r_hal])
        r_hals = [Res() for _ in range(44)]
        for rr in r_hals:
            rr.w = list(r_hal.w)
        r_uhs = [Res() for _ in range(4)]
        h2 = Ring([S.sbuf(f"h2g{i}", [128, 8, GT], BF16) for i in range(2)])
        gT = S.sbuf("gT", [128, 22, GT], BF16)
        r_gT = Res()
        us = Ring([S.sbuf(f"us{i}", [128, GT + 2], F32) for i in range(4)])
        ys = Ring([S.sbuf(f"ys{i}", [128, GT], F32) for i in range(6)])
        sa_r = Ring([S.sbuf(f"sa{i}", [128, GT], F32) for i in range(2)])
        x1t = Ring([S.sbuf(f"x1t{i}", [128, D], F32) for i in range(2)])
        tmp = Ring([S.sbuf(f"tmpb{i}", [128, D], F32) for i in range(2)])
        junk = S.sbuf("junkb", [128, D], F32)
        r_junk = Res()
        stat = Ring([S.sbuf(f"stb{i}", [128, 4], F32) for i in range(2)])
        pu_r = Ring([S.psum(f"pu{i}", [128, GT], F32) for i in range(4)], excl=True)
        pd_r = Ring([S.psum(f"pd{i}", [128, 512], F32) for i in range(4)], excl=True)
        ld = {}

        def load(gi):
            h, r_h = h2.next()
            P.dma(h[:], h2T_d[:, gi * GT:(gi + 1) * GT].rearrange("(k p) t -> p k t", p=128), writes=[r_h])
            ld[gi] = (h, r_h)

        load(0)
        for gi in range(NG):
            h, r_h = ld.pop(gi)
            if gi + 1 < NG:
                load(gi + 1)
            pend_g = None

            def gate(fc_, ys_):
                sa, r_sa = sa_r.next()
                P.act.op(lambda e: e.activation(out=sa[:], in_=ys_[0][0][:], func=AF.Silu), reads=[ys_[0][1]], writes=[r_sa])
                P.dve.op(lambda e: e.tensor_tensor(out=gT[:, fc_, :], in0=sa[:], in1=ys_[1][0][:], op=ALU.mult),
                         reads=[r_sa, ys_[1][1]], writes=[r_gT])

            for fc in range(22):
                ysab = []
                uts = []
                for part in range(2):
                    ci = part * 22 + fc
                    ui = us.i
                    u, r_u = us.next()
                    r_uh = r_uhs[ui]
                    P.pool.op(lambda e: e.tensor_copy(out=u[:, 0:2], in_=hal[:, ci, :]), reads=[r_hals[ci]], writes=[r_uh])
                    uts.append((u, r_u, r_uh))
                for part in range(2):
                    ci = part * 22 + fc
                    pu, r_pu = pu_r.next()
                    for k in range(8):
                        P.pe.op(lambda e: e.matmul(pu[:], lhsT=wf[:, k, ci * 128:(ci + 1) * 128], rhs=h[:, k, :],
                                                   start=(k == 0), stop=(k == 7)),
                                reads=[r_wfb[(ci * 128) // 1408], r_h], writes=[r_pu])
                    u, r_u, r_uh = uts[part]
                    y, r_y = ys.next()
                    P.act.op(lambda e: e.activation(out=u[:, 2:GT + 2], in_=pu[:], func=AF.Copy), reads=[r_pu], writes=[r_u])
                    P.act.op(lambda e: e.activation(out=y[:], in_=pu[:], func=AF.Identity, scale=cw[:, ci, 2:3],
                                                    bias=cb[:, ci:ci + 1]), reads=[r_pu, r_cp], writes=[r_y])
                    P.act.op(lambda e: e.activation(out=hal[:, ci, :], in_=pu[:, GT - 2:GT], func=AF.Copy),
                             reads=[r_pu], writes=[r_hals[ci]])
                    P.dve.op(lambda e: e.scalar_tensor_tensor(out=y[:], in0=u[:, 1:GT + 1], scalar=cw[:, ci, 1:2], in1=y[:],
                                                              op0=ALU.mult, op1=ALU.add),
                             reads=[r_u, r_uh, r_cp, r_y], writes=[r_y])
                    P.dve.op(lambda e: e.scalar_tensor_tensor(out=y[:], in0=u[:, 0:GT], scalar=cw[:, ci, 0:1], in1=y[:],
                                                              op0=ALU.mult, op1=ALU.add),
                             reads=[r_u, r_uh, r_cp, r_y], writes=[r_y])
                    ysab.append((y, r_y))
                if pend_g is not None:
                    gate(*pend_g)
                pend_g = (fc, ysab)
            gate(*pend_g)
            for ts in range(GT // 128):
                ti = gi * (GT // 128) + ts
                rows = slice(ti * 128, (ti + 1) * 128)
                x1, r_x1 = x1t.next()
                P.dma(x1[:], x1_d[rows, :], writes=[r_x1])
                tm_, r_tm = tmp.next()
                for hf in range(2):
                    pd, r_pd = pd_r.next()
                    for fc in range(22):
                        P.pe.op(lambda e: e.matmul(pd[:], lhsT=gT[:, fc, ts * 128:(ts + 1) * 128],
                                                   rhs=wd[:, fc, hf * 512:(hf + 1) * 512], start=(fc == 0), stop=(fc == 21)),
                                reads=[r_gT, r_wd], writes=[r_pd])
                    P.dve.op(lambda e: e.tensor_tensor(out=tm_[:, hf * 512:(hf + 1) * 512], in0=pd[:],
                                                       in1=g2bc[:, hf * 512:(hf + 1) * 512], op=ALU.mult),
                             reads=[r_pd, r_cp], writes=[r_tm])
                P.pool.op(lambda e: e.tensor_tensor(out=tm_[:], in0=tm_[:], in1=x1[:], op=ALU.add),
                          reads=[r_tm, r_x1], writes=[r_tm])
                if not final:
                    P.dma(x_dst[rows, :], tm_[:], reads=[r_tm])
                else:
                    st, r_st = stat.next()
                    P.act.op(lambda e: e.activation(out=junk[:], in_=tm_[:], func=AF.Square, accum_out=st[:, 0:1]),
                             reads=[r_tm], writes=[r_junk, r_st])
                    rstd_ops(st, r_st, 0, 1, D)
                    P.dve.op(lambda e: e.scalar_tensor_tensor(out=x1[:], in0=tm_[:], scalar=st[:, 2:3], in1=fgbc[:],
                                                              op0=ALU.mult, op1=ALU.mult),
                             reads=[r_tm, r_st, r_cp, r_x1], writes=[r_x1])
                    P.dma(x_dst[rows, :], x1[:], reads=[r_x1])
        S.close()

    if stop_after is None:
        for l in range(L):
            x_src = x_in if l == 0 else xbuf_d
            phase0(l)
            P.barrier()
            phase1(l, x_src)
            P.barrier()
            phase3(l)
            P.barrier()
            phase4(l)
            P.barrier()
            phase5a(l, x_src)
            P.barrier()
            phase5b(l, out_d if l == L - 1 else xbuf_d, final=(l == L - 1))
            P.barrier()
    else:
        phase0(0)
        P.barrier()
        phase1(0, x_in)
        P.barrier()
        if stop_after in ("p3", "l0"):
            phase3(0)
            P.barrier()
        if stop_after in ("p4", "l0"):
            phase4(0)
            P.barrier()
        if stop_after == "l0":
            phase5a(0, x_in)
            P.barrier()
            phase5b(0, xbuf_d, final=False)
            P.barrier()
        S = Scope(P)
        z = S.sbuf("zz", [128, D], F32)
        rz = Res()
        P.dve.op(lambda e: e.memset(z[:], 0.0), writes=[rz])
        for ti in range(NT):
            P.dma(out_d[ti * 128:(ti + 1) * 128, :], z[:], reads=[rz])
        S.close()
    P.finish()
    return nc


def host_inputs(inputs):
    cols = _ext_cols()
    f32 = lambda a: np.ascontiguousarray(np.asarray(a, dtype=np.float32))
    w_ext = f32(np.asarray(inputs["w_in"])[:, :, cols])
    shared = {
        "ln1T": f32(np.asarray(inputs["ln1_g"]).reshape(L, 8, 128).transpose(0, 2, 1)),
        "ln2T": f32(np.asarray(inputs["ln2_g"]).reshape(L, 8, 128).transpose(0, 2, 1)),
        "w_ada": f32(inputs["w_ada"]),
        "b_adaT": f32(np.asarray(inputs["b_ada"]).reshape(L, 48, 128).transpose(0, 2, 1)),
        "w_ext": w_ext,
        "cmp_w1_k": f32(inputs["cmp_w1_k"]), "cmp_w1_v": f32(inputs["cmp_w1_v"]),
        "cmp_w2_k": f32(inputs["cmp_w2_k"]), "cmp_w2_v": f32(inputs["cmp_w2_v"]),
        "cmp_w2_k_sw": f32(np.asarray(inputs["cmp_w2_k"])[:, :, _swap64(np.arange(64))]),
        "sb_out_g": f32(inputs["sb_out_g"]), "nsa_out_g": f32(inputs["nsa_out_g"]),
        "w_out": f32(inputs["w_out"]), "ffn_w_in": f32(inputs["ffn_w_in"]), "ffn_w_down": f32(inputs["ffn_w_down"]),
        "conv_w": f32(np.asarray(inputs["ffn_conv_w"]).reshape(L, 3, 44, 128).transpose(0, 3, 2, 1)),
        "conv_b": f32(np.asarray(inputs["ffn_conv_b"]).reshape(L, 44, 128).transpose(0, 2, 1)),
        "final_g": f32(inputs["final_g"]),
        "posTk": f32(np.asarray(inputs["cmp_pos_k"]).transpose(0, 2, 1)),
        "posTv": f32(np.asarray(inputs["cmp_pos_v"]).transpose(0, 2, 1)),
    }
    shared.update(_consts())
    x = np.asarray(inputs["x"])
    c = np.asarray(inputs["c"])
    in_maps = []
    for b in range(8):
        m = dict(shared)
        m["x"] = f32(x[b])
        m["cT"] = f32(c[b].reshape(8, 128).T)
        in_maps.append(m)
    return in_maps


def kernel(**inputs):
    in_maps = host_inputs(inputs)
    nc = build_program()
    res = run_bass_kernel_spmd(nc, in_maps, core_ids=list(range(8)))
    return np.stack([r["out"] for r in res.results], axis=0).astype(np.float32)
```
